# Optimizing a Trainium2 kernel written in Bass

```python
import math
import jax, jax.numpy as jnp
from jax import lax
import numpy as np

D_MODEL = 1024
BATCH = 16
SEQ = 2048
DEPTH = 2

HEAD_DIM = 64
GDN_HEADS = 8
GDN_WIDTH = GDN_HEADS * HEAD_DIM
GDN_CONV = 4
GDN_CHUNK = 64
NSA_HEADS = 8
NSA_KV_HEADS = 2
NSA_GROUP = NSA_HEADS // NSA_KV_HEADS
NSA_WIDTH = NSA_HEADS * HEAD_DIM
NSA_KV_WIDTH = NSA_KV_HEADS * HEAD_DIM
CMP_BLOCK = 32
CMP_STRIDE = 16
CMP_HIDDEN = 256
SLC_BLOCK = 64
SLC_TOPN = 8
WINDOW = 512
SPARSE_Q_BLOCK = 64
MIX_WIDTH = GDN_WIDTH + NSA_WIDTH
N_EXPERTS = 16
N_EXPERT_GROUPS = 4
EXPERTS_PER_GROUP = N_EXPERTS // N_EXPERT_GROUPS
MOE_TOP_K = 2
EXPERT_FF = 512
MOE_ROW_BLOCK = 256
RMS_EPS = 1e-6
NEG_BIG = -1e30
SEL_BIG = 1e9
IN_SPLITS = (GDN_WIDTH,) * 4 + (GDN_HEADS,) * 2 + (NSA_WIDTH,) + (NSA_KV_WIDTH,) * 6 + (3 * NSA_HEADS,)
IN_COLS = sum(IN_SPLITS)

kernel_name = 'hybrid_gdn_nsa_grouped_moe_adaln'


def rms_norm(x, g):
    xf = x.astype(jnp.float32)
    y = xf * lax.rsqrt(jnp.mean(xf * xf, axis=-1, keepdims=True) + RMS_EPS)
    return (y * g.astype(jnp.float32)).astype(x.dtype)


def l2_norm(x):
    return x * lax.rsqrt(jnp.sum(x * x, axis=-1, keepdims=True) + RMS_EPS)


def alibi_slopes(n):
    return (2.0 ** (-8.0 * np.arange(1, n + 1) / n)).astype(np.float32)


def masked_softmax(scores, mask):
    return jax.nn.softmax(jnp.where(mask, scores, NEG_BIG), axis=-1) * mask


def causal_depthwise_conv(x, w):
    return lax.conv_general_dilated(
        x, w[:, None, :].astype(x.dtype), window_strides=(1,), padding=[(GDN_CONV - 1, 0)],
        dimension_numbers=('NWC', 'WIO', 'NWC'), feature_group_count=x.shape[-1])


def gated_delta_rule_chunked(q, k, v, g, beta):
    B, H, S, DK = q.shape
    DV = v.shape[-1]
    C = GDN_CHUNK
    N = S // C
    q = (q * DK ** -0.5).reshape(B, H, N, C, DK)
    k = k.reshape(B, H, N, C, DK)
    v = v.reshape(B, H, N, C, DV)
    beta = beta.reshape(B, H, N, C, 1)
    g = jnp.cumsum(g.reshape(B, H, N, C), axis=-1)
    pos = jnp.arange(C)
    causal = pos[:, None] >= pos[None, :]
    strict = pos[:, None] > pos[None, :]
    decay = jnp.exp(jnp.where(causal, g[..., :, None] - g[..., None, :], -jnp.inf))
    kb = k * beta
    lower = jnp.where(strict, jnp.einsum('bhnid,bhnjd->bhnij', kb, k) * decay, 0.0)
    eye = jnp.eye(C, dtype=jnp.float32)
    tmat = lax.linalg.triangular_solve(eye + lower, jnp.broadcast_to(eye, lower.shape),
                                       left_side=True, lower=True, unit_diagonal=True)
    u = tmat @ (v * beta)
    w = tmat @ (kb * jnp.exp(g)[..., None])
    qk = jnp.einsum('bhnid,bhnjd->bhnij', q, k) * decay
    g_last = g[..., -1:]
    q_dec = q * jnp.exp(g)[..., None]
    k_dec = k * jnp.exp(g_last - g)[..., None]
    chunk_decay = jnp.exp(g_last)[..., None]

    def step(state, inp):
        q_i, k_i, u_i, w_i, qk_i, cd_i = inp
        v_new = u_i - w_i @ state
        o_i = q_i @ state + qk_i @ v_new
        state = state * cd_i + jnp.swapaxes(k_i, -1, -2) @ v_new
        return state, o_i

    xs = tuple(jnp.moveaxis(t, 2, 0) for t in (q_dec, k_dec, u, w, qk, chunk_decay))
    _, o = lax.scan(step, jnp.zeros((B, H, DK, DV), jnp.float32), xs)
    return jnp.moveaxis(o, 0, 2).reshape(B, H, S, DV)


def gdn_mixer(q, k, v, z, a, b, conv_w, a_log, dt_bias, norm_g):
    B, S, _ = q.shape
    qkv = jax.nn.silu(causal_depthwise_conv(jnp.concatenate([q, k, v], axis=-1), conv_w))
    q, k, v = jnp.split(qkv, 3, axis=-1)
    heads = lambda t: t.reshape(B, S, GDN_HEADS, HEAD_DIM).transpose(0, 2, 1, 3).astype(jnp.float32)
    q, k, v = l2_norm(heads(q)), l2_norm(heads(k)), heads(v)
    beta = jax.nn.sigmoid(b.astype(jnp.float32)).transpose(0, 2, 1)
    g = (-jnp.exp(a_log.astype(jnp.float32))
         * jax.nn.softplus(a.astype(jnp.float32) + dt_bias.astype(jnp.float32))).transpose(0, 2, 1)
    o = gated_delta_rule_chunked(q, k, v, g, beta).transpose(0, 2, 1, 3)
    o = rms_norm(o, norm_g) * jax.nn.silu(z.reshape(B, S, GDN_HEADS, HEAD_DIM).astype(jnp.float32))
    return o.reshape(B, S, GDN_WIDTH).astype(z.dtype)


def compress_blocks(x, pe, w1, b1, w2, b2):
    S = x.shape[2]
    n_cmp = (S - CMP_BLOCK) // CMP_STRIDE + 1
    idx = np.arange(n_cmp)[:, None] * CMP_STRIDE + np.arange(CMP_BLOCK)[None, :]
    blk = x[:, :, idx] + pe
    flat = blk.reshape(blk.shape[0], blk.shape[1], n_cmp, CMP_BLOCK * HEAD_DIM)
    return jax.nn.silu(flat @ w1 + b1) @ w2 + b2


def overlap_matrix(n_cmp, n_slc):
    cs = np.arange(n_cmp)[:, None] * CMP_STRIDE
    ss = np.arange(n_slc)[None, :] * SLC_BLOCK
    ov = np.clip(np.minimum(cs + CMP_BLOCK, ss + SLC_BLOCK) - np.maximum(cs, ss), 0, None)
    return (ov / CMP_BLOCK).astype(np.float32)


def gather_blocks(blocks, idx):
    return jax.vmap(jax.vmap(lambda bl, ix: bl[ix]))(blocks, idx)


def nsa_mixer(q, k_cmp, v_cmp, k_slc, v_slc, k_win, v_win, gates, q_norm_g, k_norm_g,
              cmp_pe, cmp_w1, cmp_b1, cmp_w2, cmp_b2):
    B, S, _ = q.shape
    G, R, QB = NSA_KV_HEADS, NSA_GROUP, SPARSE_Q_BLOCK
    scale = HEAD_DIM ** -0.5
    slopes = jnp.asarray(alibi_slopes(NSA_HEADS).reshape(G, R, 1, 1))
    qh = rms_norm(q.reshape(B, S, G, R, HEAD_DIM), q_norm_g).transpose(0, 2, 3, 1, 4)
    kv = lambda t: t.reshape(B, S, G, HEAD_DIM).transpose(0, 2, 1, 3)
    pos = jnp.arange(S)

    kc = rms_norm(compress_blocks(kv(k_cmp), cmp_pe[0], cmp_w1[0], cmp_b1[0], cmp_w2[0], cmp_b2[0]), k_norm_g[0])
    vc = compress_blocks(kv(v_cmp), cmp_pe[1], cmp_w1[1], cmp_b1[1], cmp_w2[1], cmp_b2[1])
    n_cmp = kc.shape[2]
    cmp_end = jnp.arange(n_cmp) * CMP_STRIDE + CMP_BLOCK - 1
    cdist = (pos[:, None] - cmp_end[None, :]).astype(jnp.float32)
    s_cmp = jnp.einsum('bgrtd,bgnd->bgrtn', qh, kc).astype(jnp.float32) * scale - slopes * cdist
    p_cmp = masked_softmax(s_cmp, cdist >= 0)
    o_cmp = jnp.einsum('bgrtn,bgnd->bgrtd', p_cmp.astype(vc.dtype), vc)

    n_slc = S // SLC_BLOCK
    imp = jnp.einsum('bgrtn,nj->bgtj', p_cmp, jnp.asarray(overlap_matrix(n_cmp, n_slc)))
    blk = jnp.arange(n_slc)[None, :]
    cur = (pos // SLC_BLOCK)[:, None]
    valid = blk <= cur
    forced = (blk == 0) | (blk == cur) | (blk == cur - 1)
    sel_score = jnp.where(forced, SEL_BIG, jnp.where(valid, imp, -SEL_BIG))
    n_top = min(SLC_TOPN, n_slc)
    top_val, top_idx = lax.top_k(sel_score, n_top)
    top_ok = top_val > -0.5 * SEL_BIG

    ks_blocks = rms_norm(kv(k_slc), k_norm_g[1]).reshape(B, G, n_slc, SLC_BLOCK, HEAD_DIM)
    vs_blocks = kv(v_slc).reshape(B, G, n_slc, SLC_BLOCK, HEAD_DIM)
    pad = ((0, 0), (0, 0), (WINDOW, 0), (0, 0))
    kw_pad = jnp.pad(rms_norm(kv(k_win), k_norm_g[2]), pad)
    vw_pad = jnp.pad(kv(v_win), pad)
    nqb = S // QB
    q_blocks = jnp.moveaxis(qh.reshape(B, G, R, nqb, QB, HEAD_DIM), 3, 0)
    idx_blocks = jnp.moveaxis(top_idx.reshape(B, G, nqb, QB, n_top), 2, 0)
    ok_blocks = jnp.moveaxis(top_ok.reshape(B, G, nqb, QB, n_top), 2, 0)

    def sparse_block(args):
        i, qb, ib, okb = args
        t = i * QB + jnp.arange(QB)
        kg = gather_blocks(ks_blocks, ib).reshape(B, G, QB, n_top * SLC_BLOCK, HEAD_DIM)
        vg = gather_blocks(vs_blocks, ib).reshape(B, G, QB, n_top * SLC_BLOCK, HEAD_DIM)
        s_pos = (ib[..., None] * SLC_BLOCK + jnp.arange(SLC_BLOCK)).reshape(B, G, QB, n_top * SLC_BLOCK)
        sdist = t[:, None] - s_pos
        ok = jnp.repeat(okb, SLC_BLOCK, axis=-1) & (sdist >= 0)
        s = (jnp.einsum('bgrqd,bgqkd->bgrqk', qb, kg).astype(jnp.float32) * scale
             - slopes * sdist[:, :, None].astype(jnp.float32))
        p = masked_softmax(s, ok[:, :, None])
        o_slc = jnp.einsum('bgrqk,bgqkd->bgrqd', p.astype(vg.dtype), vg)
        kwb = lax.dynamic_slice_in_dim(kw_pad, i * QB, QB + WINDOW, axis=2)
        vwb = lax.dynamic_slice_in_dim(vw_pad, i * QB, QB + WINDOW, axis=2)
        w_pos = i * QB - WINDOW + jnp.arange(QB + WINDOW)
        wd = t[:, None] - w_pos[None, :]
        w_ok = (w_pos[None, :] >= 0) & (wd >= 0) & (wd < WINDOW)
        s = (jnp.einsum('bgrqd,bgkd->bgrqk', qb, kwb).astype(jnp.float32) * scale
             - slopes * wd.astype(jnp.float32))
        p = masked_softmax(s, w_ok)
        o_win = jnp.einsum('bgrqk,bgkd->bgrqd', p.astype(vwb.dtype), vwb)
        return o_slc, o_win

    o_slc, o_win = lax.map(sparse_block, (jnp.arange(nqb), q_blocks, idx_blocks, ok_blocks))
    unblock = lambda t: jnp.moveaxis(t, 0, 3).reshape(B, G, R, S, HEAD_DIM)
    o_slc, o_win = unblock(o_slc), unblock(o_win)
    gt = jax.nn.sigmoid(gates.reshape(B, S, 3, G, R)).transpose(2, 0, 3, 4, 1)[..., None]
    o = gt[0] * o_cmp + gt[1] * o_slc + gt[2] * o_win
    return o.transpose(0, 3, 1, 2, 4).reshape(B, S, NSA_WIDTH).astype(q.dtype)


def grouped_moe(h, router_w, router_bias, w_gate, w_up, w_down):
    B, S, D = h.shape
    T = B * S
    A = T * MOE_TOP_K
    hf = h.reshape(T, D)
    scores = jax.nn.sigmoid((hf @ router_w).astype(jnp.float32))
    biased = scores + router_bias.astype(jnp.float32)
    grp_score = lax.top_k(biased.reshape(T, N_EXPERT_GROUPS, EXPERTS_PER_GROUP), 2)[0].sum(-1)
    best_group = jnp.argmax(grp_score, axis=-1)
    in_group = (jnp.arange(N_EXPERTS) // EXPERTS_PER_GROUP)[None, :] == best_group[:, None]
    _, ids = lax.top_k(jnp.where(in_group, biased, -jnp.inf), MOE_TOP_K)
    gate = jnp.take_along_axis(scores, ids, axis=-1)
    gate = gate / jnp.sum(gate, axis=-1, keepdims=True)
    flat_ids = ids.reshape(A)
    order = jnp.argsort(flat_ids)
    sorted_ids = flat_ids[order]
    tok = order // MOE_TOP_K
    counts = jnp.bincount(flat_ids, length=N_EXPERTS)
    starts = jnp.cumsum(counts) - counts
    padded = (counts + MOE_ROW_BLOCK - 1) // MOE_ROW_BLOCK * MOE_ROW_BLOCK
    pad_ends = jnp.cumsum(padded)
    pad_starts = pad_ends - padded
    dest = pad_starts[sorted_ids] + jnp.arange(A) - starts[sorted_ids]
    n_blocks = -(-(A + N_EXPERTS * (MOE_ROW_BLOCK - 1)) // MOE_ROW_BLOCK)
    rows = n_blocks * MOE_ROW_BLOCK
    xs = jnp.zeros((rows, D), h.dtype).at[dest].set(hf[tok])
    block_expert = jnp.minimum(
        jnp.searchsorted(pad_ends, jnp.arange(n_blocks) * MOE_ROW_BLOCK, side='right'), N_EXPERTS - 1)

    def expert_block(args):
        xb, e = args
        return (jax.nn.silu(xb @ w_gate[e]) * (xb @ w_up[e])) @ w_down[e]

    ys = lax.map(expert_block, (xs.reshape(n_blocks, MOE_ROW_BLOCK, D), block_expert)).reshape(rows, D)
    y = ys[dest] * gate.reshape(A)[order][:, None].astype(ys.dtype)
    return jnp.zeros((T, D), ys.dtype).at[tok].add(y).reshape(B, S, D)


def setup_inputs(seed: int = 0) -> dict:
    key = jax.random.key(seed)
    ks = iter(jax.random.split(key, 32))
    nrm = lambda shape, s: jax.random.normal(next(ks), shape, jnp.float32) * s
    L = DEPTH
    x = nrm((BATCH, SEQ, D_MODEL), 1.0)
    c = nrm((BATCH, D_MODEL), 1.0)
    ada_w = nrm((L, D_MODEL, 6 * D_MODEL), 0.5 * D_MODEL ** -0.5)
    ada_b = nrm((L, 6 * D_MODEL), 0.02)
    norm1_g = 1.0 + nrm((L, D_MODEL), 0.05)
    norm2_g = 1.0 + nrm((L, D_MODEL), 0.05)
    w_in = nrm((L, D_MODEL, IN_COLS), D_MODEL ** -0.5)
    gdn_conv_w = nrm((L, GDN_CONV, 3 * GDN_WIDTH), GDN_CONV ** -0.5)
    gdn_a_log = jnp.log(jax.random.uniform(next(ks), (L, GDN_HEADS), jnp.float32, 1.0, 16.0))
    dt = jnp.exp(jax.random.uniform(next(ks), (L, GDN_HEADS), jnp.float32, math.log(1e-3), math.log(1e-1)))
    gdn_dt_bias = dt + jnp.log(-jnp.expm1(-dt))
    gdn_norm_g = 1.0 + nrm((L, HEAD_DIM), 0.05)
    nsa_q_norm_g = 1.0 + nrm((L, HEAD_DIM), 0.05)
    nsa_k_norm_g = 1.0 + nrm((L, 3, HEAD_DIM), 0.05)
    cmp_pe = nrm((L, 2, CMP_BLOCK, HEAD_DIM), 0.1)
    cmp_w1 = nrm((L, 2, CMP_BLOCK * HEAD_DIM, CMP_HIDDEN), (CMP_BLOCK * HEAD_DIM) ** -0.5)
    cmp_b1 = nrm((L, 2, CMP_HIDDEN), 0.02)
    cmp_w2 = nrm((L, 2, CMP_HIDDEN, HEAD_DIM), CMP_HIDDEN ** -0.5)
    cmp_b2 = nrm((L, 2, HEAD_DIM), 0.02)
    w_out = nrm((L, MIX_WIDTH, D_MODEL), MIX_WIDTH ** -0.5)
    router_w = nrm((D_MODEL, N_EXPERTS), D_MODEL ** -0.5)
    router_bias = nrm((N_EXPERTS,), 0.01)
    exp_w_gate = nrm((L, N_EXPERTS, D_MODEL, EXPERT_FF), D_MODEL ** -0.5)
    exp_w_up = nrm((L, N_EXPERTS, D_MODEL, EXPERT_FF), D_MODEL ** -0.5)
    exp_w_down = nrm((L, N_EXPERTS, EXPERT_FF, D_MODEL), EXPERT_FF ** -0.5)
    return {'x': x, 'c': c, 'ada_w': ada_w, 'ada_b': ada_b, 'norm1_g': norm1_g, 'norm2_g': norm2_g,
            'w_in': w_in, 'gdn_conv_w': gdn_conv_w, 'gdn_a_log': gdn_a_log, 'gdn_dt_bias': gdn_dt_bias,
            'gdn_norm_g': gdn_norm_g, 'nsa_q_norm_g': nsa_q_norm_g, 'nsa_k_norm_g': nsa_k_norm_g,
            'cmp_pe': cmp_pe, 'cmp_w1': cmp_w1, 'cmp_b1': cmp_b1, 'cmp_w2': cmp_w2, 'cmp_b2': cmp_b2,
            'w_out': w_out, 'router_w': router_w, 'router_bias': router_bias,
            'exp_w_gate': exp_w_gate, 'exp_w_up': exp_w_up, 'exp_w_down': exp_w_down}


def reference(x, c, ada_w, ada_b, norm1_g, norm2_g, w_in, gdn_conv_w, gdn_a_log, gdn_dt_bias,
              gdn_norm_g, nsa_q_norm_g, nsa_k_norm_g, cmp_pe, cmp_w1, cmp_b1, cmp_w2, cmp_b2,
              w_out, router_w, router_bias, exp_w_gate, exp_w_up, exp_w_down):
    cond = jax.nn.silu(c)
    split_points = np.cumsum(IN_SPLITS)[:-1].tolist()
    for layer in range(DEPTH):
        mod = (cond @ ada_w[layer] + ada_b[layer])[:, None, :]
        sh1, sc1, g1, sh2, sc2, g2 = jnp.split(mod, 6, axis=-1)
        h = rms_norm(x, norm1_g[layer]) * (1.0 + sc1) + sh1
        (gq, gk, gv, gz, ga, gb, nq, nkc, nvc, nks, nvs, nkw, nvw, ngate) = jnp.split(
            h @ w_in[layer], split_points, axis=-1)
        y_gdn = gdn_mixer(gq, gk, gv, gz, ga, gb, gdn_conv_w[layer], gdn_a_log[layer],
                          gdn_dt_bias[layer], gdn_norm_g[layer])
        y_nsa = nsa_mixer(nq, nkc, nvc, nks, nvs, nkw, nvw, ngate, nsa_q_norm_g[layer],
                          nsa_k_norm_g[layer], cmp_pe[layer], cmp_w1[layer], cmp_b1[layer],
                          cmp_w2[layer], cmp_b2[layer])
        x = x + g1 * (jnp.concatenate([y_gdn, y_nsa], axis=-1) @ w_out[layer])
        h = rms_norm(x, norm2_g[layer]) * (1.0 + sc2) + sh2
        x = x + g2 * grouped_moe(h, router_w, router_bias, exp_w_gate[layer], exp_w_up[layer],
                                 exp_w_down[layer])
    return x
```

```python
import os
from contextlib import ExitStack
import numpy as np
import ml_dtypes
import concourse.bass as bass
import concourse.mybir as mybir
from concourse.bass_utils import run_bass_kernel_spmd

F32 = mybir.dt.float32
BF16 = mybir.dt.bfloat16
AF = mybir.ActivationFunctionType
ALU = mybir.AluOpType
AX = mybir.AxisListType

D = 1024
S = 2048
NB = 2
NT = S // 128
DEPTH = 2
HD = 64
GW = 512
INC = 3368
NEXP = 16
FF = 512
EPS = 1e-6
NEG = -30000.0
C_Q, C_K, C_V, C_Z, C_A, C_B = 0, 512, 1024, 1536, 2048, 2056
C_NQ = 2064
C_KV = 2576
C_NG = 3344
PADR = 3


class Prog:
    ENG = ("pe", "act", "dve", "pool", "sp")

    def __init__(self, nc, es):
        self.nc = nc
        self.es = es
        self.eng = {"pe": nc.tensor, "act": nc.scalar, "dve": nc.vector, "pool": nc.gpsimd, "sp": nc.sync}
        self.esem = {e: es.enter_context(nc.semaphore("es_" + e)) for e in self.ENG}
        self.ecount = {e: 0 for e in self.ENG}
        self.eidx = {e: 0 for e in self.ENG}
        self.dsem = {}
        self.dcount = {}
        self.ops = []
        self.res = {}
        self.waited = {e: {} for e in self.ENG}
        self.nins = 0

    def _r(self, name):
        r = self.res.get(name)
        if r is None:
            r = {"w": {}, "r": {}}
            self.res[name] = r
        return r

    def _deps(self, me_eng, reads, writes, is_dma):
        deps = {}

        def add(p, st):
            if isinstance(p, tuple):
                st = self.dcount[p]
            if deps.get(p, -1) < st:
                deps[p] = st
        for n in reads:
            for p, st in self._r(n)["w"].items():
                add(p, st)
        for n in writes:
            rr = self._r(n)
            for p, st in rr["w"].items():
                if is_dma and isinstance(p, tuple):
                    continue
                add(p, st)
            for p, st in rr["r"].items():
                add(p, st)
        return deps

    def op(self, eng, fn, r=(), w=()):
        w = list(w) + [n for n in r if n.startswith("ps")]
        r = [n for n in r if not n.startswith("ps")]
        deps = self._deps(eng, r, w, False)
        idx = self.eidx[eng]
        self.eidx[eng] += 1
        if eng in deps:
            if eng == "pe" or idx - deps[eng] > 3:
                del deps[eng]
        o = {"eng": eng, "fn": fn, "deps": deps, "idx": idx, "inc": False, "dkey": None}
        self.ops.append(o)
        for n in r:
            self._r(n)["r"][eng] = idx
        for n in w:
            rr = self._r(n)
            rr["w"] = {eng: idx}
            rr["r"] = {}
        return o

    def dma(self, q, out, in_, r=(), w=(), key=None):
        assert key is not None
        deps = self._deps(q, r, w, True)
        deps.pop(q, None) if False else None
        idx = self.eidx[q]
        self.eidx[q] += 1
        if q in deps:
            if idx - deps[q] > 3:
                del deps[q]
        k = ("d", key)
        self.dcount[k] = self.dcount.get(k, 0) + 1
        st = self.dcount[k]
        o = {"eng": q, "fn": None, "dma": (out, in_), "deps": deps, "idx": idx, "inc": False, "dkey": k}
        self.ops.append(o)
        for n in r:
            self._r(n)["r"][k] = st
        for n in w:
            rr = self._r(n)
            rr["w"] = {p: s for p, s in rr["w"].items() if isinstance(p, tuple)}
            rr["w"][k] = st
            rr["r"] = {}
        return o

    def _sem_for(self, k):
        s = self.dsem.get(k)
        if s is None:
            s = self.es.enter_context(self.nc.semaphore("ds_%d" % len(self.dsem)))
            self.dsem[k] = s
        return s

    def flush(self, barrier=True):
        ops = self.ops
        byeng = {e: {} for e in self.ENG}
        for o in ops:
            if o["dkey"] is None:
                byeng[o["eng"]][o["idx"]] = o
        for o in ops:
            for p, st in o["deps"].items():
                if not isinstance(p, tuple):
                    t = byeng[p].get(st)
                    if t is not None:
                        t["inc"] = True
        last = {}
        for o in ops:
            if o["dkey"] is None:
                last[o["eng"]] = o
        if barrier:
            for o in last.values():
                o["inc"] = True
        cnt_of = {e: {} for e in self.ENG}
        run = dict(self.ecount)
        for o in ops:
            if o["dkey"] is None and o["inc"]:
                run[o["eng"]] += 1
                cnt_of[o["eng"]][o["idx"]] = run[o["eng"]]
        for o in ops:
            e = o["eng"]
            eo = self.eng[e]
            for p, st in o["deps"].items():
                if isinstance(p, tuple):
                    sem = self._sem_for(p)
                    val = 16 * st
                else:
                    if st not in cnt_of[p]:
                        continue
                    sem = self.esem[p]
                    val = cnt_of[p][st]
                if self.waited[e].get(p, 0) >= val:
                    continue
                self.waited[e][p] = val
                eo.wait_ge(sem, val)
                self.nins += 1
            if o["dkey"] is not None:
                out, in_ = o["dma"]
                eo.dma_start(out=out, in_=in_).then_inc(self._sem_for(o["dkey"]), 16)
            else:
                ins = o["fn"]()
                if o["inc"]:
                    ins.then_inc(self.esem[e], 1)
            self.nins += 1
        self.ecount = run
        if barrier:
            for e in self.ENG:
                eo = self.eng[e]
                for p in self.ENG:
                    if p != e and self.ecount[p] > self.waited[e].get(p, 0):
                        eo.wait_ge(self.esem[p], self.ecount[p])
                        self.waited[e][p] = self.ecount[p]
                for k, c in self.dcount.items():
                    if 16 * c > self.waited[e].get(k, 0):
                        eo.wait_ge(self._sem_for(k), 16 * c)
                        self.waited[e][k] = 16 * c
            self.res = {}
        self.ops = []


def make_consts():
    c = {}
    c["ident"] = np.eye(128, dtype=np.float32)
    m = np.arange(128)
    c["cU"] = (m[:, None] <= m[None, :]).astype(np.float32)
    c["cOnes"] = np.ones((128, 128), np.float32)
    c["cBm"] = (m[:, None] > m[None, :]).astype(np.float32)
    m2 = np.zeros((128, 2, 128), np.float32)
    m2[:, 0, :] = -(m[None, :] > m[:, None]).astype(np.float32)
    m2[:, 1, :] = (m[None, :] >= m[:, None]).astype(np.float32)
    c["cM2"] = m2
    slopes = (2.0 ** (-np.arange(1, 9))).astype(np.float32)
    qa_sw = np.zeros((3, 2, 4, 128), np.float32)
    qa_c = np.zeros((3, 2, 4, 128), np.float32)
    for g in range(2):
        for r in range(4):
            sp_ = slopes[g * 4 + r]
            qa_sw[0, g, r] = sp_
            qa_sw[1, g, r] = 128 * sp_
            qa_c[0, g, r] = 16 * sp_
            qa_c[1, g, r] = 31 * sp_
            qa_c[2, g, r] = -128 * sp_
    c["nQAc"] = qa_c.reshape(3, 2, 512)
    ka_sw = np.zeros((3, 16, 128), np.float32)
    ka_c = np.zeros((3, 16, 128), np.float32)
    for dl in range(16):
        ka_sw[0, dl] = m
        ka_sw[1, dl] = -dl
        ka_c[0, dl] = m
        ka_c[1, dl] = 1
        ka_c[2, dl] = dl
    c["nKAc"] = ka_c
    tpos = (np.arange(16)[:, None] * 128 + m[None, :])[None]
    cend = (16 * m + 31)[:, None, None]
    negc = np.where((tpos >= cend) & (m[:, None, None] < 127), 0.0, NEG).astype(np.float32)
    c["nNEGc"] = negc
    c["nCMd"] = np.where(m[:, None] <= m[None, :], 0.0, NEG).astype(np.float32)
    c["nCMw4"] = np.where(m[:, None] > m[None, :], 0.0, NEG).astype(np.float32)
    e = np.zeros((32, 16, 128), np.float32)
    for kt in range(16):
        for p in range(128):
            e[2 * kt + p // 64, kt, p] = 1.0
    n_cmp = 127
    cs = np.arange(n_cmp)[:, None] * 16
    ss_ = np.arange(32)[None, :] * 64
    ov = np.clip(np.minimum(cs + 32, ss_ + 64) - np.maximum(cs, ss_), 0, None) / 32.0
    ovp = np.zeros((128, 32), np.float32)
    ovp[:127] = ov
    c["nOV"] = ovp
    pos = np.arange(2048)
    blk = np.arange(32)[None, :]
    cur = (pos // 64)[:, None]
    valid = blk <= cur
    forced = (blk == 0) | (blk == cur) | (blk == cur - 1)
    cv = np.where(forced | ~valid, 0.0, 1.0).astype(np.float32)
    cb = np.where(forced, 1e9, np.where(valid, 0.0, -1e9)).astype(np.float32)
    tpos1 = np.arange(2048)
    qrows = np.zeros((3, 8, 2048), np.float32)
    for h in range(8):
        qrows[0, h] = slopes[h]
        qrows[1, h] = 128 * slopes[h]
        qrows[2, h] = -128.0 * (tpos1 // 128) * slopes[h]
    krows = np.zeros((35, 2048), np.float32)
    krows[0] = tpos1 % 128
    krows[1] = tpos1 // 128
    krows[2] = 1.0
    for j in range(32):
        krows[3 + j] = (tpos1 // 64 == j)
    c["nQrows"] = qrows.astype(ml_dtypes.bfloat16)
    c["nKrows"] = krows.astype(ml_dtypes.bfloat16)
    c["nCV"] = cv.reshape(16, 128, 32).transpose(1, 0, 2).copy()
    c["nCB"] = cb.reshape(16, 128, 32).transpose(1, 0, 2).copy()
    return c


CONST_SHAPES = {k: (v.shape, v.dtype) for k, v in make_consts().items()}


def bcast_mid(ap, n):
    return ap.unsqueeze(1).to_broadcast([ap.shape[0], n, ap.shape[1]])


def bcast_last(ap, n):
    return ap.unsqueeze(2).to_broadcast([ap.shape[0], ap.shape[1], n])


class RecQ:
    def __init__(self):
        self.stages, self.cur = {}, None

    def stage(self, name):
        self.cur = self.stages.setdefault(name, [])

    def op(self, eng, fn, r=(), w=()):
        self.cur.append(("op", eng, fn, list(r), list(w), None))

    def dma(self, q_, out, in_, r=(), w=(), key=None):
        self.cur.append(("dma", q_, (out, in_), list(r), list(w), key))


def emit_ops(P, ops):
    for (kind, eng, fn, r, w, key) in ops:
        if kind == "op":
            P.op(eng, fn, r=r, w=w)
        else:
            P.dma(eng, fn[0], fn[1], r=r, w=w, key=key)


def interleave_ops(A, B):
    if not B:
        return list(A)
    if not A:
        return list(B)
    out, j = [], 0
    for i, a in enumerate(A):
        out.append(a)
        want = (i + 1) * len(B) // len(A)
        while j < want:
            out.append(B[j])
            j += 1
    out.extend(B[j:])
    return out


def phase_gdn(nc, P, T, PS, I, l, PROJ, YMIX, C, debug, dbg):
    identb, cU, cOnes, cBm, cM2, epsc = C["identb"], C["cU"], C["cOnes"], C["cBm"], C["cM2"], C["epsc"]
    V, A_, G = nc.vector, nc.scalar, nc.gpsimd
    NTL = (debug or {}).get("_gdn_tiles", NT)
    NBL = (debug or {}).get("_gdn_nb", NB)
    with ExitStack() as es:
        wc = T(es, "wc", [128, 4, 1536], F32)
        dtb = T(es, "dtb", [128, 8], F32)
        nea = T(es, "nea", [128, 8], F32)
        gng = T(es, "gng", [128, 64], F32)
        XS = [T(es, "XS%d" % i, [128, 4, 1536], F32) for i in range(2)]
        ZAB = [T(es, "ZAB%d" % i, [128, 528], F32) for i in range(4)]
        QKV = T(es, "QKV", [128, 1536], F32)
        junk = T(es, "gjunk", [128, 1024], F32)
        SM = [T(es, "gsm%d" % i, [128, 96], F32) for i in range(2)]
        qn32 = T(es, "qn32", [128, 8, 64], F32)
        kn32 = T(es, "kn32", [128, 8, 64], F32)
        OPBs = [T(es, "OPB%d" % i, [128, 7, 8, 64], BF16) for i in range(2)]
        TTs = [T(es, "TT%d" % i, [128, 4, 4, 128], BF16) for i in range(2)]
        AH = T(es, "AH", [128, 8, 128], F32)
        DT_ = T(es, "DT", [128, 8, 128], F32)
        DM = T(es, "DM", [128, 8, 2, 128], F32)
        N32 = [T(es, "N32%d" % i, [128, 128], F32) for i in range(8)]
        N1T = [T(es, "N1T%d" % i, [128, 128], F32) for i in range(8)]
        QK = [T(es, "QK%d" % i, [128, 128], BF16) for i in range(8)]
        NK = [[T(es, "NK%d_%d" % (i, j), [128, 128], F32) for j in range(2)] for i in range(8)]
        NKT = [[T(es, "NKT%d_%d" % (i, j), [128, 128], F32) for j in range(2)] for i in range(8)]
        PK = [[T(es, "PK%d_%d" % (i, j), [128, 128], F32) for j in range(2)] for i in range(8)]
        TTB = [T(es, "TTB%d" % i, [128, 128], BF16) for i in range(8)]
        UH = [T(es, "UH%d" % i, [128, 64], F32) for i in range(8)]
        WT = [T(es, "WT%d" % i, [128, 128], BF16) for i in range(8)]
        VN = [T(es, "VN%d" % i, [128, 64], BF16) for i in range(8)]
        O = T(es, "O", [128, 8, 64], F32)
        YG = [T(es, "YG%d" % i, [128, 512], F32) for i in range(2)]
        SZ = T(es, "SZ", [128, 512], F32)
        SS = T(es, "SS", [128, 4, 64], F32)
        SB = T(es, "SB", [128, 4, 64], BF16)
        SSQ, RN, BETA, SP, GRAW, GG, EG, DG, EK, OSS, ORS, RQ8 = (slice(0, 16), slice(16, 32), slice(32, 40), slice(40, 48), slice(48, 56),
                                                                 slice(56, 72), slice(72, 88), None, slice(88, 96), None, None, None)
        SM2 = [T(es, "gsm2%d" % i, [128, 32], F32) for i in range(2)]
        P.dma("sp", wc[:].rearrange("p k c -> p (k c)"), I["gdn_conv_w"][l].partition_broadcast(128), w=["wc"], key="gc")
        P.dma("sp", dtb[:], I["gdn_dt_bias"][l].partition_broadcast(128), w=["dtb"], key="gc")
        P.dma("sp", nea[:], I["gdn_a_log"][l].partition_broadcast(128), w=["nea"], key="gc")
        P.dma("sp", gng[:], I["gdn_norm_g"][l].partition_broadcast(128), w=["gng"], key="gc")
        P.op("act", lambda: A_.activation(out=nea[:], in_=nea[:], func=AF.Exp), r=["nea"], w=["nea"])
        P.op("dve", lambda: V.tensor_scalar(out=nea[:], in0=nea[:], scalar1=-1.0, scalar2=None, op0=ALU.mult), r=["nea"], w=["nea"])
        DBL = set(["ssq", "rn", "rq8", "beta", "sp", "graw", "gg", "eg", "dg", "ek", "oss", "ors", "TT01", "TT23"] + ["opb%d" % i for i in range(7)])

        class Rec:
            def __init__(self, q):
                self.q, self.stages, self.cur = q, {}, None

            def stage(self, name):
                self.cur = self.stages.setdefault(name, [])

            def _nm(self, names):
                return [(n + "_q%d" % self.q) if n in DBL else n for n in names]

            def op(self, eng, fn, r=(), w=()):
                self.cur.append(("op", eng, fn, self._nm(r), self._nm(w), None))

            def dma(self, q_, out, in_, r=(), w=(), key=None):
                self.cur.append(("dma", q_, (out, in_), self._nm(r), self._nm(w), key))

        def emit(ops):
            for (kind, eng, fn, r, w, key) in ops:
                if kind == "op":
                    P.op(eng, fn, r=r, w=w)
                else:
                    P.dma(eng, fn[0], fn[1], r=r, w=w, key=key)

        def tile_body(b, tt, sl, Q):
            sm, sm2, OPB, TT = SM[sl], SM2[sl], OPBs[sl], TTs[sl]
            t0 = tt * 128
            zs = tt % 4
            X, Z = XS[sl], ZAB[zs]
            Q.stage("L")
            for k in range(4):
                Q.dma("sp", X[:, k, :], PROJ[b, t0 + k:t0 + k + 128, 0:1536], r=["PROJ"], w=["XS%d" % sl], key="XS%d" % sl)
            Q.dma("sp", Z[:], PROJ[b, PADR + t0:PADR + t0 + 128, 1536:2064], r=["PROJ"], w=["ZAB%d" % zs], key="ZAB%d" % zs)
            Q.stage("P1")
            Q.op("pool", lambda X=X: G.tensor_tensor(out=X[:, 0:2, :], in0=X[:, 0:2, :], in1=wc[:, 0:2, :], op=ALU.mult), r=["XS%d" % sl, "wc"], w=["XS%da" % sl])
            Q.op("dve", lambda X=X: V.tensor_tensor(out=X[:, 2:4, :], in0=X[:, 2:4, :], in1=wc[:, 2:4, :], op=ALU.mult), r=["XS%d" % sl, "wc"], w=["XS%db" % sl])
            Q.op("dve", lambda X=X: V.tensor_tensor(out=X[:, 0:2, :], in0=X[:, 0:2, :], in1=X[:, 2:4, :], op=ALU.add), r=["XS%da" % sl, "XS%db" % sl], w=["XS%d" % sl, "XS%da" % sl, "XS%db" % sl])
            Q.op("dve", lambda X=X: V.tensor_tensor(out=X[:, 0, :], in0=X[:, 0, :], in1=X[:, 1, :], op=ALU.add), r=["XS%d" % sl], w=["XS%d" % sl])
            Q.op("act", lambda X=X: A_.activation(out=QKV[:], in_=X[:, 0, :], func=AF.Silu), r=["XS%d" % sl], w=["QKV"])
            Q.op("dve", lambda: V.tensor_tensor(out=junk[:], in0=QKV[:, 0:1024], in1=QKV[:, 0:1024], op=ALU.mult), r=["QKV"], w=["gjunk"])
            Q.op("dve", lambda: V.tensor_reduce(out=sm[:, SSQ], in_=junk[:].rearrange("p (g d) -> p g d", d=64), axis=AX.X, op=ALU.add), r=["gjunk"], w=["ssq"])
            Q.op("act", lambda: A_.activation(out=sm[:, RN], in_=sm[:, SSQ], func=AF.Sqrt, bias=epsc[:, 0:1]), r=["ssq", "epsc"], w=["rn"])
            Q.op("dve", lambda: V.reciprocal(out=sm[:, RN], in_=sm[:, RN]), r=["rn"], w=["rn"])
            Q.op("dve", lambda: V.tensor_scalar(out=sm2[:, 24:32], in0=sm[:, 16:24], scalar1=0.125, scalar2=None, op0=ALU.mult), r=["rn"], w=["rq8"])
            Q.op("act", lambda Z=Z: A_.activation(out=sm[:, BETA], in_=Z[:, 520:528], func=AF.Sigmoid), r=["ZAB%d" % zs], w=["beta"])
            Q.op("dve", lambda Z=Z: V.tensor_tensor(out=sm[:, SP], in0=Z[:, 512:520], in1=dtb[:], op=ALU.add), r=["ZAB%d" % zs, "dtb"], w=["sp"])
            Q.op("act", lambda: A_.activation(out=sm[:, SP], in_=sm[:, SP], func=AF.Exp), r=["sp"], w=["sp"])
            Q.op("act", lambda: A_.activation(out=sm[:, SP], in_=sm[:, SP], func=AF.Ln, bias=epsc[:, 1:2]), r=["sp", "epsc"], w=["sp"])
            Q.op("dve", lambda: V.tensor_tensor(out=sm[:, GRAW], in0=sm[:, SP], in1=nea[:], op=ALU.mult), r=["sp", "nea"], w=["graw"])
            Q.stage("P2")
            def mmg():
                nc.tensor.matmul(PS[6][:, 0:8], lhsT=cU[:], rhs=sm[:, GRAW], start=True, stop=True)
                return nc.tensor.matmul(PS[6][:, 8:16], lhsT=cOnes[:], rhs=sm[:, GRAW], start=True, stop=True)
            Q.op("pe", mmg, r=["graw", "cU", "cOnes"], w=["ps6"])
            Q.op("dve", lambda: V.tensor_copy(out=sm[:, GG], in_=PS[6][:, 0:16]), r=["ps6"], w=["gg"])
            Q.op("act", lambda: A_.activation(out=sm[:, EG], in_=sm[:, GG], func=AF.Exp), r=["gg"], w=["eg"])
            Q.op("dve", lambda: V.tensor_tensor(out=sm2[:, 0:8], in0=sm[:, 64:72], in1=sm[:, 56:64], op=ALU.subtract), r=["gg"], w=["dg"])
            Q.op("act", lambda: A_.activation(out=sm[:, EK], in_=sm2[:, 0:8], func=AF.Exp), r=["dg"], w=["ek"])
            Q3 = QKV[:, 0:512].rearrange("p (h d) -> p h d", d=64)
            K3 = QKV[:, 512:1024].rearrange("p (h d) -> p h d", d=64)
            V3 = QKV[:, 1024:1536].rearrange("p (h d) -> p h d", d=64)
            bl = lambda sl_: bcast_last(sl_, 64)
            Q.op("dve", lambda: V.tensor_tensor(out=qn32[:], in0=Q3, in1=bl(sm2[:, 24:32]), op=ALU.mult), r=["QKV", "rq8"], w=["qn32"])
            Q.op("pool", lambda: G.tensor_tensor(out=kn32[:], in0=K3, in1=bl(sm[:, 24:32]), op=ALU.mult), r=["QKV", "rn"], w=["kn32"])
            Q.op("act", lambda: A_.copy(out=OPB[:, 2], in_=qn32[:]), r=["qn32"], w=["opb2"])
            Q.op("act", lambda: A_.copy(out=OPB[:, 0], in_=kn32[:]), r=["kn32"], w=["opb0"])
            Q.op("dve", lambda: V.tensor_tensor(out=OPB[:, 3], in0=qn32[:], in1=bl(sm[:, 72:80]), op=ALU.mult), r=["qn32", "eg"], w=["opb3"])
            Q.op("pool", lambda: G.tensor_tensor(out=kn32[:], in0=kn32[:], in1=bl(sm[:, 88:96]), op=ALU.mult) if False else G.tensor_tensor(out=OPB[:, 5], in0=kn32[:], in1=bl(sm[:, 88:96]), op=ALU.mult),
                 r=["kn32", "ek"], w=["opb5"])
            Q.op("dve", lambda: V.tensor_tensor(out=OPB[:, 6], in0=V3, in1=bl(sm[:, BETA]), op=ALU.mult), r=["QKV", "beta"], w=["opb6"])
            Q.op("pool", lambda: G.tensor_tensor(out=qn32[:], in0=kn32[:], in1=bl(sm[:, BETA]), op=ALU.mult), r=["kn32", "beta", "opb2", "opb3"], w=["qn32"])
            Q.op("act", lambda: A_.copy(out=OPB[:, 1], in_=qn32[:]), r=["qn32"], w=["opb1"])
            Q.op("dve", lambda: V.tensor_tensor(out=OPB[:, 4], in0=qn32[:], in1=bl(sm[:, 72:80]), op=ALU.mult), r=["qn32", "eg"], w=["opb4"])
            def trs():
                ins = None
                for p in range(4):
                    bank = PS[6 + p // 2][:].bitcast(BF16)
                    for kd in range(4):
                        col = ((p % 2) * 4 + kd) * 128
                        ins = nc.tensor.transpose(out=bank[:, col:col + 128],
                                                  in_=OPB[:, kd, 2 * p:2 * p + 2, :].rearrange("p a d -> p (a d)"), identity=identb[:])
                return ins
            Q.op("pe", trs, r=["opb0", "opb1", "opb2", "opb3", "identb"], w=["ps6", "ps7"])
            Q.op("act", lambda: A_.copy(out=TT[:, 0:2].rearrange("p a k t -> p (a k t)"), in_=PS[6][:].bitcast(BF16)), r=["ps6"], w=["TT01"])
            Q.op("dve", lambda: V.tensor_copy(out=TT[:, 2:4].rearrange("p a k t -> p (a k t)"), in_=PS[7][:].bitcast(BF16)), r=["ps7"], w=["TT23"])
            Q.op("dve", lambda: V.tensor_tensor(out=AH[:], in0=bcast_mid(cU[:], 8), in1=bcast_last(sm[:, GRAW], 128), op=ALU.mult), r=["cU", "graw"], w=["AH"])

            def mmG():
                nc.tensor.matmul(PS[6][:], lhsT=cBm[:], rhs=AH[:, 0:4, :].rearrange("p h i -> p (h i)"), start=True, stop=True)
                return nc.tensor.matmul(PS[7][:], lhsT=cBm[:], rhs=AH[:, 4:8, :].rearrange("p h i -> p (h i)"), start=True, stop=True)
            Q.op("pe", mmG, r=["AH", "cBm"], w=["ps6", "ps7"])
            Q.op("act", lambda: A_.activation(out=DT_[:, 0:4].rearrange("p h i -> p (h i)"), in_=PS[6][:], func=AF.Exp), r=["ps6"], w=["DT0"])
            Q.op("act", lambda: A_.activation(out=DT_[:, 4:8].rearrange("p h i -> p (h i)"), in_=PS[7][:], func=AF.Exp), r=["ps7"], w=["DT1"])
            Q.op("dve", lambda: V.tensor_tensor(out=DM[:, :, 0, :], in0=DT_[:], in1=bcast_mid(cM2[:, 0, :], 8), op=ALU.mult), r=["DT0", "DT1", "cM2"], w=["DM0"])
            Q.op("pool", lambda: G.tensor_tensor(out=DM[:, :, 1, :], in0=DT_[:], in1=bcast_mid(cM2[:, 1, :], 8), op=ALU.mult), r=["DT0", "DT1", "cM2"], w=["DM1"])
            ident = C["ident"]
            HS = list(range(8))
            hp_ = lambda h: (h % 2) * 64
            RI = lambda h: (h % 2) + 2 * (h // 4)
            RB = lambda h: PS[RI(h)]
            RC = lambda h: ((h // 2) % 2) * 256
            RN_ = lambda h: "ps%d" % RI(h)
            PB = lambda h: PS[4 + h % 2]
            PC = lambda h: (h // 2) * 128
            PN = lambda h: "ps%d" % (4 + h % 2)
            ER = lambda h: "act" if h % 2 == 0 else "dve"
            EP = lambda h: "dve" if h % 2 == 0 else "act"

            def cp(eng, out, in_, r, w):
                if eng == "act":
                    Q.op("act", lambda: A_.copy(out=out, in_=in_), r=r, w=w)
                else:
                    Q.op("dve", lambda: V.tensor_copy(out=out, in_=in_), r=r, w=w)
            Q.stage("S2")
            if (debug or {}).get("_gs", 9) < 2:
                HS = []
            for h in HS:
                Q.op("pe", lambda h=h: nc.tensor.matmul(
                    RB(h)[:, RC(h):RC(h) + 256], lhsT=TT[hp_(h):hp_(h) + 64, h // 2, 0, :], rhs=TT[hp_(h):hp_(h) + 64, h // 2, 1:3, :].rearrange("p k t -> p (k t)"),
                    start=True, stop=True), r=["TT01", "TT23"], w=[RN_(h)])
            for h in HS:
                Q.op("dve", lambda h=h: V.tensor_tensor(out=N32[h][:], in0=RB(h)[:, RC(h):RC(h) + 128], in1=DM[:, h, 0, :], op=ALU.mult),
                     r=[RN_(h), "DM0"], w=["N32_%d" % h])
                Q.op("dve", lambda h=h: V.tensor_tensor(out=QK[h][:], in0=RB(h)[:, RC(h) + 128:RC(h) + 256], in1=DM[:, h, 1, :], op=ALU.mult),
                     r=[RN_(h), "DM1"], w=["QK_%d" % h])
            for h in (HS if not os.environ.get("K2") else []):
                Q.op("pe", lambda h=h: nc.tensor.transpose(out=PB(h)[:, PC(h):PC(h) + 128], in_=N32[h][:], identity=ident[:]),
                     r=["N32_%d" % h, "ident"], w=[PN(h)])
            for h in HS:
                if os.environ.get("K2"):
                    continue
                cp(EP(h), N1T[h][:], PB(h)[:, PC(h):PC(h) + 128], [PN(h)], ["N1T_%d" % h])
                Q.op("pool", lambda h=h: G.tensor_tensor(out=PK[h][0][:], in0=N32[h][:], in1=ident[:], op=ALU.add),
                     r=["N32_%d" % h, "ident"], w=["PK%d_0" % h])
            for h in (HS if not os.environ.get("K1") else []):
                Q.op("pe", lambda h=h: nc.tensor.matmul(PB(h)[:, PC(h):PC(h) + 128], lhsT=ident[:], rhs=PK[h][0][:], start=(h < 2), stop=True, skip_group_check=True),
                     r=["PK%d_0" % h, "ident"], w=[PN(h)])
            GS = (debug or {}).get("_gs", 9)
            def emitP(k):
                for h in HS:
                    Q.op("pe", lambda h=h, k=k: nc.tensor.matmul(PB(h)[:, PC(h):PC(h) + 128], lhsT=NKT[h][k % 2][:], rhs=PK[h][(k - 1) % 2][:],
                                                              start=False, stop=True, skip_group_check=True),
                         r=["NKT%d_%d" % (h, k % 2), "PK%d_%d" % (h, (k - 1) % 2)], w=[PN(h)])
                for h in HS:
                    dst = TTB[h] if k == 6 else PK[h][k % 2]
                    dn = ("TTB%d" % h) if k == 6 else ("PK%d_%d" % (h, k % 2))
                    cp(EP(h), dst[:], PB(h)[:, PC(h):PC(h) + 128], [PN(h)], [dn])

            for k in range(1, 7 if GS >= 3 else 1):
                Q.stage("S3_%d" % k)
                last = (k == 6)
                cur = {}
                for h in HS:
                    if k == 1:
                        cur[h] = (N32[h][:], N1T[h][:], ["N32_%d" % h, "N1T_%d" % h])
                    else:
                        cur[h] = (NK[h][(k - 1) % 2][:], NKT[h][(k - 1) % 2][:], ["NK%d_%d" % (h, (k - 1) % 2), "NKT%d_%d" % (h, (k - 1) % 2)])
                if not last:
                    for h in HS:
                        cN, cNT, rn_ = cur[h]
                        Q.op("pe", lambda h=h, cN=cN, cNT=cNT: nc.tensor.matmul(RB(h)[:, RC(h):RC(h) + 128], lhsT=cNT, rhs=cN, start=True, stop=True), r=rn_, w=[RN_(h)])
                    for h in HS:
                        cp(ER(h), NK[h][k % 2][:], RB(h)[:, RC(h):RC(h) + 128], [RN_(h)], ["NK%d_%d" % (h, k % 2)])
                    for h in HS:
                        Q.op("pe", lambda h=h, k=k: nc.tensor.transpose(out=RB(h)[:, RC(h) + 128:RC(h) + 256], in_=NK[h][k % 2][:], identity=ident[:]),
                             r=["NK%d_%d" % (h, k % 2), "ident"], w=[RN_(h)])
                else:
                    for h in HS:
                        cN, cNT, rn_ = cur[h]
                        Q.op("pe", lambda h=h, cN=cN, cNT=cNT: nc.tensor.matmul(RB(h)[:, RC(h) + 128:RC(h) + 256], lhsT=cN, rhs=cNT, start=True, stop=True), r=rn_, w=[RN_(h)])
                for h in HS:
                    cp(ER(h), NKT[h][k % 2][:], RB(h)[:, RC(h) + 128:RC(h) + 256], [RN_(h)], ["NKT%d_%d" % (h, k % 2)])
                if k >= 2:
                    emitP(k - 1)
                if last:
                    emitP(k)
            Q.stage("S4")
            if GS < 4:
                HS = []
            for h in HS:
                def mmu(h=h):
                    pr = h // 2
                    nc.tensor.matmul(RB(h)[:, RC(h):RC(h) + 64], lhsT=TTB[h][:], rhs=OPB[:, 6, h, :], start=True, stop=True)
                    return nc.tensor.matmul(RB(h)[:, RC(h) + 64:RC(h) + 192], lhsT=OPB[:, 4, 2 * pr:2 * pr + 2, :].rearrange("p a d -> p (a d)"), rhs=TTB[h][:], start=True, stop=True)
                Q.op("pe", mmu, r=["TTB%d" % h, "opb6", "opb4"], w=[RN_(h)])
            for h in HS:
                cp(ER(h), UH[h][:], RB(h)[:, RC(h):RC(h) + 64], [RN_(h)], ["UH_%d" % h])
                cp(ER(h), WT[h][:], RB(h)[:, RC(h) + 64:RC(h) + 192], [RN_(h)], ["WT_%d" % h])
            for h in HS:
                Q.op("pe", lambda h=h: nc.tensor.matmul(RB(h)[:, RC(h) + 192:RC(h) + 256], lhsT=WT[h][hp_(h):hp_(h) + 64, :], rhs=SB[hp_(h):hp_(h) + 64, h // 2, :], start=True, stop=True),
                     r=["WT_%d" % h, "SB%d" % h], w=[RN_(h)])
            for h in HS:
                Q.op("dve", lambda h=h: V.tensor_tensor(out=VN[h][:], in0=UH[h][:], in1=RB(h)[:, RC(h) + 192:RC(h) + 256], op=ALU.subtract), r=["UH_%d" % h, RN_(h)], w=["VN_%d" % h])
            for h in HS:
                def mmo(h=h):
                    pr, hp = h // 2, hp_(h)
                    nc.tensor.matmul(PB(h)[:, PC(h):PC(h) + 64], lhsT=TT[hp:hp + 64, pr, 3, :], rhs=SB[hp:hp + 64, pr, :], start=True, stop=False)
                    nc.tensor.matmul(PB(h)[:, PC(h):PC(h) + 64], lhsT=QK[h][:], rhs=VN[h][:], start=False, stop=True)
                    return nc.tensor.matmul(PB(h)[:, PC(h) + 64:PC(h) + 128], lhsT=OPB[:, 5, 2 * pr:2 * pr + 2, :].rearrange("p a d -> p (a d)"), rhs=VN[h][:], start=True, stop=True)
                Q.op("pe", mmo, r=["TT01", "TT23", "SB%d" % h, "QK_%d" % h, "VN_%d" % h, "opb5"], w=[PN(h)])
            for h in HS:
                Q.op("act", lambda h=h: A_.copy(out=O[:, h, :], in_=PB(h)[:, PC(h):PC(h) + 64]), r=[PN(h)], w=["O%d" % h])
                Q.op("dve", lambda h=h: V.scalar_tensor_tensor(
                    out=SS[hp_(h):hp_(h) + 64, h // 2, :], in0=SS[hp_(h):hp_(h) + 64, h // 2, :], scalar=sm[hp_(h):hp_(h) + 64, 80 + h:81 + h],
                    in1=PB(h)[hp_(h):hp_(h) + 64, PC(h) + 64:PC(h) + 128], op0=ALU.mult, op1=ALU.add), r=[PN(h), "eg", "SS"], w=["SS%d" % h])
            for h in HS:
                Q.op("pool", lambda h=h: G.tensor_copy(out=SB[hp_(h):hp_(h) + 64, h // 2, :], in_=SS[hp_(h):hp_(h) + 64, h // 2, :]), r=["SS%d" % h, "SB"], w=["SB%d" % h])
            Q.stage("FIN")
            yg = YG[sl]
            O2 = O[:].rearrange("p h d -> p (h d)")
            Q.op("dve", lambda: V.tensor_tensor(out=junk[:, 0:512], in0=O2, in1=O2, op=ALU.mult), r=["O%d" % h for h in range(8)], w=["gjunk"])
            Q.op("dve", lambda: V.tensor_reduce(out=sm2[:, 8:16], in_=junk[:, 0:512].rearrange("p (g d) -> p g d", d=64), axis=AX.X, op=ALU.add), r=["gjunk"], w=["oss"])
            Q.op("act", lambda: A_.activation(out=sm2[:, 16:24], in_=sm2[:, 8:16], func=AF.Sqrt, scale=1.0 / 64, bias=epsc[:, 0:1]), r=["oss", "epsc"], w=["ors"])
            Q.op("dve", lambda: V.reciprocal(out=sm2[:, 16:24], in_=sm2[:, 16:24]), r=["ors"], w=["ors"])
            Q.op("act", lambda Z=Z: A_.activation(out=SZ[:], in_=Z[:, 0:512], func=AF.Silu), r=["ZAB%d" % zs], w=["SZ"])
            Q.op("dve", lambda yg=yg: V.tensor_tensor(out=yg[:].rearrange("p (h d) -> p h d", d=64), in0=O[:], in1=bcast_last(sm2[:, 16:24], 64), op=ALU.mult),
                 r=["O%d" % h for h in range(8)] + ["ors"], w=["YG%d" % sl])
            Q.op("pool", lambda yg=yg: G.tensor_tensor(out=yg[:].rearrange("p (h d) -> p h d", d=64), in0=yg[:].rearrange("p (h d) -> p h d", d=64),
                                                      in1=bcast_mid(gng[:], 8), op=ALU.mult), r=["YG%d" % sl, "gng"], w=["YG%d" % sl])
            Q.op("dve", lambda yg=yg: V.tensor_tensor(out=yg[:], in0=yg[:], in1=SZ[:], op=ALU.mult), r=["YG%d" % sl, "SZ"], w=["YG%d" % sl])
            Q.dma("sp", YMIX[b, t0:t0 + 128, 0:512], yg[:], r=["YG%d" % sl], w=["YMIX"], key="YG%d" % sl)
        it = 0
        for b in range(NBL):
            P.op("dve", lambda: V.memset(SS[:], 0.0), w=["SS"])
            P.op("dve", lambda: V.memset(SB[:], 0.0), w=["SB"])
            recs = []
            for tt in range(NTL):
                sl = it % 2
                it += 1
                Q = Rec(sl)
                tile_body(b, tt, sl, Q)
                recs.append(Q.stages)
            g_ = lambda st, nm: st.get(nm, [])
            emit(g_(recs[0], "L"))
            if NTL > 1:
                emit(g_(recs[1], "L"))
            emit(g_(recs[0], "P1"))
            emit(g_(recs[0], "P2"))
            def interleave(A, B):
                if not B:
                    return list(A)
                if not A:
                    return list(B)
                out, j = [], 0
                for i, a in enumerate(A):
                    out.append(a)
                    want = (i + 1) * len(B) // len(A)
                    while j < want:
                        out.append(B[j])
                        j += 1
                out.extend(B[j:])
                return out

            for tt in range(NTL):
                nxt = recs[tt + 1] if tt + 1 < NTL else {}
                if tt + 2 < NTL:
                    emit(g_(recs[tt + 2], "L"))
                head = g_(recs[tt], "S2") + g_(recs[tt], "S3_1")
                emit(interleave(head, g_(recs[tt - 1], "FIN") if tt > 0 else []))
                mid = []
                for k in (2, 3, 4):
                    mid += g_(recs[tt], "S3_%d" % k)
                emit(interleave(mid, g_(nxt, "P1")))
                tail = g_(recs[tt], "S3_5") + g_(recs[tt], "S3_6") + g_(recs[tt], "S4")
                emit(interleave(tail, g_(nxt, "P2")))
                if tt == NTL - 1:
                    emit(g_(recs[tt], "FIN"))
        P.flush()
    if debug and "ygdn" in debug:
        with ExitStack() as es:
            t = T(es, "dbgy", [128, 512], F32)
            for tt in range(NTL):
                P.dma("sp", t[:], YMIX[0, tt * 128:(tt + 1) * 128, 0:512], r=["YMIX"], w=["dbgy"], key="dbgy")
                P.dma("sp", dbg["ygdn"][tt * 128:(tt + 1) * 128, :], t[:], r=["dbgy"], w=["dbgyo"], key="dbgyo")
            P.flush()


def phase_nsa(nc, P, T, PS, I, l, PROJ, YMIX, C, debug, dbg):
    identb, epsc, ident = C["identb"], C["epsc"], C["ident"]
    V, A_, G = nc.vector, nc.scalar, nc.gpsimd
    NTL = (debug or {}).get("_nsa_tiles", NT)
    NBL = (debug or {}).get("_nsa_nb", NB)
    with ExitStack() as es:
        es0 = ExitStack()
        specs = [("QAc", [3, 2, 512], "nQAc", BF16),
                 ("KAc", [3, 16, 128], "nKAc", BF16), ("NEGc", [128, 16, 128], "nNEGc", BF16), ("CMd", [128, 128], "nCMd", BF16),
                 ("CMw4", [128, 128], "nCMw4", BF16), ("OV", [128, 32], "nOV", F32),
                 ("CV", [128, 16, 32], "nCV", F32), ("CB", [128, 16, 32], "nCB", F32)]
        ct = {}
        for (name, shape, src, dt) in specs:
            ct[name] = T(es, name, shape, dt)
        QAc, KAc, NEGc, CMd, CMw4, OV, CV, CB = [ct[sp[0]] for sp in specs]
        gq = T(es, "gq", [128, 64], F32)
        gk = T(es, "gk", [128, 3, 64], F32)
        b2 = T(es, "b2", [128, 2, 64], F32)
        b1T = T(es, "b1T", [128, 2, 2], F32)
        P.dma("sp", gq[:], I["nsa_q_norm_g"][l].partition_broadcast(128), w=["gq"], key="nc2")
        P.dma("sp", gk[:].rearrange("p a d -> p (a d)"), I["nsa_k_norm_g"][l].partition_broadcast(128), w=["gk"], key="nc2")
        for kv in range(2):
            P.dma("sp", b2[:, kv, :], I["cmp_b2"][l, kv].partition_broadcast(128), w=["b2"], key="nc2")
        P.dma("sp", b1T[:], I["cmp_b1T"][l].rearrange("k p c -> p k c"), w=["b1T"], key="nc2")
        P.op("dve", lambda: V.tensor_scalar(out=gq[:], in0=gq[:], scalar1=0.125, scalar2=None, op0=ALU.mult), r=["gq"], w=["gq"])
        W1 = T(es, "W1", [64, 2, 32, 256], BF16)
        W2 = T(es, "W2", [128, 2, 2, 64], BF16)
        peT = T(es, "peT", [64, 2, 32], BF16)
        bias1 = T(es, "bias1", [128, 2, 2], F32)
        for (name, shape, src, dt) in specs:
            if dt == F32:
                P.dma("sp", ct[name][:], I[src], w=[name + "32"], key="nc_" + name)
            else:
                t32 = T(es0, name + "32", shape, F32)
                P.dma("sp", t32[:], I[src], w=[name + "32"], key="nc_" + name)
                P.op("pool", lambda t=ct[name], t32=t32: G.tensor_copy(out=t[:], in_=t32[:]), r=[name + "32"], w=[name])
        w1s = T(es0, "w1s", [64, 8, 256], F32)
        w2s = T(es0, "w2s", [128, 2, 2, 64], F32)
        pes = T(es0, "pes", [64, 2, 32], F32)
        for kv in range(2):
            for q4 in range(4):
                P.dma("sp", w1s[:], I["cmp_w1"][l, kv].rearrange("(l d) h -> d l h", d=64)[:, q4 * 8:(q4 + 1) * 8, :], r=[], w=["w1s"], key="w1s")
                P.op("pool", lambda kv=kv, q4=q4: G.tensor_copy(out=W1[:, kv, q4 * 8:(q4 + 1) * 8, :], in_=w1s[:]), r=["w1s"], w=["W1"])
            P.dma("sp", w2s[:, kv], I["cmp_w2"][l, kv].rearrange("(c p) d -> p c d", p=128), w=["w2s"], key="w2s")
            P.dma("sp", pes[:, kv, :], I["cmp_peT"][l, kv], w=["pes"], key="w2s")
        P.op("pool", lambda: G.tensor_copy(out=W2[:], in_=w2s[:]), r=["w2s"], w=["W2"])
        P.op("pool", lambda: G.tensor_copy(out=peT[:], in_=pes[:]), r=["pes"], w=["peT"])

        def mmpe():
            ins = None
            for kv in range(2):
                for hc in range(2):
                    col = kv * 2 + hc
                    for ll in range(32):
                        ins = nc.tensor.matmul(PS[0][:, col:col + 1], lhsT=W1[:, kv, ll, hc * 128:(hc + 1) * 128], rhs=peT[:, kv, ll:ll + 1],
                                               start=(ll == 0), stop=(ll == 31))
            return ins
        P.op("pe", mmpe, r=["W1", "peT"], w=["ps0"])
        P.op("dve", lambda: V.tensor_tensor(out=bias1[:].rearrange("p a b -> p (a b)"), in0=PS[0][:, 0:4], in1=b1T[:].rearrange("p a b -> p (a b)"), op=ALU.add),
             r=["ps0", "b1T"], w=["bias1"])
        P.flush()
        es0.close()
        qT = T(es, "qT", [99, 8, S], BF16)
        kT = T(es, "kT", [99, 2, 2, S], BF16)
        P.dma("sp", qT[64:67, :, :], I["nQrows"], w=["qTaug"], key="qTaug")
        for br_ in range(2):
            for g_ in range(2):
                P.dma("sp", kT[64:99, br_, g_, :], I["nKrows"], w=["kTaug"], key="kTaug")
        cT_ = T(es, "cT", [64, 2, 2, S], BF16)
        VA = T(es, "VA", [128, NT, 2, 2, 65], BF16)
        SG = T(es, "SG", [128, NT, 24], F32)
        YN = T(es, "YN", [128, NT, 8, 64], F32)
        IMP = T(es, "IMP", [128, NT, 2, 32], F32)
        selbT = T(es, "selbT", [32, 2, S], BF16)
        kcT = T(es, "kcT", [64, 2, 128], BF16)
        vcA = T(es, "vcA", [128, 2, 97], BF16)
        hid = T(es, "hid", [128, 2, 128], BF16)
        NIN = [T(es, "NIN%d" % i, [128, 1304], F32) for i in range(2)]
        NJs = [T(es, "nj%d" % i, [128, 896], F32) for i in range(2)]
        NSMs = [T(es, "nsm%d" % i, [128, 64], F32) for i in range(2)]
        NB16s = [T(es, "NB16%d" % i, [128, 16, 64], BF16) for i in range(2)]
        nj, nsm, NB16 = NJs[0], NSMs[0], NB16s[0]
        PT = [T(es, "PT%d" % i, [128, 4, 128], BF16) for i in range(6)]
        FIN = [T(es, "fin%d" % i, [128, 4, 64], F32) for i in range(2)]
        FSM = [T(es, "fsm%d" % i, [128, 56], F32) for i in range(2)]
        SELB = T(es, "SELB", [128, 2, NT, 32], BF16)
        P.op("dve", lambda: V.memset(VA[:, :, :, :, 64:65], 1.0), w=["VA"])
        it = 0
        for b in range(NBL):
            def prep_tile(tt, sl, Q):
                nj, nsm, NB16 = NJs[sl], NSMs[sl], NB16s[sl]
                Q.stage("N1")
                t0 = tt * 128
                X = NIN[sl]
                Q.dma("sp", X[:], PROJ[b, PADR + t0:PADR + t0 + 128, C_NQ:INC], r=["PROJ"], w=["NIN%d" % sl], key="NIN%d" % sl)
                Q.op("dve", lambda X=X: V.tensor_tensor(out=nj[:, 0:512], in0=X[:, 0:512], in1=X[:, 0:512], op=ALU.mult), r=["NIN%d" % sl], w=["nj_%d" % sl])
                Q.op("pool", lambda X=X: G.tensor_tensor(out=nj[:, 512:640], in0=X[:, 768:896], in1=X[:, 768:896], op=ALU.mult), r=["NIN%d" % sl], w=["nj2_%d" % sl])
                Q.op("pool", lambda X=X: G.tensor_tensor(out=nj[:, 640:768], in0=X[:, 1024:1152], in1=X[:, 1024:1152], op=ALU.mult), r=["NIN%d" % sl], w=["nj3_%d" % sl])
                Q.op("dve", lambda: V.tensor_reduce(out=nsm[:, 0:12], in_=nj[:, 0:768].rearrange("p (g d) -> p g d", d=64), axis=AX.X, op=ALU.add),
                     r=["nj_%d" % sl, "nj2_%d" % sl, "nj3_%d" % sl], w=["nss_%d" % sl])
                Q.op("act", lambda: A_.activation(out=nsm[:, 16:28], in_=nsm[:, 0:12], func=AF.Sqrt, scale=1.0 / 64, bias=epsc[:, 0:1]), r=["nss_%d" % sl, "epsc"], w=["nrs_%d" % sl])
                Q.op("dve", lambda: V.reciprocal(out=nsm[:, 16:28], in_=nsm[:, 16:28]), r=["nrs_%d" % sl], w=["nrs_%d" % sl])
                Xq = X[:, 0:512].rearrange("p (h d) -> p h d", d=64)
                Q.op("dve", lambda Xq=Xq: V.tensor_tensor(out=nj[:, 0:512].rearrange("p (h d) -> p h d", d=64), in0=Xq, in1=bcast_last(nsm[:, 16:24], 64), op=ALU.mult),
                     r=["NIN%d" % sl, "nrs_%d" % sl, "nss_%d" % sl], w=["nj_%d" % sl])
                Q.op("dve", lambda: V.tensor_tensor(out=NB16[:, 0:8, :], in0=nj[:, 0:512].rearrange("p (h d) -> p h d", d=64), in1=bcast_mid(gq[:], 8), op=ALU.mult),
                     r=["nj_%d" % sl, "gq"], w=["NB16q_%d" % sl])
                for bi, (c0, rs0) in enumerate(((768, 24), (1024, 26))):
                    Xk = X[:, c0:c0 + 128].rearrange("p (h d) -> p h d", d=64)
                    Q.op("pool", lambda Xk=Xk, bi=bi, rs0=rs0: G.tensor_tensor(out=nj[:, 768 + bi * 64 * 0:768 + 128].rearrange("p (h d) -> p h d", d=64) if False else nj[:, 768:896].rearrange("p (h d) -> p h d", d=64),
                                                                           in0=Xk, in1=bcast_last(nsm[:, rs0:rs0 + 2], 64), op=ALU.mult),
                         r=["NIN%d" % sl, "nrs_%d" % sl, "nss_%d" % sl], w=["njk_%d" % sl])
                    Q.op("pool", lambda bi=bi: G.tensor_tensor(out=NB16[:, 8 + 2 * bi:10 + 2 * bi, :], in0=nj[:, 768:896].rearrange("p (h d) -> p h d", d=64),
                                                           in1=bcast_mid(gk[:, 1 + bi, :], 2), op=ALU.mult), r=["njk_%d" % sl, "gk"], w=["NB16k%d_%d" % (bi, sl)])
                Q.op("act", lambda X=X: A_.copy(out=NB16[:, 12:14, :].rearrange("p h d -> p (h d)"), in_=X[:, 512:640]), r=["NIN%d" % sl], w=["NB16c_%d" % sl])
                Q.op("act", lambda X=X: A_.copy(out=NB16[:, 14:16, :].rearrange("p h d -> p (h d)"), in_=X[:, 640:768]), r=["NIN%d" % sl], w=["NB16v_%d" % sl])
                Q.op("act", lambda X=X, tt=tt: A_.copy(out=VA[:, tt, 0, :, 0:64], in_=X[:, 896:1024].rearrange("p (g d) -> p g d", d=64)), r=["NIN%d" % sl], w=["VA"])
                Q.op("act", lambda X=X, tt=tt: A_.copy(out=VA[:, tt, 1, :, 0:64], in_=X[:, 1152:1280].rearrange("p (g d) -> p g d", d=64)), r=["NIN%d" % sl], w=["VA"])
                Q.op("act", lambda X=X, tt=tt: A_.activation(out=SG[:, tt, :], in_=X[:, 1280:1304], func=AF.Sigmoid), r=["NIN%d" % sl], w=["SG"])

                Q.stage("N2")

                def trs():
                    ins = None
                    for j in range(16):
                        bank = PS[1 + 2 * sl + j // 8][:].bitcast(BF16)
                        ins = nc.tensor.transpose(out=bank[0:64, (j % 8) * 128:(j % 8 + 1) * 128], in_=NB16[:, j, :], identity=identb[:])
                    return ins
                Q.op("pe", trs, r=["NB16q_%d" % sl, "NB16k0_%d" % sl, "NB16k1_%d" % sl, "NB16c_%d" % sl, "NB16v_%d" % sl, "identb"], w=["ps%d" % (1 + 2 * sl), "ps%d" % (2 + 2 * sl)])
                Q.op("act", lambda t0=t0: A_.copy(out=qT[0:64, :, t0:t0 + 128], in_=PS[1 + 2 * sl][:].bitcast(BF16)[0:64, :].rearrange("p (h t) -> p h t", t=128)), r=["ps%d" % (1 + 2 * sl)], w=["qT"])
                Q.op("dve", lambda t0=t0: V.tensor_copy(out=kT[0:64, :, :, t0:t0 + 128].rearrange("p a g t -> p (a g) t"),
                                                        in_=PS[2 + 2 * sl][:].bitcast(BF16)[0:64, 0:512].rearrange("p (h t) -> p h t", t=128)), r=["ps%d" % (2 + 2 * sl)], w=["kT"])
                Q.op("dve", lambda t0=t0: V.tensor_copy(out=cT_[:, :, :, t0:t0 + 128].rearrange("p a g t -> p (a g) t"),
                                                        in_=PS[2 + 2 * sl][:].bitcast(BF16)[0:64, 512:1024].rearrange("p (h t) -> p h t", t=128)), r=["ps%d" % (2 + 2 * sl)], w=["cT"])

            precs = []
            for tt in range(NTL):
                sl = it % 2
                it += 1
                Q = RecQ()
                prep_tile(tt, sl, Q)
                precs.append(Q.stages)
            emit_ops(P, precs[0]["N1"])
            for tt in range(NTL):
                emit_ops(P, interleave_ops(precs[tt]["N2"], precs[tt + 1]["N1"] if tt + 1 < NTL else []))
            P.flush()
            if NTL < NT:
                continue
            for kv in range(2):
                for g in range(2):
                    for hc in range(2):
                        def mm1(kv=kv, g=g, hc=hc):
                            ins = None
                            for ll in range(32):
                                ins = nc.tensor.matmul(PS[3][:, 0:127], lhsT=W1[:, kv, ll, hc * 128:(hc + 1) * 128],
                                                       rhs=cT_[:, kv, g, ll:ll + 16 * 126 + 1:16], start=(ll == 0), stop=(ll == 31))
                            return ins
                        P.op("pe", mm1, r=["W1", "cT"], w=["ps3"])
                        P.op("act", lambda kv=kv, hc=hc: A_.activation(out=hid[:, hc, 0:127], in_=PS[3][:, 0:127], func=AF.Silu, bias=bias1[:, kv, hc:hc + 1]),
                             r=["ps3", "bias1"], w=["hid%d" % hc])

                    def mm2(kv=kv):
                        ins = None
                        for hc in range(2):
                            ins = nc.tensor.matmul(PS[4][0:127, 0:64], lhsT=hid[:, hc, 0:127], rhs=W2[:, kv, hc, :], start=(hc == 0), stop=(hc == 1))
                        return ins
                    P.op("pe", mm2, r=["hid0", "hid1", "W2"], w=["ps4"])
                    if kv == 0:
                        P.op("dve", lambda: V.memset(nj[:, 0:64], 0.0), w=["nj"])
                        P.op("dve", lambda: V.tensor_tensor(out=nj[0:127, 0:64], in0=PS[4][0:127, 0:64], in1=b2[0:127, 0, :], op=ALU.add), r=["ps4", "b2"], w=["nj"])
                        P.op("dve", lambda: V.tensor_tensor(out=nj[:, 64:128], in0=nj[:, 0:64], in1=nj[:, 0:64], op=ALU.mult), r=["nj"], w=["nj2"])
                        P.op("dve", lambda: V.tensor_reduce(out=nsm[:, 32:33], in_=nj[:, 64:128], axis=AX.X, op=ALU.add), r=["nj2"], w=["nss"])
                        P.op("act", lambda: A_.activation(out=nsm[:, 33:34], in_=nsm[:, 32:33], func=AF.Sqrt, scale=1.0 / 64, bias=epsc[:, 0:1]), r=["nss", "epsc"], w=["nrs"])
                        P.op("dve", lambda: V.reciprocal(out=nsm[:, 33:34], in_=nsm[:, 33:34]), r=["nrs"], w=["nrs"])
                        P.op("dve", lambda: V.scalar_tensor_tensor(out=NB16[:, 0, :], in0=nj[:, 0:64], scalar=nsm[:, 33:34], in1=gk[:, 0, :], op0=ALU.mult, op1=ALU.mult),
                             r=["nj", "nrs", "gk"], w=["NB16q"])
                        P.op("pe", lambda: nc.tensor.transpose(out=PS[4][:].bitcast(BF16)[0:64, 512:640], in_=NB16[:, 0, :], identity=identb[:]), r=["NB16q", "identb"], w=["ps4"])
                        P.op("dve", lambda g=g: V.tensor_copy(out=kcT[:, g, :], in_=PS[4][:].bitcast(BF16)[0:64, 512:640]), r=["ps4"], w=["kcT"])
                    else:
                        P.op("dve", lambda g=g: V.memset(vcA[:, g, 0:64], 0.0), w=["vcA"])
                        P.op("dve", lambda g=g: V.tensor_tensor(out=vcA[0:127, g, 0:64], in0=PS[4][0:127, 0:64], in1=b2[0:127, 1, :], op=ALU.add), r=["ps4", "b2"], w=["vcA"])
                        P.op("dve", lambda g=g: V.memset(vcA[:, g, 64:65], 1.0), w=["vcA"])
                        P.op("dve", lambda g=g: V.tensor_copy(out=vcA[:, g, 65:97], in_=OV[:]), r=["OV32"], w=["vcA"])

            cnt = [0]
            SK = 3

            def add_attend(jobs, g, tt, kts, branch, post):
                t0 = tt * 128
                acc_i = 6 + (cnt[0] % 2)
                cnt[0] += 1
                acc, accn = PS[acc_i], "ps%d" % acc_i
                W = 97 if branch == 0 else 65
                for ki, kt in enumerate(kts):
                    dl = tt - kt

                    def mms(sb, kt=kt, dl=dl):
                        if branch == 0:
                            full = [(kcT[:, g, :], qT[0:64, 4 * g:4 * g + 4, t0:t0 + 128]), (KAc[:, tt, :], QAc[:, g, :])]
                            masks = [(identb[:], NEGc[:, tt, :])]
                        else:
                            kk = 99 if branch == 1 else 67
                            full = [(kT[0:kk, branch - 1, g, kt * 128:(kt + 1) * 128], qT[0:kk, 4 * g:4 * g + 4, t0:t0 + 128])]
                            masks = []
                            if dl == 0:
                                masks.append((identb[:], CMd[:]))
                            if branch == 2 and dl == 4:
                                masks.append((identb[:], CMw4[:]))
                        ins = None
                        for fi, (lt, rh) in enumerate(full):
                            ins = nc.tensor.matmul(sb[:], lhsT=lt, rhs=rh, start=(fi == 0), stop=(fi == len(full) - 1 and not masks), skip_group_check=True)
                        for mi, (lt, rh) in enumerate(masks):
                            for r in range(4):
                                ins = nc.tensor.matmul(sb[:, r * 128:(r + 1) * 128], lhsT=lt, rhs=rh, start=False, stop=(mi == len(masks) - 1), skip_group_check=True)
                        return ins

                    def mmv(pt, kt=kt, ki=ki, nk=len(kts)):
                        ins = None
                        for r in range(4):
                            rhs = vcA[:, g, :] if branch == 0 else VA[:, kt, branch - 1, g, :]
                            ins = nc.tensor.matmul(acc[:, r * W:(r + 1) * W], lhsT=pt[:, r, :], rhs=rhs, start=(ki == 0 and r == 0), stop=(ki == nk - 1), skip_group_check=True)
                        return ins
                    last = (ki == len(kts) - 1)
                    jobs.append(dict(mms=mms, mmv=mmv, accn=accn, post=(lambda acc=acc, accn=accn, W=W: post(acc, accn, W)) if last else None))

            def run_jobs(jobs):
                n = len(jobs)
                rd = ["qT", "qTaug", "qTsel", "kcT", "kT", "kTaug", "KAc", "QAc", "NEGc", "CMd", "CMw4", "identb"]
                for i in range(n + SK):
                    if i < n:
                        j = jobs[i]
                        sb_i = 1 + (i % 5)
                        sb, sbn = PS[sb_i], "ps%d" % sb_i
                        pt, ptn = PT[i % 6], "PT%d" % (i % 6)
                        j["pt"], j["ptn"] = pt, ptn
                        P.op("pe", lambda j=j, sb=sb: j["mms"](sb), r=rd, w=[sbn])
                        P.op("act", lambda sb=sb, pt=pt: A_.activation(out=pt[:].rearrange("p r t -> p (r t)"), in_=sb[:], func=AF.Exp), r=[sbn], w=[ptn])
                    if i >= SK:
                        j = jobs[i - SK]
                        P.op("pe", lambda j=j: j["mmv"](j["pt"]), r=[j["ptn"], "vcA", "VA"], w=[j["accn"]])
                        if j["post"] is not None:
                            j["post"]()

            fcnt = [0]

            def finalize(acc, accn, W, g, tt, branch):
                a3 = acc[:, 0:4 * W].rearrange("p (r w) -> p r w", w=W)
                fi = fcnt[0] % 2
                fcnt[0] += 1
                fs, fn_ = FSM[fi], FIN[fi]
                fsn, finn = "fsm%d" % fi, "fin%d" % fi
                P.op("dve", lambda: V.tensor_scalar(out=fs[:, 0:4], in0=a3[:, :, 64], scalar1=1e-30, scalar2=None, op0=ALU.add), r=[accn], w=[fsn])
                P.op("dve", lambda: V.reciprocal(out=fs[:, 0:4], in_=fs[:, 0:4]), r=[fsn], w=[fsn])
                P.op("dve", lambda: V.tensor_tensor(out=fs[:, 4:8], in0=fs[:, 0:4], in1=SG[:, tt, branch * 8 + 4 * g:branch * 8 + 4 * g + 4], op=ALU.mult), r=[fsn, "SG"], w=[fsn + "b"])
                yv = YN[:, tt, 4 * g:4 * g + 4, :]
                if branch == 0:
                    P.op("dve", lambda: V.tensor_tensor(out=yv, in0=a3[:, :, 0:64], in1=bcast_last(fs[:, 4:8], 64), op=ALU.mult), r=[accn, fsn + "b"], w=["YN"])
                    P.op("dve", lambda: V.tensor_tensor(out=fn_[:, :, 0:32], in0=a3[:, :, 65:97], in1=bcast_last(fs[:, 0:4], 32), op=ALU.mult), r=[accn, fsn], w=[finn])
                    P.op("dve", lambda: V.tensor_reduce(out=IMP[:, tt, g, :], in_=fn_[:, :, 0:32].rearrange("p r j -> p j r"), axis=AX.X, op=ALU.add), r=[finn], w=["IMP%d_%d" % (tt, g)])
                    sc = fs[:, 8:40]
                    P.op("pool", lambda: G.tensor_tensor(out=sc, in0=IMP[:, tt, g, :], in1=CV[:, tt, :], op=ALU.mult), r=["IMP%d_%d" % (tt, g), "CV32"], w=[fsn + "c"])
                    P.op("pool", lambda: G.tensor_tensor(out=sc, in0=sc, in1=CB[:, tt, :], op=ALU.add), r=[fsn + "c", "CB32"], w=[fsn + "c"])
                    P.op("dve", lambda: V.max(out=fs[:, 40:48], in_=sc), r=[fsn + "c"], w=[fsn + "d"])
                    P.op("dve", lambda: V.tensor_scalar(out=fs[:, 48:49], in0=fs[:, 47:48], scalar1=-0.5e9, scalar2=None, op0=ALU.max), r=[fsn + "d"], w=[fsn + "e"])
                    P.op("dve", lambda: V.tensor_scalar(out=sc, in0=sc, scalar1=fs[:, 48:49], scalar2=30000.0, op0=ALU.is_ge, op1=ALU.mult), r=[fsn + "c", fsn + "e"], w=[fsn + "c"])
                    P.op("dve", lambda: V.tensor_scalar(out=SELB[:, g, tt, :], in0=sc, scalar1=-30000.0, scalar2=None, op0=ALU.add), r=[fsn + "c"], w=["SELB"])
                else:
                    P.op("dve", lambda: V.tensor_tensor(out=fn_[:], in0=a3[:, :, 0:64], in1=bcast_last(fs[:, 4:8], 64), op=ALU.mult), r=[accn, fsn + "b"], w=[finn])
                    P.op("pool", lambda: G.tensor_tensor(out=yv, in0=yv, in1=fn_[:], op=ALU.add), r=[finn, "YN"], w=["YN"])

            jobs = []
            for tt in range(NT):
                for g in range(2):
                    add_attend(jobs, g, tt, [0], 0, lambda acc, accn, W, g=g, tt=tt: finalize(acc, accn, W, g, tt, 0))
            run_jobs(jobs)
            for g in range(2):
                for tq in range(4):
                    bk = 1 + (tq % 2)

                    def trsel(g=g, tq=tq, bk=bk):
                        ins = None
                        for j in range(4):
                            ins = nc.tensor.transpose(out=PS[bk][:].bitcast(BF16)[0:32, j * 128:(j + 1) * 128], in_=SELB[:, g, tq * 4 + j, :], identity=identb[:])
                        return ins
                    P.op("pe", trsel, r=["SELB", "identb"], w=["ps%d" % bk])
                    if bk == 1:
                        P.op("dve", lambda g=g, tq=tq, bk=bk: V.tensor_copy(out=selbT[:, g, tq * 512:(tq + 1) * 512], in_=PS[bk][:].bitcast(BF16)[0:32, 0:512]), r=["ps%d" % bk], w=["selbT"])
                    else:
                        P.op("act", lambda g=g, tq=tq, bk=bk: A_.copy(out=selbT[:, g, tq * 512:(tq + 1) * 512], in_=PS[bk][:].bitcast(BF16)[0:32, 0:512]), r=["ps%d" % bk], w=["selbT"])
            for h in range(8):
                P.dma("sp", qT[67:99, h, :], selbT[:, h // 4, :], r=["selbT"], w=["qTsel"], key="qTsel")
            jobs = []
            for tt in range(NT):
                for g in range(2):
                    add_attend(jobs, g, tt, list(range(0, tt + 1)), 1, lambda acc, accn, W, g=g, tt=tt: finalize(acc, accn, W, g, tt, 1))
                    add_attend(jobs, g, tt, list(range(max(0, tt - 4), tt + 1)), 2, lambda acc, accn, W, g=g, tt=tt: finalize(acc, accn, W, g, tt, 2))
            run_jobs(jobs)
            P.dma("sp", YMIX[b].rearrange("(tt p) c -> p tt c", p=128)[:, :, 512:1024], YN[:].rearrange("p t h d -> p t (h d)"), r=["YN"], w=["YMIX"], key="YN")
        P.flush()
    if debug and "ynsa" in debug:
        with ExitStack() as es:
            t = T(es, "dbgy2", [128, 512], F32)
            for tt in range(NT):
                P.dma("sp", t[:], YMIX[0, tt * 128:(tt + 1) * 128, 512:1024], r=["YMIX"], w=["dbgy2"], key="dbgy2")
                P.dma("sp", dbg["ynsa"][tt * 128:(tt + 1) * 128, :], t[:], r=["dbgy2"], w=["dbgy2o"], key="dbgy2o")
            P.flush()


def phase_out_moe(nc, P, T, PS, I, l, YMIX, X1, xsrc, xdst, MODS, C, debug, dbg):
    identb, epsc, modT, a2 = C["identb"], C["epsc"], C["modT"], C["a2"]
    V, A_, G = nc.vector, nc.scalar, nc.gpsimd
    NBL = (debug or {}).get("_moe_nb", NB)
    NEL = (debug or {}).get("_moe_ne", NEXP)
    with ExitStack() as es:
        h2T = T(es, "h2T", [128, 8, S], BF16)
        yacc = T(es, "yacc", [128, NT, D], F32)
        GATE = T(es, "GATE", [128, NT, NEXP], F32)
        gbc = T(es, "gbc", [128, D], F32)
        rwb = T(es, "rwb", [128, 8, NEXP], BF16)
        rwl = T(es, "rwl", [128, 8, NEXP], BF16)
        rbias = T(es, "rbias", [128, NEXP], F32)
        ss = T(es, "mss", [128, 4], F32)
        xa = [T(es, "xa%d" % i, [128, D], F32) for i in range(2)]
        for b in range(NBL):
            with ExitStack() as es2:
                RSC = T(es2, "RSC", [128, NT, NEXP], F32)
                RBI = T(es2, "RBI", [128, NT, NEXP], F32)
                RSEL = T(es2, "RSEL", [128, NT, NEXP], F32)
                RTM = T(es2, "RTM", [128, 11, NT * 4], F32)
                RT1 = T(es2, "RT1", [128, 3, NT], F32)
                wob = T(es2, "wob", [128, 8, D], BF16)
                ym = [T(es2, "ym%d" % i, [128, D], F32) for i in range(2)]
                ymb = [T(es2, "ymb%d" % i, [128, D], BF16) for i in range(2)]
                ymT = [T(es2, "ymT%d" % i, [128, 8, 128], BF16) for i in range(2)]
                xnb = [T(es2, "xnb%d" % i, [128, D], BF16) for i in range(2)]
                junk = T(es2, "mjunk", [128, D], F32)
                wst = T(es2, "wst", [128, 8, 512], F32)
                rw32 = T(es2, "rw32", [128, 8, NEXP], F32)
                rw32b = T(es2, "rw32b", [128, 8, NEXP], F32)
                for hf in range(2):
                    P.dma("sp", wst[:], I["w_out"][l].rearrange("(kc p) f -> p kc f", p=128)[:, :, hf * 512:(hf + 1) * 512], w=["wst"], key="wst")
                    P.op("pool", lambda hf=hf: G.tensor_copy(out=wob[:, :, hf * 512:(hf + 1) * 512], in_=wst[:]), r=["wst"], w=["wob"])
                P.dma("sp", rw32[:], I["router_w"].rearrange("(kc p) e -> p kc e", p=128), w=["rw32"], key="rw32")
                P.dma("sp", rbias[:], I["router_bias"].partition_broadcast(128), w=["rbias"], key="rw32")
                P.op("dve", lambda: V.tensor_copy(out=rwb[:], in_=rw32[:]), r=["rw32"], w=["rwb"])
                P.op("dve", lambda: V.tensor_copy(out=rw32b[:], in_=rwb[:]), r=["rwb"], w=["rw32b"])
                P.op("dve", lambda: V.tensor_tensor(out=rw32b[:], in0=rw32[:], in1=rw32b[:], op=ALU.subtract), r=["rw32", "rw32b"], w=["rw32b"])
                P.op("dve", lambda: V.tensor_copy(out=rwl[:], in_=rw32b[:]), r=["rw32b"], w=["rwl"])
                P.dma("sp", gbc[:], MODS[l, b:b + 1, 2048:3072].partition_broadcast(128), r=["MODS"], w=["gbc"], key="gbc")
                erecs = []
                for tt in range(NT):
                    Q = RecQ()
                    Q.stage("E1")
                    sl = tt % 2
                    t0 = tt * 128
                    Y, YB, YT, XA, XN = ym[sl], ymb[sl], ymT[sl], xa[sl], xnb[sl]
                    Q.dma("sp", Y[:], YMIX[b, t0:t0 + 128, :], r=["YMIX"], w=["ym%d" % sl], key="ym%d" % sl)
                    Q.dma("sp", XA[:], xsrc[b, t0:t0 + 128, :], r=["X2"], w=["xa%d" % sl], key="xa%d" % sl)
                    Q.op("pool", lambda Y=Y, YB=YB: G.tensor_copy(out=YB[:], in_=Y[:]), r=["ym%d" % sl], w=["ymb%d" % sl])
                    Q.stage("E1b")

                    def tr(src, b0, b1):
                        def f():
                            ins = None
                            for kc in range(8):
                                tp = (PS[b0] if kc < 4 else PS[b1])[:].bitcast(BF16)
                                ins = nc.tensor.transpose(out=tp[:, (kc % 4) * 128:(kc % 4 + 1) * 128], in_=src[:, kc * 128:(kc + 1) * 128], identity=identb[:])
                            return ins
                        return f
                    Q.op("pe", tr(YB, 0, 1), r=["ymb%d" % sl, "identb"], w=["ps0", "ps1"])
                    Q.op("act", lambda YT=YT: A_.copy(out=YT[:, 0:4, :].rearrange("p k t -> p (k t)"), in_=PS[0][:].bitcast(BF16)[:, 0:512]), r=["ps0"], w=["ymT%da" % sl])
                    Q.op("dve", lambda YT=YT: V.tensor_copy(out=YT[:, 4:8, :].rearrange("p k t -> p (k t)"), in_=PS[1][:].bitcast(BF16)[:, 0:512]), r=["ps1"], w=["ymT%db" % sl])
                    for hf in range(2):
                        def mm(YT=YT, hf=hf):
                            ins = None
                            for kc in range(8):
                                ins = nc.tensor.matmul(PS[2 + hf][:], lhsT=YT[:, kc, :], rhs=wob[:, kc, hf * 512:(hf + 1) * 512], start=(kc == 0), stop=(kc == 7))
                            return ins
                        Q.op("pe", mm, r=["ymT%da" % sl, "ymT%db" % sl, "wob"], w=["ps%d" % (2 + hf)])
                        Q.op("dve", lambda Y=Y, hf=hf: V.tensor_tensor(out=Y[:, hf * 512:(hf + 1) * 512], in0=PS[2 + hf][:], in1=gbc[:, hf * 512:(hf + 1) * 512], op=ALU.mult),
                             r=["ps%d" % (2 + hf), "gbc", "ymb%d" % sl], w=["ym%d" % sl])
                    Q.op("pool", lambda Y=Y, XA=XA: G.tensor_tensor(out=XA[:], in0=XA[:], in1=Y[:], op=ALU.add), r=["ym%d" % sl, "xa%d" % sl], w=["xa%d" % sl])
                    Q.dma("pool", X1[b, t0:t0 + 128, :], XA[:], r=["xa%d" % sl], w=["X1"], key="xs%d" % sl)
                    Q.op("act", lambda XA=XA, sl=sl: A_.activation(out=junk[:], in_=XA[:], func=AF.Square, accum_out=ss[:, sl:sl + 1]), r=["xa%d" % sl], w=["mjunk", "mss%d" % sl])
                    Q.op("act", lambda sl=sl: A_.activation(out=ss[:, 2 + sl:3 + sl], in_=ss[:, sl:sl + 1], func=AF.Sqrt, scale=1.0 / D, bias=epsc[:, 0:1]), r=["mss%d" % sl, "epsc"], w=["mrs%d" % sl])
                    Q.op("dve", lambda sl=sl: V.reciprocal(out=ss[:, 2 + sl:3 + sl], in_=ss[:, 2 + sl:3 + sl]), r=["mrs%d" % sl], w=["mrs%d" % sl])
                    Q.op("dve", lambda XA=XA, XN=XN, sl=sl: V.tensor_scalar(out=XN[:], in0=XA[:], scalar1=ss[:, 2 + sl:3 + sl], scalar2=None, op0=ALU.mult), r=["xa%d" % sl, "mrs%d" % sl], w=["xnb%d" % sl])
                    Q.stage("E2")
                    Q.op("pe", tr(XN, 4, 5), r=["xnb%d" % sl, "identb"], w=["ps4", "ps5"])
                    for kc in range(8):
                        bk = 4 if kc < 4 else 5
                        src = PS[bk][:].bitcast(BF16)[:, (kc % 4) * 128:(kc % 4 + 1) * 128]
                        if kc < 4:
                            Q.op("act", lambda kc=kc, src=src, t0=t0: A_.activation(out=h2T[:, kc, t0:t0 + 128], in_=src, func=AF.Identity,
                                                                              scale=a2[:, l, kc, b:b + 1], bias=modT[:, l, 24 + kc, b:b + 1]), r=["ps4", "a2", "modT"], w=["h2T"])
                        else:
                            Q.op("dve", lambda kc=kc, src=src, t0=t0: V.tensor_scalar(out=h2T[:, kc, t0:t0 + 128], in0=src, scalar1=a2[:, l, kc, b:b + 1],
                                                                                 scalar2=modT[:, l, 24 + kc, b:b + 1], op0=ALU.mult, op1=ALU.add), r=["ps5", "a2", "modT"], w=["h2T"])
                    def mmr(t0=t0):
                        ins = None
                        for kc in range(8):
                            nc.tensor.matmul(PS[6][:, 0:NEXP], lhsT=h2T[:, kc, t0:t0 + 128], rhs=rwb[:, kc, :], start=(kc == 0), stop=False)
                            ins = nc.tensor.matmul(PS[6][:, 0:NEXP], lhsT=h2T[:, kc, t0:t0 + 128], rhs=rwl[:, kc, :], start=False, stop=(kc == 7))
                        return ins
                    Q.op("pe", mmr, r=["h2T", "rwb", "rwl"], w=["ps6"])
                    Q.op("act", lambda tt=tt: A_.activation(out=RSC[:, tt, :], in_=PS[6][:, 0:NEXP], func=AF.Sigmoid), r=["ps6"], w=["r_sc"])
                    erecs.append(Q.stages)
                emit_ops(P, erecs[0]["E1"])
                if NT > 1:
                    emit_ops(P, erecs[1]["E1"])
                emit_ops(P, erecs[0]["E1b"])
                for tt in range(NT):
                    if tt + 2 < NT:
                        emit_ops(P, erecs[tt + 2]["E1"])
                    emit_ops(P, interleave_ops(erecs[tt]["E2"], erecs[tt + 1]["E1b"] if tt + 1 < NT else []))
                G4 = NT * 4
                TTm = lambda o, a_, b_, op: V.tensor_tensor(out=o, in0=a_, in1=b_, op=op)
                sc2 = RSC[:].rearrange("p t e -> p (t e)")
                bi2 = RBI[:].rearrange("p t e -> p (t e)")
                P.op("dve", lambda: TTm(RBI[:], RSC[:], bcast_mid(rbias[:], NT), ALU.add), r=["r_sc", "rbias"], w=["r_bi"])
                v4 = RBI[:].rearrange("p t (g e) -> p (t g) e", e=4)
                rt = lambda i: RTM[:, i, :]
                P.op("dve", lambda: TTm(rt(0), v4[:, :, 0], v4[:, :, 1], ALU.max), r=["r_bi"], w=["r1"])
                P.op("dve", lambda: TTm(rt(1), v4[:, :, 0], v4[:, :, 1], ALU.min), r=["r_bi"], w=["r2"])
                P.op("dve", lambda: TTm(rt(2), v4[:, :, 2], v4[:, :, 3], ALU.max), r=["r_bi"], w=["r3"])
                P.op("dve", lambda: TTm(rt(3), v4[:, :, 2], v4[:, :, 3], ALU.min), r=["r_bi"], w=["r4"])
                P.op("dve", lambda: TTm(rt(4), rt(0), rt(2), ALU.max), r=["r1", "r3"], w=["r5"])
                P.op("dve", lambda: TTm(rt(5), rt(0), rt(2), ALU.min), r=["r1", "r3"], w=["r6"])
                P.op("dve", lambda: TTm(rt(6), rt(1), rt(3), ALU.max), r=["r2", "r4"], w=["r7"])
                P.op("dve", lambda: TTm(rt(7), rt(5), rt(6), ALU.max), r=["r6", "r7"], w=["r8"])
                P.op("dve", lambda: TTm(rt(8), rt(4), rt(7), ALU.add), r=["r5", "r8"], w=["r9"])
                P.op("dve", lambda: V.tensor_reduce(out=RT1[:, 0, :], in_=rt(8).rearrange("p (t g) -> p t g", g=4), axis=AX.X, op=ALU.max), r=["r9"], w=["r10"])
                P.op("dve", lambda: TTm(rt(9).rearrange("p (t g) -> p t g", g=4), rt(8).rearrange("p (t g) -> p t g", g=4), bcast_last(RT1[:, 0, :], 4), ALU.is_ge),
                     r=["r9", "r10"], w=["r11"])
                P.op("dve", lambda: TTm(rt(10), rt(9), rt(7), ALU.mult), r=["r11", "r8"], w=["r12"])
                P.op("dve", lambda: V.tensor_reduce(out=RT1[:, 1, :], in_=rt(10).rearrange("p (t g) -> p t g", g=4), axis=AX.X, op=ALU.add), r=["r12"], w=["r13"])
                P.op("dve", lambda: TTm(RSEL[:], RBI[:], bcast_last(RT1[:, 1, :], NEXP), ALU.is_ge), r=["r_bi", "r13"], w=["r14"])
                P.op("dve", lambda: TTm(RSEL[:].rearrange("p t (g e) -> p (t g) e", e=4), RSEL[:].rearrange("p t (g e) -> p (t g) e", e=4), bcast_last(rt(9), 4), ALU.mult),
                     r=["r14", "r11"], w=["r14"])
                P.op("dve", lambda: TTm(RSEL[:], RSEL[:], RSC[:], ALU.mult), r=["r14", "r_sc"], w=["r14"])
                P.op("dve", lambda: V.tensor_reduce(out=RT1[:, 2, :], in_=RSEL[:], axis=AX.X, op=ALU.add), r=["r14"], w=["r15"])
                P.op("dve", lambda: V.reciprocal(out=RT1[:, 2, :], in_=RT1[:, 2, :]), r=["r15"], w=["r15"])
                P.op("dve", lambda: TTm(GATE[:], RSEL[:], bcast_last(RT1[:, 2, :], NEXP), ALU.mult), r=["r14", "r15"], w=["GATE"])
                P.flush()
            with ExitStack() as es2:
                wg = [T(es2, "wg%d" % i, [128, 8, FF], BF16) for i in range(2)]
                wu = [T(es2, "wu%d" % i, [128, 8, FF], BF16) for i in range(2)]
                wd = [T(es2, "wd%d" % i, [128, 4, D], BF16) for i in range(2)]
                stg = [T(es2, "stg%d" % i, [128, 4096], F32) for i in range(2)]
                actT = [T(es2, "actT%d" % i, [128, 4, 512], BF16) for i in range(2)]
                sil = [T(es2, "sil%d" % i, [128, 512], F32) for i in range(2)]
                P.op("dve", lambda: V.memset(yacc[:], 0.0), w=["yacc"])
                P.dma("sp", gbc[:], MODS[l, b:b + 1, 5120:6144].partition_broadcast(128), r=["MODS"], w=["gbc"], key="gbc")
                si = 0
                ci = 0
                mrecs = []
                for e in range(NEL):
                    ws = e % 2
                    Q = RecQ()
                    Q.stage("W")
                    for (dst, src, nm) in ((wg[ws], I["exp_w_gate"][l, e].rearrange("(kc p) f -> p kc f", p=128), "wg%d" % ws),
                                           (wu[ws], I["exp_w_up"][l, e].rearrange("(kc p) f -> p kc f", p=128), "wu%d" % ws),
                                           (wd[ws], I["exp_w_down"][l, e].rearrange("(fc p) d -> p fc d", p=128), "wd%d" % ws)):
                        k_ = si % 2
                        si += 1
                        st_ = stg[k_]
                        sv = st_[:].rearrange("p (a f) -> p a f", a=dst.shape[1])
                        Q.dma("sp", sv, src, w=["stg%d" % k_], key="stg%d" % k_)
                        Q.op("pool", lambda dst=dst, sv=sv: G.tensor_copy(out=dst[:], in_=sv), r=["stg%d" % k_], w=[nm])
                    for tq in range(4):
                        if tq > 0:
                            Q = RecQ()
                            Q.stage("W")
                        mrecs.append(Q.stages)
                        tq0 = tq * 512
                        at = actT[ci % 2]
                        atn = "actT%d" % (ci % 2)
                        ci += 1
                        for fc in range(4):
                            Q.stage("G%d" % fc)

                            def mmgu(fc=fc, ws=ws, tq0=tq0):
                                ins = None
                                for kc in range(8):
                                    ins = nc.tensor.matmul(PS[fc % 2][:], lhsT=wg[ws][:, kc, fc * 128:(fc + 1) * 128], rhs=h2T[:, kc, tq0:tq0 + 512], start=(kc == 0), stop=(kc == 7))
                                for kc in range(8):
                                    ins = nc.tensor.matmul(PS[2 + fc % 2][:], lhsT=wu[ws][:, kc, fc * 128:(fc + 1) * 128], rhs=h2T[:, kc, tq0:tq0 + 512], start=(kc == 0), stop=(kc == 7))
                                return ins
                            Q.op("pe", mmgu, r=["wg%d" % ws, "wu%d" % ws, "h2T"], w=["ps%d" % (fc % 2), "ps%d" % (2 + fc % 2)])
                            sl_ = sil[fc % 2]
                            Q.op("act", lambda fc=fc, sl_=sl_: A_.activation(out=sl_[:], in_=PS[fc % 2][:], func=AF.Silu), r=["ps%d" % (fc % 2)], w=["sil%d" % (fc % 2)])
                            Q.op("dve", lambda fc=fc, sl_=sl_, at=at: V.tensor_tensor(out=at[:, fc, :], in0=sl_[:], in1=PS[2 + fc % 2][:], op=ALU.mult),
                                 r=["sil%d" % (fc % 2), "ps%d" % (2 + fc % 2)], w=[atn + "_%d" % fc])
                        Q.stage("D")
                        for ts in range(4):
                            tt = tq * 4 + ts
                            for hf in range(2):
                                pb = 4 + (ts * 2 + hf) % 4

                                def mmd(at=at, ts=ts, hf=hf, pb=pb, ws=ws):
                                    ins = None
                                    for fc in range(4):
                                        ins = nc.tensor.matmul(PS[pb][:], lhsT=at[:, fc, ts * 128:(ts + 1) * 128], rhs=wd[ws][:, fc, hf * 512:(hf + 1) * 512], start=(fc == 0), stop=(fc == 3))
                                    return ins
                                Q.op("pe", mmd, r=[atn + "_%d" % fc for fc in range(4)] + ["wd%d" % ws], w=["ps%d" % pb])
                                Q.op("dve", lambda tt=tt, hf=hf, pb=pb, e=e: V.scalar_tensor_tensor(
                                    out=yacc[:, tt, hf * 512:(hf + 1) * 512], in0=PS[pb][:], scalar=GATE[:, tt, e:e + 1], in1=yacc[:, tt, hf * 512:(hf + 1) * 512],
                                    op0=ALU.mult, op1=ALU.add), r=["ps%d" % pb, "GATE", "yacc"], w=["yacc"])
                gm = lambda j, nm: mrecs[j].get(nm, [])
                nj_ = len(mrecs)
                emit_ops(P, gm(0, "W") + gm(0, "G0") + gm(0, "G1") + gm(0, "G2") + gm(0, "G3"))
                for j in range(nj_):
                    if j + 1 < nj_:
                        emit_ops(P, gm(j + 1, "W") + gm(j + 1, "G0"))
                    emit_ops(P, gm(j, "D"))
                    if j + 1 < nj_:
                        emit_ops(P, gm(j + 1, "G1") + gm(j + 1, "G2") + gm(j + 1, "G3"))
                for tt in range(NT):
                    sl = tt % 2
                    t0 = tt * 128
                    XA = xa[sl]
                    P.dma("sp", XA[:], X1[b, t0:t0 + 128, :], r=["X1"], w=["xa%d" % sl], key="xa%d" % sl)
                    P.op("dve", lambda tt=tt: V.tensor_tensor(out=yacc[:, tt, :], in0=yacc[:, tt, :], in1=gbc[:], op=ALU.mult), r=["yacc", "gbc"], w=["yacc"])
                    P.op("pool", lambda tt=tt, XA=XA: G.tensor_tensor(out=XA[:], in0=XA[:], in1=yacc[:, tt, :], op=ALU.add), r=["yacc", "xa%d" % sl], w=["xa%d" % sl])
                    P.dma("pool", xdst[b, t0:t0 + 128, :], XA[:], r=["xa%d" % sl], w=["X2"], key="xs%d" % sl)
                P.flush()


def build(debug=None):
    nc = bass.Bass("TRN2", target_bir_lowering=False)
    dt_in = lambda name, shape: nc.dram_tensor(name, list(shape), F32, kind="ExternalInput").ap()
    I = {}
    I["x"] = dt_in("x", [NB, S, D])
    I["cT"] = dt_in("cT", [128, 8, NB])
    I["ada_w"] = dt_in("ada_w", [DEPTH, D, 6 * D])
    I["ada_bT"] = dt_in("ada_bT", [DEPTH, 128, 48])
    I["ada_b"] = dt_in("ada_b", [DEPTH, 1, 6 * D])
    I["norm1_gT"] = dt_in("norm1_gT", [DEPTH, 128, 8])
    I["norm2_gT"] = dt_in("norm2_gT", [DEPTH, 128, 8])
    I["w_in"] = dt_in("w_in", [DEPTH, D, INC])
    I["gdn_conv_w"] = dt_in("gdn_conv_w", [DEPTH, 1, 4 * 1536])
    I["gdn_a_log"] = dt_in("gdn_a_log", [DEPTH, 1, 8])
    I["gdn_dt_bias"] = dt_in("gdn_dt_bias", [DEPTH, 1, 8])
    I["gdn_norm_g"] = dt_in("gdn_norm_g", [DEPTH, 1, 64])
    I["nsa_q_norm_g"] = dt_in("nsa_q_norm_g", [DEPTH, 1, 64])
    I["nsa_k_norm_g"] = dt_in("nsa_k_norm_g", [DEPTH, 1, 192])
    I["cmp_peT"] = dt_in("cmp_peT", [DEPTH, 2, 64, 32])
    I["cmp_w1"] = dt_in("cmp_w1", [DEPTH, 2, 2048, 256])
    I["cmp_b1T"] = dt_in("cmp_b1T", [DEPTH, 2, 128, 2])
    I["cmp_w2"] = dt_in("cmp_w2", [DEPTH, 2, 256, 64])
    I["cmp_b2"] = dt_in("cmp_b2", [DEPTH, 2, 1, 64])
    I["w_out"] = dt_in("w_out", [DEPTH, D, D])
    I["router_w"] = dt_in("router_w", [D, NEXP])
    I["router_bias"] = dt_in("router_bias", [1, NEXP])
    I["exp_w_gate"] = dt_in("exp_w_gate", [DEPTH, NEXP, D, FF])
    I["exp_w_up"] = dt_in("exp_w_up", [DEPTH, NEXP, D, FF])
    I["exp_w_down"] = dt_in("exp_w_down", [DEPTH, NEXP, FF, D])
    for k, (shp, npdt) in CONST_SHAPES.items():
        if npdt == np.float32:
            I[k] = dt_in(k, shp)
        else:
            I[k] = nc.dram_tensor(k, list(shp), BF16, kind="ExternalInput").ap()
    out = nc.dram_tensor("out", [NB, S, D], F32, kind="ExternalOutput").ap()
    dbg = {}
    if debug:
        for name, shp in debug.items():
            if name.startswith("_"):
                continue
            dbg[name] = nc.dram_tensor("dbg_" + name, list(shp), F32, kind="ExternalOutput").ap()
    PROJ = nc.dram_tensor("PROJ", [NB, PADR + S, INC], F32, kind="Internal").ap()
    MODS = nc.dram_tensor("MODS", [DEPTH, NB, 6 * D], F32, kind="Internal").ap()
    YMIX = nc.dram_tensor("YMIX", [NB, S, D], F32, kind="Internal").ap()
    X1 = nc.dram_tensor("X1", [NB, S, D], F32, kind="Internal").ap()
    X2 = nc.dram_tensor("X2", [NB, S, D], F32, kind="Internal").ap()

    with ExitStack() as ges:
        P = Prog(nc, ges)
        _cnt = [0]

        def T(es, name, shape, dt):
            _cnt[0] += 1
            return es.enter_context(nc.sbuf_tensor("s%d_%s" % (_cnt[0], name), list(shape), dt))
        PS = [ges.enter_context(nc.psum_tensor("psb%d" % i, [128, 512], F32)) for i in range(8)]
        ident = T(ges, "ident", [128, 128], F32)
        identb = T(ges, "identb", [128, 128], BF16)
        condT = T(ges, "condT", [128, 8, NB], F32)
        modT = T(ges, "modT", [128, DEPTH, 48, NB], F32)
        a1 = T(ges, "a1", [128, DEPTH, 8, NB], F32)
        a2 = T(ges, "a2", [128, DEPTH, 8, NB], F32)
        epsc = T(ges, "epsc", [128, 4], F32)
        P.op("dve", lambda: nc.vector.memset(epsc[:, 0:1], EPS), w=["epsc"])
        P.op("dve", lambda: nc.vector.memset(epsc[:, 1:2], 1.0), w=["epsc"])
        P.op("dve", lambda: nc.vector.memset(epsc[:, 2:3], 0.0), w=["epsc"])
        P.op("dve", lambda: nc.vector.memset(epsc[:, 3:4], 1e-30), w=["epsc"])
        cU = T(ges, "cU", [128, 128], F32)
        cOnes = T(ges, "cOnes", [128, 128], F32)
        cBm = T(ges, "cBm", [128, 128], F32)
        cM2 = T(ges, "cM2", [128, 2, 128], F32)
        for nm, t in (("cU", cU), ("cOnes", cOnes), ("cBm", cBm), ("cM2", cM2)):
            P.dma("sp", t[:], I[nm], w=[nm], key="c0")
        P.dma("sp", ident[:], I["ident"], w=["ident"], key="c0")
        P.dma("sp", condT[:], I["cT"], w=["condT"], key="c0")
        P.op("dve", lambda: nc.vector.tensor_copy(out=identb[:], in_=ident[:]), r=["ident"], w=["identb"])
        P.op("act", lambda: nc.scalar.activation(out=condT[:], in_=condT[:], func=AF.Silu), r=["condT"], w=["condT"])
        P.flush()

        with ExitStack() as es:
            wst = [T(es, "adaw%d" % i, [128, 8, 512], F32) for i in range(2)]
            mrow = T(es, "mrow", [NB, 6 * D], F32)
            brow = T(es, "brow", [NB, 6 * D], F32)
            bT = T(es, "bT", [128, DEPTH, 48], F32)
            g1T = T(es, "g1T", [128, DEPTH, 8], F32)
            g2T = T(es, "g2T", [128, DEPTH, 8], F32)
            P.dma("sp", bT[:], I["ada_bT"].rearrange("l p c -> p l c"), w=["bT"], key="c1")
            P.dma("sp", g1T[:], I["norm1_gT"].rearrange("l p c -> p l c"), w=["g1T"], key="c1")
            P.dma("sp", g2T[:], I["norm2_gT"].rearrange("l p c -> p l c"), w=["g2T"], key="c1")
            gi = 0
            for l in range(DEPTH):
                P.dma("sp", brow[:], I["ada_b"][l].partition_broadcast(NB), w=["brow"], key="brow")
                for fg in range(12):
                    sl = gi % 2
                    gi += 1
                    wt = wst[sl]
                    src = I["ada_w"][l].rearrange("(kc p) f -> p kc f", p=128)[:, :, fg * 512:(fg + 1) * 512]
                    P.dma("sp" if sl == 0 else "pool", wt[:], src, w=["adaw%d" % sl], key="adaw%d" % sl)
                    psr = PS[sl * 2]
                    psc = PS[sl * 2 + 1]

                    def mm(wt=wt, psr=psr):
                        ins = None
                        for kc in range(8):
                            ins = nc.tensor.matmul(psr[0:NB, :], lhsT=condT[:, kc, :], rhs=wt[:, kc, :],
                                                   start=(kc == 0), stop=(kc == 7))
                        return ins
                    P.op("pe", mm, r=["adaw%d" % sl, "condT"], w=["ps%d" % (sl * 2)])
                    P.op("dve", lambda psr=psr, fg=fg: nc.vector.tensor_tensor(
                        out=mrow[:, fg * 512:(fg + 1) * 512], in0=psr[0:NB, :], in1=brow[:, fg * 512:(fg + 1) * 512], op=ALU.add),
                        r=["ps%d" % (sl * 2), "brow"], w=["mrow_%d" % fg])
                mnames = ["mrow_%d" % fg for fg in range(12)]

                def trm():
                    ins = None
                    for c_ in range(48):
                        ins = nc.tensor.transpose(out=PS[1][:, c_ * NB:(c_ + 1) * NB], in_=mrow[:, c_ * 128:(c_ + 1) * 128], identity=ident[0:NB, 0:NB])
                    return ins
                P.op("pe", trm, r=mnames + ["ident"], w=["ps1"])
                P.op("dve", lambda l=l: nc.vector.tensor_copy(out=modT[:, l].rearrange("p c b -> p (c b)"), in_=PS[1][:, 0:48 * NB]), r=["ps1"], w=["modT"])
                P.dma("sp", MODS[l], mrow[:], r=mnames, w=["MODS"], key="mods")
                P.op("dve", lambda l=l: nc.vector.scalar_tensor_tensor(
                    out=a1[:, l], in0=modT[:, l, 8:16, :], scalar=1.0, in1=bcast_last(g1T[:, l, :], NB),
                    op0=ALU.add, op1=ALU.mult), r=["modT", "g1T"], w=["a1"])
                P.op("dve", lambda l=l: nc.vector.scalar_tensor_tensor(
                    out=a2[:, l], in0=modT[:, l, 32:40, :], scalar=1.0, in1=bcast_last(g2T[:, l, :], NB),
                    op0=ALU.add, op1=ALU.mult), r=["modT", "g2T"], w=["a2"])
            P.flush()

        if debug and "mods" in debug:
            with ExitStack() as es:
                t = T(es, "dbgm", [NB, 6 * D], F32)
                P.dma("sp", t[:], MODS[0], r=["MODS"], w=["dbgm"], key="dbgm")
                P.dma("sp", dbg["mods"], t[:], r=["dbgm"], w=["dbgmo"], key="dbgmo")
                t2 = T(es, "dbgm2", [128, DEPTH * 48 * NB], F32)
                P.op("dve", lambda: nc.vector.tensor_copy(out=t2[:], in_=modT[:].rearrange("p l c b -> p (l c b)")), r=["modT"], w=["dbgm2"])
                P.dma("sp", dbg["modT"], t2[:], r=["dbgm2"], w=["dbgmo2"], key="dbgmo2")
                P.flush()
        for l in range(DEPTH):
            if debug and debug.get("_stop") == "A":
                break
            xsrc = I["x"] if l == 0 else X2
            xdst = X2 if l == 0 else out
            with ExitStack() as es:
                wbf = T(es, "winbf", [128, 8, INC], BF16)
                wstg = [T(es, "winst%d" % i, [128, 8, 512], F32) for i in range(2)]
                xt = [T(es, "xt%d" % i, [128, D], F32) for i in range(2)]
                junk = T(es, "junk", [128, D], F32)
                xn = [T(es, "xn%d" % i, [128, D], BF16) for i in range(2)]
                hT = [T(es, "hT%d" % i, [128, 8, 128], BF16) for i in range(2)]
                stage = [T(es, "stage%d" % i, [128, INC], F32) for i in range(2)]
                ss = T(es, "ss", [128, 4], F32)
                zpad = T(es, "zpad", [PADR, INC], F32)
                P.op("dve", lambda: nc.vector.memset(zpad[:], 0.0), w=["zpad"])
                for b in range(NB):
                    P.dma("sp", PROJ[b, 0:PADR, :], zpad[:], r=["zpad"], w=["PROJ"], key="zpad")
                for cg in range(7):
                    c0 = cg * 512
                    cw = min(512, INC - c0)
                    sl = cg % 2
                    P.dma("sp" if sl == 0 else "pool", wstg[sl][:, :, 0:cw], I["w_in"][l].rearrange("(kc p) f -> p kc f", p=128)[:, :, c0:c0 + cw],
                          w=["winst%d" % sl], key="winst%d" % sl)
                    if sl == 0:
                        P.op("pool", lambda sl=sl, c0=c0, cw=cw: nc.gpsimd.tensor_copy(out=wbf[:, :, c0:c0 + cw], in_=wstg[sl][:, :, 0:cw]),
                             r=["winst%d" % sl], w=["winbf%d" % cg])
                    else:
                        P.op("act", lambda sl=sl, c0=c0, cw=cw: nc.scalar.copy(out=wbf[:, :, c0:c0 + cw], in_=wstg[sl][:, :, 0:cw]),
                             r=["winst%d" % sl], w=["winbf%d" % cg])
                it = 0
                BM = (debug or {}).get("_bm", 9)
                brecs = []
                for b in range(NB):
                    for tt in range(NT):
                        sl = it % 2
                        it += 1
                        Q = RecQ()
                        brecs.append(Q.stages)
                        Q.stage("B1")
                        X, XN, HT, ST = xt[sl], xn[sl], hT[sl], stage[sl]
                        Q.dma("sp", X[:], xsrc[b, tt * 128:(tt + 1) * 128, :], w=["xt%d" % sl], key="xt%d" % sl)
                        Q.op("act", lambda X=X, sl=sl: nc.scalar.activation(out=junk[:], in_=X[:], func=AF.Square, accum_out=ss[:, sl:sl + 1]),
                             r=["xt%d" % sl], w=["junk", "ss%d" % sl])
                        Q.op("act", lambda sl=sl: nc.scalar.activation(out=ss[:, 2 + sl:3 + sl], in_=ss[:, sl:sl + 1], func=AF.Sqrt, scale=1.0 / D, bias=epsc[:, 0:1]),
                             r=["ss%d" % sl], w=["rs%d" % sl])
                        Q.op("dve", lambda sl=sl: nc.vector.reciprocal(out=ss[:, 2 + sl:3 + sl], in_=ss[:, 2 + sl:3 + sl]), r=["rs%d" % sl], w=["rs%d" % sl])
                        Q.op("dve", lambda X=X, XN=XN, sl=sl: nc.vector.tensor_scalar(out=XN[:], in0=X[:], scalar1=ss[:, 2 + sl:3 + sl], scalar2=None, op0=ALU.mult),
                             r=["xt%d" % sl, "rs%d" % sl], w=["xn%d" % sl])
                        tpa, tpb_ = PS[sl * 2], PS[sl * 2 + 1]

                        def tr(XN=XN, tpa=tpa, tpb_=tpb_):
                            ins = None
                            for kc in range(8):
                                tp = (tpa if kc < 4 else tpb_)[:].bitcast(BF16)
                                ins = nc.tensor.transpose(out=tp[:, (kc % 4) * 128:(kc % 4 + 1) * 128], in_=XN[:, kc * 128:(kc + 1) * 128], identity=identb[:])
                            return ins
                        Q.op("pe", tr, r=["xn%d" % sl, "identb"], w=["ps%d" % (sl * 2), "ps%d" % (sl * 2 + 1)])
                        for kc in range(8):
                            e = "act" if kc < 4 else "dve"
                            tp = tpa if kc < 4 else tpb_
                            if e == "act":
                                f = lambda HT=HT, tp=tp, kc=kc, b=b: nc.scalar.activation(
                                    out=HT[:, kc, :], in_=tp[:].bitcast(BF16)[:, (kc % 4) * 128:(kc % 4 + 1) * 128], func=AF.Identity,
                                    scale=a1[:, l, kc, b:b + 1], bias=modT[:, l, kc, b:b + 1])
                            else:
                                f = lambda HT=HT, tp=tp, kc=kc, b=b: nc.vector.tensor_scalar(
                                    out=HT[:, kc, :], in0=tp[:].bitcast(BF16)[:, (kc % 4) * 128:(kc % 4 + 1) * 128],
                                    scalar1=a1[:, l, kc, b:b + 1], scalar2=modT[:, l, kc, b:b + 1], op0=ALU.mult, op1=ALU.add)
                            Q.op(e, f, r=["ps%d" % (sl * 2 + (0 if kc < 4 else 1)), "a1", "modT"], w=["hT%d_%d" % (sl, kc)])
                        Q.stage("B2")
                        for cg in range(7):
                            c0 = cg * 512
                            cw = min(512, INC - c0)
                            pb = 4 + (cg % 4)
                            pt = PS[pb]

                            def mm(HT=HT, pt=pt, c0=c0, cw=cw):
                                ins = None
                                for kc in range(8):
                                    ins = nc.tensor.matmul(pt[:, 0:cw], lhsT=HT[:, kc, :], rhs=wbf[:, kc, c0:c0 + cw], start=(kc == 0), stop=(kc == 7))
                                return ins
                            Q.op("pe", mm, r=["hT%d_%d" % (sl, kc) for kc in range(8)] + ["winbf%d" % cg], w=["ps%d" % pb])
                            if cg % 2 == 0:
                                Q.op("act", lambda ST=ST, pt=pt, c0=c0, cw=cw: nc.scalar.copy(out=ST[:, c0:c0 + cw], in_=pt[:, 0:cw]),
                                     r=["ps%d" % pb], w=["stage%d" % sl])
                            else:
                                Q.op("dve", lambda ST=ST, pt=pt, c0=c0, cw=cw: nc.vector.tensor_copy(out=ST[:, c0:c0 + cw], in_=pt[:, 0:cw]),
                                     r=["ps%d" % pb], w=["stage%d" % sl])
                        Q.dma("pool", PROJ[b, PADR + tt * 128:PADR + (tt + 1) * 128, :], ST[:], r=["stage%d" % sl], w=["PROJ"], key="stage%d" % sl)
                emit_ops(P, brecs[0]["B1"])
                for i_ in range(len(brecs)):
                    emit_ops(P, interleave_ops(brecs[i_]["B2"], brecs[i_ + 1]["B1"] if i_ + 1 < len(brecs) else []))
                P.flush()
            if debug and "proj" in debug and l == debug.get("_layer", 0):
                with ExitStack() as es:
                    t = T(es, "dbgt", [128, INC], F32)
                    for tt in range(NT):
                        P.dma("sp", t[:], PROJ[0, PADR + tt * 128:PADR + (tt + 1) * 128, :], r=["PROJ"], w=["dbgt"], key="dbgt")
                        P.dma("sp", dbg["proj"][tt * 128:(tt + 1) * 128, :], t[:], r=["dbgt"], w=["dbgo"], key="dbgo")
                    P.flush()
            if debug and debug.get("_stop") == "B":
                break
            CC = dict(ident=ident, identb=identb, cU=cU, cOnes=cOnes, cBm=cBm, cM2=cM2, epsc=epsc)
            if not (debug and debug.get("_skip_gdn")):
                phase_gdn(nc, P, T, PS, I, l, PROJ, YMIX, CC, debug, dbg)
            if debug and debug.get("_stop") == "GDN":
                break
            if not (debug and debug.get("_skip_nsa")):
                phase_nsa(nc, P, T, PS, I, l, PROJ, YMIX, CC, debug, dbg)
            if debug and debug.get("_stop") == "NSA":
                break
            CC.update(modT=modT, a2=a2)
            phase_out_moe(nc, P, T, PS, I, l, YMIX, X1, xsrc, xdst, MODS, CC, debug, dbg)
            if debug and "x2" in debug:
                with ExitStack() as es:
                    t = T(es, "dbgx2", [128, D], F32)
                    for tt in range(NT):
                        P.dma("sp", t[:], X2[0, tt * 128:(tt + 1) * 128, :], r=["X2"], w=["dbgx2"], key="dbgx2")
                        P.dma("sp", dbg["x2"][tt * 128:(tt + 1) * 128, :], t[:], r=["dbgx2"], w=["dbgx2o"], key="dbgx2o")
                    P.flush()
            if debug and debug.get("_stop") == "L0":
                break
        P.flush()
        print("instructions emitted:", P.nins)
        if os.environ.get("SEMDBG"):
            print({v.name if hasattr(v, "name") else str(v): k for k, v in P.dsem.items()})
            print(list(P.dsem.keys()))
    return nc


def prep_inputs(inputs, core):
    f = lambda a: np.ascontiguousarray(np.asarray(a, dtype=np.float32))
    b0 = core * NB
    m = {}
    m["x"] = f(inputs["x"][b0:b0 + NB])
    m["cT"] = f(np.asarray(inputs["c"])[b0:b0 + NB].reshape(NB, 8, 128).transpose(2, 1, 0))
    m["ada_w"] = f(inputs["ada_w"])
    m["ada_bT"] = f(np.asarray(inputs["ada_b"]).reshape(DEPTH, 48, 128).transpose(0, 2, 1))
    m["ada_b"] = f(np.asarray(inputs["ada_b"]).reshape(DEPTH, 1, 6 * D))
    m["norm1_gT"] = f(np.asarray(inputs["norm1_g"]).reshape(DEPTH, 8, 128).transpose(0, 2, 1))
    m["norm2_gT"] = f(np.asarray(inputs["norm2_g"]).reshape(DEPTH, 8, 128).transpose(0, 2, 1))
    m["w_in"] = f(inputs["w_in"])
    m["gdn_conv_w"] = f(np.asarray(inputs["gdn_conv_w"]).reshape(DEPTH, 1, 4 * 1536))
    m["gdn_a_log"] = f(np.asarray(inputs["gdn_a_log"]).reshape(DEPTH, 1, 8))
    m["gdn_dt_bias"] = f(np.asarray(inputs["gdn_dt_bias"]).reshape(DEPTH, 1, 8))
    m["gdn_norm_g"] = f(np.asarray(inputs["gdn_norm_g"]).reshape(DEPTH, 1, 64))
    m["nsa_q_norm_g"] = f(np.asarray(inputs["nsa_q_norm_g"]).reshape(DEPTH, 1, 64))
    m["nsa_k_norm_g"] = f(np.asarray(inputs["nsa_k_norm_g"]).reshape(DEPTH, 1, 192))
    m["cmp_peT"] = f(np.asarray(inputs["cmp_pe"]).transpose(0, 1, 3, 2))
    m["cmp_w1"] = f(inputs["cmp_w1"])
    m["cmp_b1T"] = f(np.asarray(inputs["cmp_b1"]).reshape(DEPTH, 2, 2, 128).transpose(0, 1, 3, 2))
    m["cmp_w2"] = f(inputs["cmp_w2"])
    m["cmp_b2"] = f(np.asarray(inputs["cmp_b2"]).reshape(DEPTH, 2, 1, 64))
    m["w_out"] = f(inputs["w_out"])
    m["router_w"] = f(inputs["router_w"])
    m["router_bias"] = f(np.asarray(inputs["router_bias"]).reshape(1, NEXP))
    m["exp_w_gate"] = f(inputs["exp_w_gate"])
    m["exp_w_up"] = f(inputs["exp_w_up"])
    m["exp_w_down"] = f(inputs["exp_w_down"])
    m.update(make_consts())
    return m


_NC_CACHE = {}


def kernel(**inputs):
    if "nc" not in _NC_CACHE:
        _NC_CACHE["nc"] = build()
    nc = _NC_CACHE["nc"]
    in_maps = [prep_inputs(inputs, c) for c in range(8)]
    res = run_bass_kernel_spmd(nc, in_maps, core_ids=list(range(8)))
    return np.concatenate([np.asarray(r["out"], dtype=np.float32) for r in res.results], axis=0)
```

```python
import os
from contextlib import ExitStack
import numpy as np
import ml_dtypes
import concourse.bass as bass
import concourse.mybir as mybir
from concourse.bass_utils import run_bass_kernel_spmd

F32 = mybir.dt.float32
BF16 = mybir.dt.bfloat16
AF = mybir.ActivationFunctionType
ALU = mybir.AluOpType
AX = mybir.AxisListType

D = 1024
S = 2048
NB = 2
NT = S // 128
DEPTH = 2
HD = 64
GW = 512
INC = 3368
NEXP = 16
FF = 512
EPS = 1e-6
NEG = -30000.0
C_Q, C_K, C_V, C_Z, C_A, C_B = 0, 512, 1024, 1536, 2048, 2056
C_NQ = 2064
C_KV = 2576
C_NG = 3344
PADR = 3


class Prog:
    ENG = ("pe", "act", "dve", "pool", "sp")

    def __init__(self, nc, es):
        self.nc = nc
        self.es = es
        self.eng = {"pe": nc.tensor, "act": nc.scalar, "dve": nc.vector, "pool": nc.gpsimd, "sp": nc.sync}
        self.esem = {e: es.enter_context(nc.semaphore("es_" + e)) for e in self.ENG}
        self.ecount = {e: 0 for e in self.ENG}
        self.eidx = {e: 0 for e in self.ENG}
        self.dsem = {}
        self.dcount = {}
        self.ops = []
        self.res = {}
        self.waited = {e: {} for e in self.ENG}
        self.nins = 0

    def _r(self, name):
        r = self.res.get(name)
        if r is None:
            r = {"w": {}, "r": {}}
            self.res[name] = r
        return r

    def _deps(self, me_eng, reads, writes, is_dma):
        deps = {}

        def add(p, st):
            if isinstance(p, tuple):
                st = self.dcount[p]
            if deps.get(p, -1) < st:
                deps[p] = st
        for n in reads:
            for p, st in self._r(n)["w"].items():
                add(p, st)
        for n in writes:
            rr = self._r(n)
            for p, st in rr["w"].items():
                if is_dma and isinstance(p, tuple):
                    continue
                add(p, st)
            for p, st in rr["r"].items():
                add(p, st)
        return deps

    def op(self, eng, fn, r=(), w=()):
        w = list(w) + [n for n in r if n.startswith("ps")]
        r = [n for n in r if not n.startswith("ps")]
        deps = self._deps(eng, r, w, False)
        idx = self.eidx[eng]
        self.eidx[eng] += 1
        if eng in deps:
            if eng == "pe" or idx - deps[eng] > 3:
                del deps[eng]
        o = {"eng": eng, "fn": fn, "deps": deps, "idx": idx, "inc": False, "dkey": None}
        self.ops.append(o)
        for n in r:
            self._r(n)["r"][eng] = idx
        for n in w:
            rr = self._r(n)
            rr["w"] = {eng: idx}
            rr["r"] = {}
        return o

    def dma(self, q, out, in_, r=(), w=(), key=None):
        assert key is not None
        deps = self._deps(q, r, w, True)
        deps.pop(q, None) if False else None
        idx = self.eidx[q]
        self.eidx[q] += 1
        if q in deps:
            if idx - deps[q] > 3:
                del deps[q]
        k = ("d", key)
        self.dcount[k] = self.dcount.get(k, 0) + 1
        st = self.dcount[k]
        o = {"eng": q, "fn": None, "dma": (out, in_), "deps": deps, "idx": idx, "inc": False, "dkey": k}
        self.ops.append(o)
        for n in r:
            self._r(n)["r"][k] = st
        for n in w:
            rr = self._r(n)
            rr["w"] = {p: s for p, s in rr["w"].items() if isinstance(p, tuple)}
            rr["w"][k] = st
            rr["r"] = {}
        return o

    def _sem_for(self, k):
        s = self.dsem.get(k)
        if s is None:
            s = self.es.enter_context(self.nc.semaphore("ds_%d" % len(self.dsem)))
            self.dsem[k] = s
        return s

    def flush(self, barrier=True):
        ops = self.ops
        byeng = {e: {} for e in self.ENG}
        for o in ops:
            if o["dkey"] is None:
                byeng[o["eng"]][o["idx"]] = o
        for o in ops:
            for p, st in o["deps"].items():
                if not isinstance(p, tuple):
                    t = byeng[p].get(st)
                    if t is not None:
                        t["inc"] = True
        last = {}
        for o in ops:
            if o["dkey"] is None:
                last[o["eng"]] = o
        if barrier:
            for o in last.values():
                o["inc"] = True
        cnt_of = {e: {} for e in self.ENG}
        run = dict(self.ecount)
        for o in ops:
            if o["dkey"] is None and o["inc"]:
                run[o["eng"]] += 1
                cnt_of[o["eng"]][o["idx"]] = run[o["eng"]]
        for o in ops:
            e = o["eng"]
            eo = self.eng[e]
            for p, st in o["deps"].items():
                if isinstance(p, tuple):
                    sem = self._sem_for(p)
                    val = 16 * st
                else:
                    if st not in cnt_of[p]:
                        continue
                    sem = self.esem[p]
                    val = cnt_of[p][st]
                if self.waited[e].get(p, 0) >= val:
                    continue
                self.waited[e][p] = val
                eo.wait_ge(sem, val)
                self.nins += 1
            if o["dkey"] is not None:
                out, in_ = o["dma"]
                eo.dma_start(out=out, in_=in_).then_inc(self._sem_for(o["dkey"]), 16)
            else:
                ins = o["fn"]()
                if o["inc"]:
                    ins.then_inc(self.esem[e], 1)
            self.nins += 1
        self.ecount = run
        if barrier:
            for e in self.ENG:
                eo = self.eng[e]
                for p in self.ENG:
                    if p != e and self.ecount[p] > self.waited[e].get(p, 0):
                        eo.wait_ge(self.esem[p], self.ecount[p])
                        self.waited[e][p] = self.ecount[p]
                for k, c in self.dcount.items():
                    if 16 * c > self.waited[e].get(k, 0):
                        eo.wait_ge(self._sem_for(k), 16 * c)
                        self.waited[e][k] = 16 * c
            self.res = {}
        self.ops = []


def make_consts():
    c = {}
    c["ident"] = np.eye(128, dtype=np.float32)
    m = np.arange(128)
    c["cU"] = (m[:, None] <= m[None, :]).astype(np.float32)
    c["cOnes"] = np.ones((128, 128), np.float32)
    c["cBm"] = (m[:, None] > m[None, :]).astype(np.float32)
    m2 = np.zeros((128, 2, 128), np.float32)
    m2[:, 0, :] = -(m[None, :] > m[:, None]).astype(np.float32)
    m2[:, 1, :] = (m[None, :] >= m[:, None]).astype(np.float32)
    c["cM2"] = m2
    slopes = (2.0 ** (-np.arange(1, 9))).astype(np.float32)
    qa_sw = np.zeros((3, 2, 4, 128), np.float32)
    qa_c = np.zeros((3, 2, 4, 128), np.float32)
    for g in range(2):
        for r in range(4):
            sp_ = slopes[g * 4 + r]
            qa_sw[0, g, r] = sp_
            qa_sw[1, g, r] = 128 * sp_
            qa_c[0, g, r] = 16 * sp_
            qa_c[1, g, r] = 31 * sp_
            qa_c[2, g, r] = -128 * sp_
    c["nQAc"] = qa_c.reshape(3, 2, 512)
    ka_sw = np.zeros((3, 16, 128), np.float32)
    ka_c = np.zeros((3, 16, 128), np.float32)
    for dl in range(16):
        ka_sw[0, dl] = m
        ka_sw[1, dl] = -dl
        ka_c[0, dl] = m
        ka_c[1, dl] = 1
        ka_c[2, dl] = dl
    c["nKAc"] = ka_c
    tpos = (np.arange(16)[:, None] * 128 + m[None, :])[None]
    cend = (16 * m + 31)[:, None, None]
    negc = np.where((tpos >= cend) & (m[:, None, None] < 127), 0.0, NEG).astype(np.float32)
    c["nNEGc"] = negc
    c["nCMd"] = np.where(m[:, None] <= m[None, :], 0.0, NEG).astype(np.float32)
    c["nCMw4"] = np.where(m[:, None] > m[None, :], 0.0, NEG).astype(np.float32)
    e = np.zeros((32, 16, 128), np.float32)
    for kt in range(16):
        for p in range(128):
            e[2 * kt + p // 64, kt, p] = 1.0
    n_cmp = 127
    cs = np.arange(n_cmp)[:, None] * 16
    ss_ = np.arange(32)[None, :] * 64
    ov = np.clip(np.minimum(cs + 32, ss_ + 64) - np.maximum(cs, ss_), 0, None) / 32.0
    ovp = np.zeros((128, 32), np.float32)
    ovp[:127] = ov
    c["nOV"] = ovp
    pos = np.arange(2048)
    blk = np.arange(32)[None, :]
    cur = (pos // 64)[:, None]
    valid = blk <= cur
    forced = (blk == 0) | (blk == cur) | (blk == cur - 1)
    cv = np.where(forced | ~valid, 0.0, 1.0).astype(np.float32)
    cb = np.where(forced, 1e9, np.where(valid, 0.0, -1e9)).astype(np.float32)
    tpos1 = np.arange(2048)
    qrows = np.zeros((3, 8, 2048), np.float32)
    for h in range(8):
        qrows[0, h] = slopes[h]
        qrows[1, h] = 128 * slopes[h]
        qrows[2, h] = -128.0 * (tpos1 // 128) * slopes[h]
    krows = np.zeros((35, 2048), np.float32)
    krows[0] = tpos1 % 128
    krows[1] = tpos1 // 128
    krows[2] = 1.0
    for j in range(32):
        krows[3 + j] = (tpos1 // 64 == j)
    c["nQrows"] = qrows.astype(ml_dtypes.bfloat16)
    c["nKrows"] = krows.astype(ml_dtypes.bfloat16)
    c["nCV"] = cv.reshape(16, 128, 32).transpose(1, 0, 2).copy()
    c["nCB"] = cb.reshape(16, 128, 32).transpose(1, 0, 2).copy()
    return c


CONST_SHAPES = {k: (v.shape, v.dtype) for k, v in make_consts().items()}


def bcast_mid(ap, n):
    return ap.unsqueeze(1).to_broadcast([ap.shape[0], n, ap.shape[1]])


def bcast_last(ap, n):
    return ap.unsqueeze(2).to_broadcast([ap.shape[0], ap.shape[1], n])


class RecQ:
    def __init__(self):
        self.stages, self.cur = {}, None

    def stage(self, name):
        self.cur = self.stages.setdefault(name, [])

    def op(self, eng, fn, r=(), w=()):
        self.cur.append(("op", eng, fn, list(r), list(w), None))

    def dma(self, q_, out, in_, r=(), w=(), key=None):
        self.cur.append(("dma", q_, (out, in_), list(r), list(w), key))


def emit_ops(P, ops):
    for (kind, eng, fn, r, w, key) in ops:
        if kind == "op":
            P.op(eng, fn, r=r, w=w)
        else:
            P.dma(eng, fn[0], fn[1], r=r, w=w, key=key)


def interleave_ops(A, B):
    if not B:
        return list(A)
    if not A:
        return list(B)
    out, j = [], 0
    for i, a in enumerate(A):
        out.append(a)
        want = (i + 1) * len(B) // len(A)
        while j < want:
            out.append(B[j])
            j += 1
    out.extend(B[j:])
    return out


def phase_gdn(nc, P, T, PS, I, l, PROJ, YMIX, C, debug, dbg):
    identb, cU, cOnes, cBm, cM2, epsc = C["identb"], C["cU"], C["cOnes"], C["cBm"], C["cM2"], C["epsc"]
    V, A_, G = nc.vector, nc.scalar, nc.gpsimd
    NTL = (debug or {}).get("_gdn_tiles", NT)
    NBL = (debug or {}).get("_gdn_nb", NB)
    with ExitStack() as es:
        wc = T(es, "wc", [128, 4, 1536], F32)
        dtb = T(es, "dtb", [128, 8], F32)
        nea = T(es, "nea", [128, 8], F32)
        gng = T(es, "gng", [128, 64], F32)
        XS = [T(es, "XS%d" % i, [128, 4, 1536], F32) for i in range(2)]
        ZAB = [T(es, "ZAB%d" % i, [128, 528], F32) for i in range(4)]
        QKV = T(es, "QKV", [128, 1536], F32)
        junk = T(es, "gjunk", [128, 1024], F32)
        SM = [T(es, "gsm%d" % i, [128, 96], F32) for i in range(2)]
        qn32 = T(es, "qn32", [128, 8, 64], F32)
        kn32 = T(es, "kn32", [128, 8, 64], F32)
        OPBs = [T(es, "OPB%d" % i, [128, 7, 8, 64], BF16) for i in range(2)]
        TTs = [T(es, "TT%d" % i, [128, 4, 4, 128], BF16) for i in range(2)]
        AH = T(es, "AH", [128, 8, 128], F32)
        DT_ = T(es, "DT", [128, 8, 128], F32)
        DM = T(es, "DM", [128, 8, 2, 128], F32)
        N32 = [T(es, "N32%d" % i, [128, 128], F32) for i in range(8)]
        N1T = [T(es, "N1T%d" % i, [128, 128], F32) for i in range(8)]
        QK = [T(es, "QK%d" % i, [128, 128], BF16) for i in range(8)]
        NK = [[T(es, "NK%d_%d" % (i, j), [128, 128], F32) for j in range(2)] for i in range(8)]
        NKT = [[T(es, "NKT%d_%d" % (i, j), [128, 128], F32) for j in range(2)] for i in range(8)]
        PK = [[T(es, "PK%d_%d" % (i, j), [128, 128], F32) for j in range(2)] for i in range(8)]
        TTB = [T(es, "TTB%d" % i, [128, 128], BF16) for i in range(8)]
        UH = [T(es, "UH%d" % i, [128, 64], F32) for i in range(8)]
        WT = [T(es, "WT%d" % i, [128, 128], BF16) for i in range(8)]
        VN = [T(es, "VN%d" % i, [128, 64], BF16) for i in range(8)]
        O = T(es, "O", [128, 8, 64], F32)
        YG = [T(es, "YG%d" % i, [128, 512], F32) for i in range(2)]
        SZ = T(es, "SZ", [128, 512], F32)
        SS = T(es, "SS", [128, 4, 64], F32)
        SB = T(es, "SB", [128, 4, 64], BF16)
        SSQ, RN, BETA, SP, GRAW, GG, EG, DG, EK, OSS, ORS, RQ8 = (slice(0, 16), slice(16, 32), slice(32, 40), slice(40, 48), slice(48, 56),
                                                                 slice(56, 72), slice(72, 88), None, slice(88, 96), None, None, None)
        SM2 = [T(es, "gsm2%d" % i, [128, 32], F32) for i in range(2)]
        P.dma("sp", wc[:].rearrange("p k c -> p (k c)"), I["gdn_conv_w"][l].partition_broadcast(128), w=["wc"], key="gc")
        P.dma("sp", dtb[:], I["gdn_dt_bias"][l].partition_broadcast(128), w=["dtb"], key="gc")
        P.dma("sp", nea[:], I["gdn_a_log"][l].partition_broadcast(128), w=["nea"], key="gc")
        P.dma("sp", gng[:], I["gdn_norm_g"][l].partition_broadcast(128), w=["gng"], key="gc")
        P.op("act", lambda: A_.activation(out=nea[:], in_=nea[:], func=AF.Exp), r=["nea"], w=["nea"])
        P.op("dve", lambda: V.tensor_scalar(out=nea[:], in0=nea[:], scalar1=-1.0, scalar2=None, op0=ALU.mult), r=["nea"], w=["nea"])
        DBL = set(["ssq", "rn", "rq8", "beta", "sp", "graw", "gg", "eg", "dg", "ek", "oss", "ors", "TT01", "TT23"] + ["opb%d" % i for i in range(7)])

        class Rec:
            def __init__(self, q):
                self.q, self.stages, self.cur = q, {}, None

            def stage(self, name):
                self.cur = self.stages.setdefault(name, [])

            def _nm(self, names):
                return [(n + "_q%d" % self.q) if n in DBL else n for n in names]

            def op(self, eng, fn, r=(), w=()):
                self.cur.append(("op", eng, fn, self._nm(r), self._nm(w), None))

            def dma(self, q_, out, in_, r=(), w=(), key=None):
                self.cur.append(("dma", q_, (out, in_), self._nm(r), self._nm(w), key))

        def emit(ops):
            for (kind, eng, fn, r, w, key) in ops:
                if kind == "op":
                    P.op(eng, fn, r=r, w=w)
                else:
                    P.dma(eng, fn[0], fn[1], r=r, w=w, key=key)

        def tile_body(b, tt, sl, Q):
            sm, sm2, OPB, TT = SM[sl], SM2[sl], OPBs[sl], TTs[sl]
            t0 = tt * 128
            zs = tt % 4
            X, Z = XS[sl], ZAB[zs]
            Q.stage("L")
            for k in range(4):
                Q.dma("sp", X[:, k, :], PROJ[b, t0 + k:t0 + k + 128, 0:1536], r=["PROJ"], w=["XS%d" % sl], key="XS%d" % sl)
            Q.dma("sp", Z[:], PROJ[b, PADR + t0:PADR + t0 + 128, 1536:2064], r=["PROJ"], w=["ZAB%d" % zs], key="ZAB%d" % zs)
            Q.stage("P1")
            Q.op("pool", lambda X=X: G.tensor_tensor(out=X[:, 0:2, :], in0=X[:, 0:2, :], in1=wc[:, 0:2, :], op=ALU.mult), r=["XS%d" % sl, "wc"], w=["XS%da" % sl])
            Q.op("dve", lambda X=X: V.tensor_tensor(out=X[:, 2:4, :], in0=X[:, 2:4, :], in1=wc[:, 2:4, :], op=ALU.mult), r=["XS%d" % sl, "wc"], w=["XS%db" % sl])
            Q.op("dve", lambda X=X: V.tensor_tensor(out=X[:, 0:2, :], in0=X[:, 0:2, :], in1=X[:, 2:4, :], op=ALU.add), r=["XS%da" % sl, "XS%db" % sl], w=["XS%d" % sl, "XS%da" % sl, "XS%db" % sl])
            Q.op("dve", lambda X=X: V.tensor_tensor(out=X[:, 0, :], in0=X[:, 0, :], in1=X[:, 1, :], op=ALU.add), r=["XS%d" % sl], w=["XS%d" % sl])
            Q.op("act", lambda X=X: A_.activation(out=QKV[:], in_=X[:, 0, :], func=AF.Silu), r=["XS%d" % sl], w=["QKV"])
            Q.op("dve", lambda: V.tensor_tensor(out=junk[:], in0=QKV[:, 0:1024], in1=QKV[:, 0:1024], op=ALU.mult), r=["QKV"], w=["gjunk"])
            Q.op("dve", lambda: V.tensor_reduce(out=sm[:, SSQ], in_=junk[:].rearrange("p (g d) -> p g d", d=64), axis=AX.X, op=ALU.add), r=["gjunk"], w=["ssq"])
            Q.op("act", lambda: A_.activation(out=sm[:, RN], in_=sm[:, SSQ], func=AF.Sqrt, bias=epsc[:, 0:1]), r=["ssq", "epsc"], w=["rn"])
            Q.op("dve", lambda: V.reciprocal(out=sm[:, RN], in_=sm[:, RN]), r=["rn"], w=["rn"])
            Q.op("dve", lambda: V.tensor_scalar(out=sm2[:, 24:32], in0=sm[:, 16:24], scalar1=0.125, scalar2=None, op0=ALU.mult), r=["rn"], w=["rq8"])
            Q.op("act", lambda Z=Z: A_.activation(out=sm[:, BETA], in_=Z[:, 520:528], func=AF.Sigmoid), r=["ZAB%d" % zs], w=["beta"])
            Q.op("dve", lambda Z=Z: V.tensor_tensor(out=sm[:, SP], in0=Z[:, 512:520], in1=dtb[:], op=ALU.add), r=["ZAB%d" % zs, "dtb"], w=["sp"])
            Q.op("act", lambda: A_.activation(out=sm[:, SP], in_=sm[:, SP], func=AF.Exp), r=["sp"], w=["sp"])
            Q.op("act", lambda: A_.activation(out=sm[:, SP], in_=sm[:, SP], func=AF.Ln, bias=epsc[:, 1:2]), r=["sp", "epsc"], w=["sp"])
            Q.op("dve", lambda: V.tensor_tensor(out=sm[:, GRAW], in0=sm[:, SP], in1=nea[:], op=ALU.mult), r=["sp", "nea"], w=["graw"])
            Q.stage("P2")
            def mmg():
                nc.tensor.matmul(PS[6][:, 0:8], lhsT=cU[:], rhs=sm[:, GRAW], start=True, stop=True)
                return nc.tensor.matmul(PS[6][:, 8:16], lhsT=cOnes[:], rhs=sm[:, GRAW], start=True, stop=True)
            Q.op("pe", mmg, r=["graw", "cU", "cOnes"], w=["ps6"])
            Q.op("dve", lambda: V.tensor_copy(out=sm[:, GG], in_=PS[6][:, 0:16]), r=["ps6"], w=["gg"])
            Q.op("act", lambda: A_.activation(out=sm[:, EG], in_=sm[:, GG], func=AF.Exp), r=["gg"], w=["eg"])
            Q.op("dve", lambda: V.tensor_tensor(out=sm2[:, 0:8], in0=sm[:, 64:72], in1=sm[:, 56:64], op=ALU.subtract), r=["gg"], w=["dg"])
            Q.op("act", lambda: A_.activation(out=sm[:, EK], in_=sm2[:, 0:8], func=AF.Exp), r=["dg"], w=["ek"])
            Q3 = QKV[:, 0:512].rearrange("p (h d) -> p h d", d=64)
            K3 = QKV[:, 512:1024].rearrange("p (h d) -> p h d", d=64)
            V3 = QKV[:, 1024:1536].rearrange("p (h d) -> p h d", d=64)
            bl = lambda sl_: bcast_last(sl_, 64)
            Q.op("dve", lambda: V.tensor_tensor(out=qn32[:], in0=Q3, in1=bl(sm2[:, 24:32]), op=ALU.mult), r=["QKV", "rq8"], w=["qn32"])
            Q.op("pool", lambda: G.tensor_tensor(out=kn32[:], in0=K3, in1=bl(sm[:, 24:32]), op=ALU.mult), r=["QKV", "rn"], w=["kn32"])
            Q.op("act", lambda: A_.copy(out=OPB[:, 2], in_=qn32[:]), r=["qn32"], w=["opb2"])
            Q.op("act", lambda: A_.copy(out=OPB[:, 0], in_=kn32[:]), r=["kn32"], w=["opb0"])
            Q.op("dve", lambda: V.tensor_tensor(out=OPB[:, 3], in0=qn32[:], in1=bl(sm[:, 72:80]), op=ALU.mult), r=["qn32", "eg"], w=["opb3"])
            Q.op("pool", lambda: G.tensor_tensor(out=kn32[:], in0=kn32[:], in1=bl(sm[:, 88:96]), op=ALU.mult) if False else G.tensor_tensor(out=OPB[:, 5], in0=kn32[:], in1=bl(sm[:, 88:96]), op=ALU.mult),
                 r=["kn32", "ek"], w=["opb5"])
            Q.op("dve", lambda: V.tensor_tensor(out=OPB[:, 6], in0=V3, in1=bl(sm[:, BETA]), op=ALU.mult), r=["QKV", "beta"], w=["opb6"])
            Q.op("pool", lambda: G.tensor_tensor(out=qn32[:], in0=kn32[:], in1=bl(sm[:, BETA]), op=ALU.mult), r=["kn32", "beta", "opb2", "opb3"], w=["qn32"])
            Q.op("act", lambda: A_.copy(out=OPB[:, 1], in_=qn32[:]), r=["qn32"], w=["opb1"])
            Q.op("dve", lambda: V.tensor_tensor(out=OPB[:, 4], in0=qn32[:], in1=bl(sm[:, 72:80]), op=ALU.mult), r=["qn32", "eg"], w=["opb4"])
            def trs():
                ins = None
                for p in range(4):
                    bank = PS[6 + p // 2][:].bitcast(BF16)
                    for kd in range(4):
                        col = ((p % 2) * 4 + kd) * 128
                        ins = nc.tensor.transpose(out=bank[:, col:col + 128],
                                                  in_=OPB[:, kd, 2 * p:2 * p + 2, :].rearrange("p a d -> p (a d)"), identity=identb[:])
                return ins
            Q.op("pe", trs, r=["opb0", "opb1", "opb2", "opb3", "identb"], w=["ps6", "ps7"])
            Q.op("act", lambda: A_.copy(out=TT[:, 0:2].rearrange("p a k t -> p (a k t)"), in_=PS[6][:].bitcast(BF16)), r=["ps6"], w=["TT01"])
            Q.op("dve", lambda: V.tensor_copy(out=TT[:, 2:4].rearrange("p a k t -> p (a k t)"), in_=PS[7][:].bitcast(BF16)), r=["ps7"], w=["TT23"])
            Q.op("dve", lambda: V.tensor_tensor(out=AH[:], in0=bcast_mid(cU[:], 8), in1=bcast_last(sm[:, GRAW], 128), op=ALU.mult), r=["cU", "graw"], w=["AH"])

            def mmG():
                nc.tensor.matmul(PS[6][:], lhsT=cBm[:], rhs=AH[:, 0:4, :].rearrange("p h i -> p (h i)"), start=True, stop=True)
                return nc.tensor.matmul(PS[7][:], lhsT=cBm[:], rhs=AH[:, 4:8, :].rearrange("p h i -> p (h i)"), start=True, stop=True)
            Q.op("pe", mmG, r=["AH", "cBm"], w=["ps6", "ps7"])
            Q.op("act", lambda: A_.activation(out=DT_[:, 0:4].rearrange("p h i -> p (h i)"), in_=PS[6][:], func=AF.Exp), r=["ps6"], w=["DT0"])
            Q.op("act", lambda: A_.activation(out=DT_[:, 4:8].rearrange("p h i -> p (h i)"), in_=PS[7][:], func=AF.Exp), r=["ps7"], w=["DT1"])
            Q.op("dve", lambda: V.tensor_tensor(out=DM[:, :, 0, :], in0=DT_[:], in1=bcast_mid(cM2[:, 0, :], 8), op=ALU.mult), r=["DT0", "DT1", "cM2"], w=["DM0"])
            Q.op("pool", lambda: G.tensor_tensor(out=DM[:, :, 1, :], in0=DT_[:], in1=bcast_mid(cM2[:, 1, :], 8), op=ALU.mult), r=["DT0", "DT1", "cM2"], w=["DM1"])
            ident = C["ident"]
            HS = list(range(8))
            hp_ = lambda h: (h % 2) * 64
            RI = lambda h: (h % 2) + 2 * (h // 4)
            RB = lambda h: PS[RI(h)]
            RC = lambda h: ((h // 2) % 2) * 256
            RN_ = lambda h: "ps%d" % RI(h)
            PB = lambda h: PS[4 + h % 2]
            PC = lambda h: (h // 2) * 128
            PN = lambda h: "ps%d" % (4 + h % 2)
            ER = lambda h: "act" if h % 2 == 0 else "dve"
            EP = lambda h: "dve" if h % 2 == 0 else "act"

            def cp(eng, out, in_, r, w):
                if eng == "act":
                    Q.op("act", lambda: A_.copy(out=out, in_=in_), r=r, w=w)
                else:
                    Q.op("dve", lambda: V.tensor_copy(out=out, in_=in_), r=r, w=w)
            Q.stage("S2")
            if (debug or {}).get("_gs", 9) < 2:
                HS = []
            for h in HS:
                Q.op("pe", lambda h=h: nc.tensor.matmul(
                    RB(h)[:, RC(h):RC(h) + 256], lhsT=TT[hp_(h):hp_(h) + 64, h // 2, 0, :], rhs=TT[hp_(h):hp_(h) + 64, h // 2, 1:3, :].rearrange("p k t -> p (k t)"),
                    start=True, stop=True), r=["TT01", "TT23"], w=[RN_(h)])
            for h in HS:
                Q.op("dve", lambda h=h: V.tensor_tensor(out=N32[h][:], in0=RB(h)[:, RC(h):RC(h) + 128], in1=DM[:, h, 0, :], op=ALU.mult),
                     r=[RN_(h), "DM0"], w=["N32_%d" % h])
                Q.op("dve", lambda h=h: V.tensor_tensor(out=QK[h][:], in0=RB(h)[:, RC(h) + 128:RC(h) + 256], in1=DM[:, h, 1, :], op=ALU.mult),
                     r=[RN_(h), "DM1"], w=["QK_%d" % h])
            for h in (HS if not os.environ.get("K2") else []):
                Q.op("pe", lambda h=h: nc.tensor.transpose(out=PB(h)[:, PC(h):PC(h) + 128], in_=N32[h][:], identity=ident[:]),
                     r=["N32_%d" % h, "ident"], w=[PN(h)])
            for h in HS:
                if os.environ.get("K2"):
                    continue
                cp(EP(h), N1T[h][:], PB(h)[:, PC(h):PC(h) + 128], [PN(h)], ["N1T_%d" % h])
                Q.op("pool", lambda h=h: G.tensor_tensor(out=PK[h][0][:], in0=N32[h][:], in1=ident[:], op=ALU.add),
                     r=["N32_%d" % h, "ident"], w=["PK%d_0" % h])
            for h in (HS if not os.environ.get("K1") else []):
                Q.op("pe", lambda h=h: nc.tensor.matmul(PB(h)[:, PC(h):PC(h) + 128], lhsT=ident[:], rhs=PK[h][0][:], start=(h < 2), stop=True, skip_group_check=True),
                     r=["PK%d_0" % h, "ident"], w=[PN(h)])
            GS = (debug or {}).get("_gs", 9)
            def emitP(k):
                for h in HS:
                    Q.op("pe", lambda h=h, k=k: nc.tensor.matmul(PB(h)[:, PC(h):PC(h) + 128], lhsT=NKT[h][k % 2][:], rhs=PK[h][(k - 1) % 2][:],
                                                              start=False, stop=True, skip_group_check=True),
                         r=["NKT%d_%d" % (h, k % 2), "PK%d_%d" % (h, (k - 1) % 2)], w=[PN(h)])
                for h in HS:
                    dst = TTB[h] if k == 6 else PK[h][k % 2]
                    dn = ("TTB%d" % h) if k == 6 else ("PK%d_%d" % (h, k % 2))
                    cp(EP(h), dst[:], PB(h)[:, PC(h):PC(h) + 128], [PN(h)], [dn])

            for k in range(1, 7 if GS >= 3 else 1):
                Q.stage("S3_%d" % k)
                last = (k == 6)
                cur = {}
                for h in HS:
                    if k == 1:
                        cur[h] = (N32[h][:], N1T[h][:], ["N32_%d" % h, "N1T_%d" % h])
                    else:
                        cur[h] = (NK[h][(k - 1) % 2][:], NKT[h][(k - 1) % 2][:], ["NK%d_%d" % (h, (k - 1) % 2), "NKT%d_%d" % (h, (k - 1) % 2)])
                if not last:
                    for h in HS:
                        cN, cNT, rn_ = cur[h]
                        Q.op("pe", lambda h=h, cN=cN, cNT=cNT: nc.tensor.matmul(RB(h)[:, RC(h):RC(h) + 128], lhsT=cNT, rhs=cN, start=True, stop=True), r=rn_, w=[RN_(h)])
                    for h in HS:
                        cp(ER(h), NK[h][k % 2][:], RB(h)[:, RC(h):RC(h) + 128], [RN_(h)], ["NK%d_%d" % (h, k % 2)])
                    for h in HS:
                        Q.op("pe", lambda h=h, k=k: nc.tensor.transpose(out=RB(h)[:, RC(h) + 128:RC(h) + 256], in_=NK[h][k % 2][:], identity=ident[:]),
                             r=["NK%d_%d" % (h, k % 2), "ident"], w=[RN_(h)])
                else:
                    for h in HS:
                        cN, cNT, rn_ = cur[h]
                        Q.op("pe", lambda h=h, cN=cN, cNT=cNT: nc.tensor.matmul(RB(h)[:, RC(h) + 128:RC(h) + 256], lhsT=cN, rhs=cNT, start=True, stop=True), r=rn_, w=[RN_(h)])
                for h in HS:
                    cp(ER(h), NKT[h][k % 2][:], RB(h)[:, RC(h) + 128:RC(h) + 256], [RN_(h)], ["NKT%d_%d" % (h, k % 2)])
                if k >= 2:
                    emitP(k - 1)
                if last:
                    emitP(k)
            Q.stage("S4")
            if GS < 4:
                HS = []
            for h in HS:
                def mmu(h=h):
                    pr = h // 2
                    nc.tensor.matmul(RB(h)[:, RC(h):RC(h) + 64], lhsT=TTB[h][:], rhs=OPB[:, 6, h, :], start=True, stop=True)
                    return nc.tensor.matmul(RB(h)[:, RC(h) + 64:RC(h) + 192], lhsT=OPB[:, 4, 2 * pr:2 * pr + 2, :].rearrange("p a d -> p (a d)"), rhs=TTB[h][:], start=True, stop=True)
                Q.op("pe", mmu, r=["TTB%d" % h, "opb6", "opb4"], w=[RN_(h)])
            for h in HS:
                cp(ER(h), UH[h][:], RB(h)[:, RC(h):RC(h) + 64], [RN_(h)], ["UH_%d" % h])
                cp(ER(h), WT[h][:], RB(h)[:, RC(h) + 64:RC(h) + 192], [RN_(h)], ["WT_%d" % h])
            for h in HS:
                Q.op("pe", lambda h=h: nc.tensor.matmul(RB(h)[:, RC(h) + 192:RC(h) + 256], lhsT=WT[h][hp_(h):hp_(h) + 64, :], rhs=SB[hp_(h):hp_(h) + 64, h // 2, :], start=True, stop=True),
                     r=["WT_%d" % h, "SB%d" % h], w=[RN_(h)])
            for h in HS:
                Q.op("dve", lambda h=h: V.tensor_tensor(out=VN[h][:], in0=UH[h][:], in1=RB(h)[:, RC(h) + 192:RC(h) + 256], op=ALU.subtract), r=["UH_%d" % h, RN_(h)], w=["VN_%d" % h])
            for h in HS:
                def mmo(h=h):
                    pr, hp = h // 2, hp_(h)
                    nc.tensor.matmul(PB(h)[:, PC(h):PC(h) + 64], lhsT=TT[hp:hp + 64, pr, 3, :], rhs=SB[hp:hp + 64, pr, :], start=True, stop=False)
                    nc.tensor.matmul(PB(h)[:, PC(h):PC(h) + 64], lhsT=QK[h][:], rhs=VN[h][:], start=False, stop=True)
                    return nc.tensor.matmul(PB(h)[:, PC(h) + 64:PC(h) + 128], lhsT=OPB[:, 5, 2 * pr:2 * pr + 2, :].rearrange("p a d -> p (a d)"), rhs=VN[h][:], start=True, stop=True)
                Q.op("pe", mmo, r=["TT01", "TT23", "SB%d" % h, "QK_%d" % h, "VN_%d" % h, "opb5"], w=[PN(h)])
            for h in HS:
                Q.op("act", lambda h=h: A_.copy(out=O[:, h, :], in_=PB(h)[:, PC(h):PC(h) + 64]), r=[PN(h)], w=["O%d" % h])
                Q.op("dve", lambda h=h: V.scalar_tensor_tensor(
                    out=SS[hp_(h):hp_(h) + 64, h // 2, :], in0=SS[hp_(h):hp_(h) + 64, h // 2, :], scalar=sm[hp_(h):hp_(h) + 64, 80 + h:81 + h],
                    in1=PB(h)[hp_(h):hp_(h) + 64, PC(h) + 64:PC(h) + 128], op0=ALU.mult, op1=ALU.add), r=[PN(h), "eg", "SS"], w=["SS%d" % h])
            for h in HS:
                Q.op("pool", lambda h=h: G.tensor_copy(out=SB[hp_(h):hp_(h) + 64, h // 2, :], in_=SS[hp_(h):hp_(h) + 64, h // 2, :]), r=["SS%d" % h, "SB"], w=["SB%d" % h])
            Q.stage("FIN")
            yg = YG[sl]
            O2 = O[:].rearrange("p h d -> p (h d)")
            Q.op("dve", lambda: V.tensor_tensor(out=junk[:, 0:512], in0=O2, in1=O2, op=ALU.mult), r=["O%d" % h for h in range(8)], w=["gjunk"])
            Q.op("dve", lambda: V.tensor_reduce(out=sm2[:, 8:16], in_=junk[:, 0:512].rearrange("p (g d) -> p g d", d=64), axis=AX.X, op=ALU.add), r=["gjunk"], w=["oss"])
            Q.op("act", lambda: A_.activation(out=sm2[:, 16:24], in_=sm2[:, 8:16], func=AF.Sqrt, scale=1.0 / 64, bias=epsc[:, 0:1]), r=["oss", "epsc"], w=["ors"])
            Q.op("dve", lambda: V.reciprocal(out=sm2[:, 16:24], in_=sm2[:, 16:24]), r=["ors"], w=["ors"])
            Q.op("act", lambda Z=Z: A_.activation(out=SZ[:], in_=Z[:, 0:512], func=AF.Silu), r=["ZAB%d" % zs], w=["SZ"])
            Q.op("dve", lambda yg=yg: V.tensor_tensor(out=yg[:].rearrange("p (h d) -> p h d", d=64), in0=O[:], in1=bcast_last(sm2[:, 16:24], 64), op=ALU.mult),
                 r=["O%d" % h for h in range(8)] + ["ors"], w=["YG%d" % sl])
            Q.op("pool", lambda yg=yg: G.tensor_tensor(out=yg[:].rearrange("p (h d) -> p h d", d=64), in0=yg[:].rearrange("p (h d) -> p h d", d=64),
                                                      in1=bcast_mid(gng[:], 8), op=ALU.mult), r=["YG%d" % sl, "gng"], w=["YG%d" % sl])
            Q.op("dve", lambda yg=yg: V.tensor_tensor(out=yg[:], in0=yg[:], in1=SZ[:], op=ALU.mult), r=["YG%d" % sl, "SZ"], w=["YG%d" % sl])
            Q.dma("sp", YMIX[b, t0:t0 + 128, 0:512], yg[:], r=["YG%d" % sl], w=["YMIX"], key="YG%d" % sl)
        it = 0
        for b in range(NBL):
            P.op("dve", lambda: V.memset(SS[:], 0.0), w=["SS"])
            P.op("dve", lambda: V.memset(SB[:], 0.0), w=["SB"])
            recs = []
            for tt in range(NTL):
                sl = it % 2
                it += 1
                Q = Rec(sl)
                tile_body(b, tt, sl, Q)
                recs.append(Q.stages)
            g_ = lambda st, nm: st.get(nm, [])
            emit(g_(recs[0], "L"))
            if NTL > 1:
                emit(g_(recs[1], "L"))
            emit(g_(recs[0], "P1"))
            emit(g_(recs[0], "P2"))
            def interleave(A, B):
                if not B:
                    return list(A)
                if not A:
                    return list(B)
                out, j = [], 0
                for i, a in enumerate(A):
                    out.append(a)
                    want = (i + 1) * len(B) // len(A)
                    while j < want:
                        out.append(B[j])
                        j += 1
                out.extend(B[j:])
                return out

            for tt in range(NTL):
                nxt = recs[tt + 1] if tt + 1 < NTL else {}
                if tt + 2 < NTL:
                    emit(g_(recs[tt + 2], "L"))
                head = g_(recs[tt], "S2") + g_(recs[tt], "S3_1")
                emit(interleave(head, g_(recs[tt - 1], "FIN") if tt > 0 else []))
                mid = []
                for k in (2, 3, 4):
                    mid += g_(recs[tt], "S3_%d" % k)
                emit(interleave(mid, g_(nxt, "P1")))
                tail = g_(recs[tt], "S3_5") + g_(recs[tt], "S3_6") + g_(recs[tt], "S4")
                emit(interleave(tail, g_(nxt, "P2")))
                if tt == NTL - 1:
                    emit(g_(recs[tt], "FIN"))
        P.flush()
    if debug and "ygdn" in debug:
        with ExitStack() as es:
            t = T(es, "dbgy", [128, 512], F32)
            for tt in range(NTL):
                P.dma("sp", t[:], YMIX[0, tt * 128:(tt + 1) * 128, 0:512], r=["YMIX"], w=["dbgy"], key="dbgy")
                P.dma("sp", dbg["ygdn"][tt * 128:(tt + 1) * 128, :], t[:], r=["dbgy"], w=["dbgyo"], key="dbgyo")
            P.flush()


def phase_nsa(nc, P, T, PS, I, l, PROJ, YMIX, C, debug, dbg):
    identb, epsc, ident = C["identb"], C["epsc"], C["ident"]
    V, A_, G = nc.vector, nc.scalar, nc.gpsimd
    NTL = (debug or {}).get("_nsa_tiles", NT)
    NBL = (debug or {}).get("_nsa_nb", NB)
    with ExitStack() as es:
        es0 = ExitStack()
        specs = [("QAc", [3, 2, 512], "nQAc", BF16),
                 ("KAc", [3, 16, 128], "nKAc", BF16), ("NEGc", [128, 16, 128], "nNEGc", BF16), ("CMd", [128, 128], "nCMd", BF16),
                 ("CMw4", [128, 128], "nCMw4", BF16), ("OV", [128, 32], "nOV", F32),
                 ("CV", [128, 16, 32], "nCV", F32), ("CB", [128, 16, 32], "nCB", F32)]
        ct = {}
        for (name, shape, src, dt) in specs:
            ct[name] = T(es, name, shape, dt)
        QAc, KAc, NEGc, CMd, CMw4, OV, CV, CB = [ct[sp[0]] for sp in specs]
        gq = T(es, "gq", [128, 64], F32)
        gk = T(es, "gk", [128, 3, 64], F32)
        b2 = T(es, "b2", [128, 2, 64], F32)
        b1T = T(es, "b1T", [128, 2, 2], F32)
        P.dma("sp", gq[:], I["nsa_q_norm_g"][l].partition_broadcast(128), w=["gq"], key="nc2")
        P.dma("sp", gk[:].rearrange("p a d -> p (a d)"), I["nsa_k_norm_g"][l].partition_broadcast(128), w=["gk"], key="nc2")
        for kv in range(2):
            P.dma("sp", b2[:, kv, :], I["cmp_b2"][l, kv].partition_broadcast(128), w=["b2"], key="nc2")
        P.dma("sp", b1T[:], I["cmp_b1T"][l].rearrange("k p c -> p k c"), w=["b1T"], key="nc2")
        P.op("dve", lambda: V.tensor_scalar(out=gq[:], in0=gq[:], scalar1=0.125, scalar2=None, op0=ALU.mult), r=["gq"], w=["gq"])
        W1 = T(es, "W1", [64, 2, 32, 256], BF16)
        W2 = T(es, "W2", [128, 2, 2, 64], BF16)
        peT = T(es, "peT", [64, 2, 32], BF16)
        bias1 = T(es, "bias1", [128, 2, 2], F32)
        for (name, shape, src, dt) in specs:
            if dt == F32:
                P.dma("sp", ct[name][:], I[src], w=[name + "32"], key="nc_" + name)
            else:
                t32 = T(es0, name + "32", shape, F32)
                P.dma("sp", t32[:], I[src], w=[name + "32"], key="nc_" + name)
                P.op("pool", lambda t=ct[name], t32=t32: G.tensor_copy(out=t[:], in_=t32[:]), r=[name + "32"], w=[name])
        w1s = T(es0, "w1s", [64, 8, 256], F32)
        w2s = T(es0, "w2s", [128, 2, 2, 64], F32)
        pes = T(es0, "pes", [64, 2, 32], F32)
        for kv in range(2):
            for q4 in range(4):
                P.dma("sp", w1s[:], I["cmp_w1"][l, kv].rearrange("(l d) h -> d l h", d=64)[:, q4 * 8:(q4 + 1) * 8, :], r=[], w=["w1s"], key="w1s")
                P.op("pool", lambda kv=kv, q4=q4: G.tensor_copy(out=W1[:, kv, q4 * 8:(q4 + 1) * 8, :], in_=w1s[:]), r=["w1s"], w=["W1"])
            P.dma("sp", w2s[:, kv], I["cmp_w2"][l, kv].rearrange("(c p) d -> p c d", p=128), w=["w2s"], key="w2s")
            P.dma("sp", pes[:, kv, :], I["cmp_peT"][l, kv], w=["pes"], key="w2s")
        P.op("pool", lambda: G.tensor_copy(out=W2[:], in_=w2s[:]), r=["w2s"], w=["W2"])
        P.op("pool", lambda: G.tensor_copy(out=peT[:], in_=pes[:]), r=["pes"], w=["peT"])

        def mmpe():
            ins = None
            for kv in range(2):
                for hc in range(2):
                    col = kv * 2 + hc
                    for ll in range(32):
                        ins = nc.tensor.matmul(PS[0][:, col:col + 1], lhsT=W1[:, kv, ll, hc * 128:(hc + 1) * 128], rhs=peT[:, kv, ll:ll + 1],
                                               start=(ll == 0), stop=(ll == 31))
            return ins
        P.op("pe", mmpe, r=["W1", "peT"], w=["ps0"])
        P.op("dve", lambda: V.tensor_tensor(out=bias1[:].rearrange("p a b -> p (a b)"), in0=PS[0][:, 0:4], in1=b1T[:].rearrange("p a b -> p (a b)"), op=ALU.add),
             r=["ps0", "b1T"], w=["bias1"])
        P.flush()
        es0.close()
        qT = T(es, "qT", [99, 8, S], BF16)
        kT = T(es, "kT", [99, 2, 2, S], BF16)
        P.dma("sp", qT[64:67, :, :], I["nQrows"], w=["qTaug"], key="qTaug")
        for br_ in range(2):
            for g_ in range(2):
                P.dma("sp", kT[64:99, br_, g_, :], I["nKrows"], w=["kTaug"], key="kTaug")
        cT_ = T(es, "cT", [64, 2, 2, S], BF16)
        VA = T(es, "VA", [128, NT, 2, 2, 65], BF16)
        SG = T(es, "SG", [128, NT, 24], F32)
        YN = T(es, "YN", [128, NT, 8, 64], F32)
        IMP = T(es, "IMP", [128, NT, 2, 32], F32)
        selbT = T(es, "selbT", [32, 2, S], BF16)
        kcT = T(es, "kcT", [64, 2, 128], BF16)
        vcA = T(es, "vcA", [128, 2, 97], BF16)
        hid = T(es, "hid", [128, 2, 128], BF16)
        NIN = [T(es, "NIN%d" % i, [128, 1304], F32) for i in range(2)]
        NJs = [T(es, "nj%d" % i, [128, 896], F32) for i in range(2)]
        NSMs = [T(es, "nsm%d" % i, [128, 64], F32) for i in range(2)]
        NB16s = [T(es, "NB16%d" % i, [128, 16, 64], BF16) for i in range(2)]
        nj, nsm, NB16 = NJs[0], NSMs[0], NB16s[0]
        PT = [T(es, "PT%d" % i, [128, 4, 128], BF16) for i in range(6)]
        FIN = [T(es, "fin%d" % i, [128, 4, 64], F32) for i in range(2)]
        FSM = [T(es, "fsm%d" % i, [128, 56], F32) for i in range(2)]
        SELB = T(es, "SELB", [128, 2, NT, 32], BF16)
        P.op("dve", lambda: V.memset(VA[:, :, :, :, 64:65], 1.0), w=["VA"])
        it = 0
        for b in range(NBL):
            def prep_tile(tt, sl, Q):
                nj, nsm, NB16 = NJs[sl], NSMs[sl], NB16s[sl]
                Q.stage("N1")
                t0 = tt * 128
                X = NIN[sl]
                Q.dma("sp", X[:], PROJ[b, PADR + t0:PADR + t0 + 128, C_NQ:INC], r=["PROJ"], w=["NIN%d" % sl], key="NIN%d" % sl)
                Q.op("dve", lambda X=X: V.tensor_tensor(out=nj[:, 0:512], in0=X[:, 0:512], in1=X[:, 0:512], op=ALU.mult), r=["NIN%d" % sl], w=["nj_%d" % sl])
                Q.op("pool", lambda X=X: G.tensor_tensor(out=nj[:, 512:640], in0=X[:, 768:896], in1=X[:, 768:896], op=ALU.mult), r=["NIN%d" % sl], w=["nj2_%d" % sl])
                Q.op("pool", lambda X=X: G.tensor_tensor(out=nj[:, 640:768], in0=X[:, 1024:1152], in1=X[:, 1024:1152], op=ALU.mult), r=["NIN%d" % sl], w=["nj3_%d" % sl])
                Q.op("dve", lambda: V.tensor_reduce(out=nsm[:, 0:12], in_=nj[:, 0:768].rearrange("p (g d) -> p g d", d=64), axis=AX.X, op=ALU.add),
                     r=["nj_%d" % sl, "nj2_%d" % sl, "nj3_%d" % sl], w=["nss_%d" % sl])
                Q.op("act", lambda: A_.activation(out=nsm[:, 16:28], in_=nsm[:, 0:12], func=AF.Sqrt, scale=1.0 / 64, bias=epsc[:, 0:1]), r=["nss_%d" % sl, "epsc"], w=["nrs_%d" % sl])
                Q.op("dve", lambda: V.reciprocal(out=nsm[:, 16:28], in_=nsm[:, 16:28]), r=["nrs_%d" % sl], w=["nrs_%d" % sl])
                Xq = X[:, 0:512].rearrange("p (h d) -> p h d", d=64)
                Q.op("dve", lambda Xq=Xq: V.tensor_tensor(out=nj[:, 0:512].rearrange("p (h d) -> p h d", d=64), in0=Xq, in1=bcast_last(nsm[:, 16:24], 64), op=ALU.mult),
                     r=["NIN%d" % sl, "nrs_%d" % sl, "nss_%d" % sl], w=["nj_%d" % sl])
                Q.op("dve", lambda: V.tensor_tensor(out=NB16[:, 0:8, :], in0=nj[:, 0:512].rearrange("p (h d) -> p h d", d=64), in1=bcast_mid(gq[:], 8), op=ALU.mult),
                     r=["nj_%d" % sl, "gq"], w=["NB16q_%d" % sl])
                for bi, (c0, rs0) in enumerate(((768, 24), (1024, 26))):
                    Xk = X[:, c0:c0 + 128].rearrange("p (h d) -> p h d", d=64)
                    Q.op("pool", lambda Xk=Xk, bi=bi, rs0=rs0: G.tensor_tensor(out=nj[:, 768 + bi * 64 * 0:768 + 128].rearrange("p (h d) -> p h d", d=64) if False else nj[:, 768:896].rearrange("p (h d) -> p h d", d=64),
                                                                           in0=Xk, in1=bcast_last(nsm[:, rs0:rs0 + 2], 64), op=ALU.mult),
                         r=["NIN%d" % sl, "nrs_%d" % sl, "nss_%d" % sl], w=["njk_%d" % sl])
                    Q.op("pool", lambda bi=bi: G.tensor_tensor(out=NB16[:, 8 + 2 * bi:10 + 2 * bi, :], in0=nj[:, 768:896].rearrange("p (h d) -> p h d", d=64),
                                                           in1=bcast_mid(gk[:, 1 + bi, :], 2), op=ALU.mult), r=["njk_%d" % sl, "gk"], w=["NB16k%d_%d" % (bi, sl)])
                Q.op("act", lambda X=X: A_.copy(out=NB16[:, 12:14, :].rearrange("p h d -> p (h d)"), in_=X[:, 512:640]), r=["NIN%d" % sl], w=["NB16c_%d" % sl])
                Q.op("act", lambda X=X: A_.copy(out=NB16[:, 14:16, :].rearrange("p h d -> p (h d)"), in_=X[:, 640:768]), r=["NIN%d" % sl], w=["NB16v_%d" % sl])
                Q.op("act", lambda X=X, tt=tt: A_.copy(out=VA[:, tt, 0, :, 0:64], in_=X[:, 896:1024].rearrange("p (g d) -> p g d", d=64)), r=["NIN%d" % sl], w=["VA"])
                Q.op("act", lambda X=X, tt=tt: A_.copy(out=VA[:, tt, 1, :, 0:64], in_=X[:, 1152:1280].rearrange("p (g d) -> p g d", d=64)), r=["NIN%d" % sl], w=["VA"])
                Q.op("act", lambda X=X, tt=tt: A_.activation(out=SG[:, tt, :], in_=X[:, 1280:1304], func=AF.Sigmoid), r=["NIN%d" % sl], w=["SG"])

                Q.stage("N2")

                def trs():
                    ins = None
                    for j in range(16):
                        bank = PS[1 + 2 * sl + j // 8][:].bitcast(BF16)
                        ins = nc.tensor.transpose(out=bank[0:64, (j % 8) * 128:(j % 8 + 1) * 128], in_=NB16[:, j, :], identity=identb[:])
                    return ins
                Q.op("pe", trs, r=["NB16q_%d" % sl, "NB16k0_%d" % sl, "NB16k1_%d" % sl, "NB16c_%d" % sl, "NB16v_%d" % sl, "identb"], w=["ps%d" % (1 + 2 * sl), "ps%d" % (2 + 2 * sl)])
                Q.op("act", lambda t0=t0: A_.copy(out=qT[0:64, :, t0:t0 + 128], in_=PS[1 + 2 * sl][:].bitcast(BF16)[0:64, :].rearrange("p (h t) -> p h t", t=128)), r=["ps%d" % (1 + 2 * sl)], w=["qT"])
                Q.op("dve", lambda t0=t0: V.tensor_copy(out=kT[0:64, :, :, t0:t0 + 128].rearrange("p a g t -> p (a g) t"),
                                                        in_=PS[2 + 2 * sl][:].bitcast(BF16)[0:64, 0:512].rearrange("p (h t) -> p h t", t=128)), r=["ps%d" % (2 + 2 * sl)], w=["kT"])
                Q.op("dve", lambda t0=t0: V.tensor_copy(out=cT_[:, :, :, t0:t0 + 128].rearrange("p a g t -> p (a g) t"),
                                                        in_=PS[2 + 2 * sl][:].bitcast(BF16)[0:64, 512:1024].rearrange("p (h t) -> p h t", t=128)), r=["ps%d" % (2 + 2 * sl)], w=["cT"])

            precs = []
            for tt in range(NTL):
                sl = it % 2
                it += 1
                Q = RecQ()
                prep_tile(tt, sl, Q)
                precs.append(Q.stages)
            emit_ops(P, precs[0]["N1"])
            for tt in range(NTL):
                emit_ops(P, interleave_ops(precs[tt]["N2"], precs[tt + 1]["N1"] if tt + 1 < NTL else []))
            P.flush()
            if NTL < NT:
                continue
            for kv in range(2):
                for g in range(2):
                    for hc in range(2):
                        def mm1(kv=kv, g=g, hc=hc):
                            ins = None
                            for ll in range(32):
                                ins = nc.tensor.matmul(PS[3][:, 0:127], lhsT=W1[:, kv, ll, hc * 128:(hc + 1) * 128],
                                                       rhs=cT_[:, kv, g, ll:ll + 16 * 126 + 1:16], start=(ll == 0), stop=(ll == 31))
                            return ins
                        P.op("pe", mm1, r=["W1", "cT"], w=["ps3"])
                        P.op("act", lambda kv=kv, hc=hc: A_.activation(out=hid[:, hc, 0:127], in_=PS[3][:, 0:127], func=AF.Silu, bias=bias1[:, kv, hc:hc + 1]),
                             r=["ps3", "bias1"], w=["hid%d" % hc])

                    def mm2(kv=kv):
                        ins = None
                        for hc in range(2):
                            ins = nc.tensor.matmul(PS[4][0:127, 0:64], lhsT=hid[:, hc, 0:127], rhs=W2[:, kv, hc, :], start=(hc == 0), stop=(hc == 1))
                        return ins
                    P.op("pe", mm2, r=["hid0", "hid1", "W2"], w=["ps4"])
                    if kv == 0:
                        P.op("dve", lambda: V.memset(nj[:, 0:64], 0.0), w=["nj"])
                        P.op("dve", lambda: V.tensor_tensor(out=nj[0:127, 0:64], in0=PS[4][0:127, 0:64], in1=b2[0:127, 0, :], op=ALU.add), r=["ps4", "b2"], w=["nj"])
                        P.op("dve", lambda: V.tensor_tensor(out=nj[:, 64:128], in0=nj[:, 0:64], in1=nj[:, 0:64], op=ALU.mult), r=["nj"], w=["nj2"])
                        P.op("dve", lambda: V.tensor_reduce(out=nsm[:, 32:33], in_=nj[:, 64:128], axis=AX.X, op=ALU.add), r=["nj2"], w=["nss"])
                        P.op("act", lambda: A_.activation(out=nsm[:, 33:34], in_=nsm[:, 32:33], func=AF.Sqrt, scale=1.0 / 64, bias=epsc[:, 0:1]), r=["nss", "epsc"], w=["nrs"])
                        P.op("dve", lambda: V.reciprocal(out=nsm[:, 33:34], in_=nsm[:, 33:34]), r=["nrs"], w=["nrs"])
                        P.op("dve", lambda: V.scalar_tensor_tensor(out=NB16[:, 0, :], in0=nj[:, 0:64], scalar=nsm[:, 33:34], in1=gk[:, 0, :], op0=ALU.mult, op1=ALU.mult),
                             r=["nj", "nrs", "gk"], w=["NB16q"])
                        P.op("pe", lambda: nc.tensor.transpose(out=PS[4][:].bitcast(BF16)[0:64, 512:640], in_=NB16[:, 0, :], identity=identb[:]), r=["NB16q", "identb"], w=["ps4"])
                        P.op("dve", lambda g=g: V.tensor_copy(out=kcT[:, g, :], in_=PS[4][:].bitcast(BF16)[0:64, 512:640]), r=["ps4"], w=["kcT"])
                    else:
                        P.op("dve", lambda g=g: V.memset(vcA[:, g, 0:64], 0.0), w=["vcA"])
                        P.op("dve", lambda g=g: V.tensor_tensor(out=vcA[0:127, g, 0:64], in0=PS[4][0:127, 0:64], in1=b2[0:127, 1, :], op=ALU.add), r=["ps4", "b2"], w=["vcA"])
                        P.op("dve", lambda g=g: V.memset(vcA[:, g, 64:65], 1.0), w=["vcA"])
                        P.op("dve", lambda g=g: V.tensor_copy(out=vcA[:, g, 65:97], in_=OV[:]), r=["OV32"], w=["vcA"])

            cnt = [0]
            SK = 3

            def add_attend(jobs, g, tt, kts, branch, post):
                t0 = tt * 128
                acc_i = 6 + (cnt[0] % 2)
                cnt[0] += 1
                acc, accn = PS[acc_i], "ps%d" % acc_i
                W = 97 if branch == 0 else 65
                for ki, kt in enumerate(kts):
                    dl = tt - kt

                    def mms(sb, kt=kt, dl=dl):
                        if branch == 0:
                            full = [(kcT[:, g, :], qT[0:64, 4 * g:4 * g + 4, t0:t0 + 128]), (KAc[:, tt, :], QAc[:, g, :])]
                            masks = [(identb[:], NEGc[:, tt, :])]
                        else:
                            kk = 99 if branch == 1 else 67
                            full = [(kT[0:kk, branch - 1, g, kt * 128:(kt + 1) * 128], qT[0:kk, 4 * g:4 * g + 4, t0:t0 + 128])]
                            masks = []
                            if dl == 0:
                                masks.append((identb[:], CMd[:]))
                            if branch == 2 and dl == 4:
                                masks.append((identb[:], CMw4[:]))
                        ins = None
                        for fi, (lt, rh) in enumerate(full):
                            ins = nc.tensor.matmul(sb[:], lhsT=lt, rhs=rh, start=(fi == 0), stop=(fi == len(full) - 1 and not masks), skip_group_check=True)
                        for mi, (lt, rh) in enumerate(masks):
                            for r in range(4):
                                ins = nc.tensor.matmul(sb[:, r * 128:(r + 1) * 128], lhsT=lt, rhs=rh, start=False, stop=(mi == len(masks) - 1), skip_group_check=True)
                        return ins

                    def mmv(pt, kt=kt, ki=ki, nk=len(kts)):
                        ins = None
                        for r in range(4):
                            rhs = vcA[:, g, :] if branch == 0 else VA[:, kt, branch - 1, g, :]
                            ins = nc.tensor.matmul(acc[:, r * W:(r + 1) * W], lhsT=pt[:, r, :], rhs=rhs, start=(ki == 0 and r == 0), stop=(ki == nk - 1), skip_group_check=True)
                        return ins
                    last = (ki == len(kts) - 1)
                    jobs.append(dict(mms=mms, mmv=mmv, accn=accn, post=(lambda acc=acc, accn=accn, W=W: post(acc, accn, W)) if last else None))

            def run_jobs(jobs):
                n = len(jobs)
                rd = ["qT", "qTaug", "qTsel", "kcT", "kT", "kTaug", "KAc", "QAc", "NEGc", "CMd", "CMw4", "identb"]
                for i in range(n + SK):
                    if i < n:
                        j = jobs[i]
                        sb_i = 1 + (i % 5)
                        sb, sbn = PS[sb_i], "ps%d" % sb_i
                        pt, ptn = PT[i % 6], "PT%d" % (i % 6)
                        j["pt"], j["ptn"] = pt, ptn
                        P.op("pe", lambda j=j, sb=sb: j["mms"](sb), r=rd, w=[sbn])
                        P.op("act", lambda sb=sb, pt=pt: A_.activation(out=pt[:].rearrange("p r t -> p (r t)"), in_=sb[:], func=AF.Exp), r=[sbn], w=[ptn])
                    if i >= SK:
                        j = jobs[i - SK]
                        P.op("pe", lambda j=j: j["mmv"](j["pt"]), r=[j["ptn"], "vcA", "VA"], w=[j["accn"]])
                        if j["post"] is not None:
                            j["post"]()

            fcnt = [0]

            def finalize(acc, accn, W, g, tt, branch):
                a3 = acc[:, 0:4 * W].rearrange("p (r w) -> p r w", w=W)
                fi = fcnt[0] % 2
                fcnt[0] += 1
                fs, fn_ = FSM[fi], FIN[fi]
                fsn, finn = "fsm%d" % fi, "fin%d" % fi
                P.op("dve", lambda: V.tensor_scalar(out=fs[:, 0:4], in0=a3[:, :, 64], scalar1=1e-30, scalar2=None, op0=ALU.add), r=[accn], w=[fsn])
                P.op("dve", lambda: V.reciprocal(out=fs[:, 0:4], in_=fs[:, 0:4]), r=[fsn], w=[fsn])
                P.op("dve", lambda: V.tensor_tensor(out=fs[:, 4:8], in0=fs[:, 0:4], in1=SG[:, tt, branch * 8 + 4 * g:branch * 8 + 4 * g + 4], op=ALU.mult), r=[fsn, "SG"], w=[fsn + "b"])
                yv = YN[:, tt, 4 * g:4 * g + 4, :]
                if branch == 0:
                    P.op("dve", lambda: V.tensor_tensor(out=yv, in0=a3[:, :, 0:64], in1=bcast_last(fs[:, 4:8], 64), op=ALU.mult), r=[accn, fsn + "b"], w=["YN"])
                    P.op("dve", lambda: V.tensor_tensor(out=fn_[:, :, 0:32], in0=a3[:, :, 65:97], in1=bcast_last(fs[:, 0:4], 32), op=ALU.mult), r=[accn, fsn], w=[finn])
                    P.op("dve", lambda: V.tensor_reduce(out=IMP[:, tt, g, :], in_=fn_[:, :, 0:32].rearrange("p r j -> p j r"), axis=AX.X, op=ALU.add), r=[finn], w=["IMP%d_%d" % (tt, g)])
                    sc = fs[:, 8:40]
                    P.op("pool", lambda: G.tensor_tensor(out=sc, in0=IMP[:, tt, g, :], in1=CV[:, tt, :], op=ALU.mult), r=["IMP%d_%d" % (tt, g), "CV32"], w=[fsn + "c"])
                    P.op("pool", lambda: G.tensor_tensor(out=sc, in0=sc, in1=CB[:, tt, :], op=ALU.add), r=[fsn + "c", "CB32"], w=[fsn + "c"])
                    P.op("dve", lambda: V.max(out=fs[:, 40:48], in_=sc), r=[fsn + "c"], w=[fsn + "d"])
                    P.op("dve", lambda: V.tensor_scalar(out=fs[:, 48:49], in0=fs[:, 47:48], scalar1=-0.5e9, scalar2=None, op0=ALU.max), r=[fsn + "d"], w=[fsn + "e"])
                    P.op("dve", lambda: V.tensor_scalar(out=sc, in0=sc, scalar1=fs[:, 48:49], scalar2=30000.0, op0=ALU.is_ge, op1=ALU.mult), r=[fsn + "c", fsn + "e"], w=[fsn + "c"])
                    P.op("dve", lambda: V.tensor_scalar(out=SELB[:, g, tt, :], in0=sc, scalar1=-30000.0, scalar2=None, op0=ALU.add), r=[fsn + "c"], w=["SELB"])
                else:
                    P.op("dve", lambda: V.tensor_tensor(out=fn_[:], in0=a3[:, :, 0:64], in1=bcast_last(fs[:, 4:8], 64), op=ALU.mult), r=[accn, fsn + "b"], w=[finn])
                    P.op("pool", lambda: G.tensor_tensor(out=yv, in0=yv, in1=fn_[:], op=ALU.add), r=[finn, "YN"], w=["YN"])

            jobs = []
            for tt in range(NT):
                for g in range(2):
                    add_attend(jobs, g, tt, [0], 0, lambda acc, accn, W, g=g, tt=tt: finalize(acc, accn, W, g, tt, 0))
            run_jobs(jobs)
            for g in range(2):
                for tq in range(4):
                    bk = 1 + (tq % 2)

                    def trsel(g=g, tq=tq, bk=bk):
                        ins = None
                        for j in range(4):
                            ins = nc.tensor.transpose(out=PS[bk][:].bitcast(BF16)[0:32, j * 128:(j + 1) * 128], in_=SELB[:, g, tq * 4 + j, :], identity=identb[:])
                        return ins
                    P.op("pe", trsel, r=["SELB", "identb"], w=["ps%d" % bk])
                    if bk == 1:
                        P.op("dve", lambda g=g, tq=tq, bk=bk: V.tensor_copy(out=selbT[:, g, tq * 512:(tq + 1) * 512], in_=PS[bk][:].bitcast(BF16)[0:32, 0:512]), r=["ps%d" % bk], w=["selbT"])
                    else:
                        P.op("act", lambda g=g, tq=tq, bk=bk: A_.copy(out=selbT[:, g, tq * 512:(tq + 1) * 512], in_=PS[bk][:].bitcast(BF16)[0:32, 0:512]), r=["ps%d" % bk], w=["selbT"])
            for h in range(8):
                P.dma("sp", qT[67:99, h, :], selbT[:, h // 4, :], r=["selbT"], w=["qTsel"], key="qTsel")
            jobs = []
            for tt in range(NT):
                for g in range(2):
                    add_attend(jobs, g, tt, list(range(0, tt + 1)), 1, lambda acc, accn, W, g=g, tt=tt: finalize(acc, accn, W, g, tt, 1))
                    add_attend(jobs, g, tt, list(range(max(0, tt - 4), tt + 1)), 2, lambda acc, accn, W, g=g, tt=tt: finalize(acc, accn, W, g, tt, 2))
            run_jobs(jobs)
            P.dma("sp", YMIX[b].rearrange("(tt p) c -> p tt c", p=128)[:, :, 512:1024], YN[:].rearrange("p t h d -> p t (h d)"), r=["YN"], w=["YMIX"], key="YN")
        P.flush()
    if debug and "ynsa" in debug:
        with ExitStack() as es:
            t = T(es, "dbgy2", [128, 512], F32)
            for tt in range(NT):
                P.dma("sp", t[:], YMIX[0, tt * 128:(tt + 1) * 128, 512:1024], r=["YMIX"], w=["dbgy2"], key="dbgy2")
                P.dma("sp", dbg["ynsa"][tt * 128:(tt + 1) * 128, :], t[:], r=["dbgy2"], w=["dbgy2o"], key="dbgy2o")
            P.flush()


def phase_out_moe(nc, P, T, PS, I, l, YMIX, X1, xsrc, xdst, MODS, C, debug, dbg):
    identb, epsc, modT, a2 = C["identb"], C["epsc"], C["modT"], C["a2"]
    V, A_, G = nc.vector, nc.scalar, nc.gpsimd
    NBL = (debug or {}).get("_moe_nb", NB)
    NEL = (debug or {}).get("_moe_ne", NEXP)
    with ExitStack() as es:
        h2T = T(es, "h2T", [128, 8, S], BF16)
        yacc = T(es, "yacc", [128, NT, D], F32)
        GATE = T(es, "GATE", [128, NT, NEXP], F32)
        gbc = T(es, "gbc", [128, D], F32)
        rwb = T(es, "rwb", [128, 8, NEXP], BF16)
        rwl = T(es, "rwl", [128, 8, NEXP], BF16)
        rbias = T(es, "rbias", [128, NEXP], F32)
        ss = T(es, "mss", [128, 4], F32)
        xa = [T(es, "xa%d" % i, [128, D], F32) for i in range(2)]
        for b in range(NBL):
            with ExitStack() as es2:
                RSC = T(es2, "RSC", [128, NT, NEXP], F32)
                RBI = T(es2, "RBI", [128, NT, NEXP], F32)
                RSEL = T(es2, "RSEL", [128, NT, NEXP], F32)
                RTM = T(es2, "RTM", [128, 11, NT * 4], F32)
                RT1 = T(es2, "RT1", [128, 3, NT], F32)
                wob = T(es2, "wob", [128, 8, D], BF16)
                ym = [T(es2, "ym%d" % i, [128, D], F32) for i in range(2)]
                ymb = [T(es2, "ymb%d" % i, [128, D], BF16) for i in range(2)]
                ymT = [T(es2, "ymT%d" % i, [128, 8, 128], BF16) for i in range(2)]
                xnb = [T(es2, "xnb%d" % i, [128, D], BF16) for i in range(2)]
                junk = T(es2, "mjunk", [128, D], F32)
                wst = T(es2, "wst", [128, 8, 512], F32)
                rw32 = T(es2, "rw32", [128, 8, NEXP], F32)
                rw32b = T(es2, "rw32b", [128, 8, NEXP], F32)
                for hf in range(2):
                    P.dma("sp", wst[:], I["w_out"][l].rearrange("(kc p) f -> p kc f", p=128)[:, :, hf * 512:(hf + 1) * 512], w=["wst"], key="wst")
                    P.op("pool", lambda hf=hf: G.tensor_copy(out=wob[:, :, hf * 512:(hf + 1) * 512], in_=wst[:]), r=["wst"], w=["wob"])
                P.dma("sp", rw32[:], I["router_w"].rearrange("(kc p) e -> p kc e", p=128), w=["rw32"], key="rw32")
                P.dma("sp", rbias[:], I["router_bias"].partition_broadcast(128), w=["rbias"], key="rw32")
                P.op("dve", lambda: V.tensor_copy(out=rwb[:], in_=rw32[:]), r=["rw32"], w=["rwb"])
                P.op("dve", lambda: V.tensor_copy(out=rw32b[:], in_=rwb[:]), r=["rwb"], w=["rw32b"])
                P.op("dve", lambda: V.tensor_tensor(out=rw32b[:], in0=rw32[:], in1=rw32b[:], op=ALU.subtract), r=["rw32", "rw32b"], w=["rw32b"])
                P.op("dve", lambda: V.tensor_copy(out=rwl[:], in_=rw32b[:]), r=["rw32b"], w=["rwl"])
                P.dma("sp", gbc[:], MODS[l, b:b + 1, 2048:3072].partition_broadcast(128), r=["MODS"], w=["gbc"], key="gbc")
                erecs = []
                for tt in range(NT):
                    Q = RecQ()
                    Q.stage("E1")
                    sl = tt % 2
                    t0 = tt * 128
                    Y, YB, YT, XA, XN = ym[sl], ymb[sl], ymT[sl], xa[sl], xnb[sl]
                    Q.dma("sp", Y[:], YMIX[b, t0:t0 + 128, :], r=["YMIX"], w=["ym%d" % sl], key="ym%d" % sl)
                    Q.dma("sp", XA[:], xsrc[b, t0:t0 + 128, :], r=["X2"], w=["xa%d" % sl], key="xa%d" % sl)
                    Q.op("pool", lambda Y=Y, YB=YB: G.tensor_copy(out=YB[:], in_=Y[:]), r=["ym%d" % sl], w=["ymb%d" % sl])
                    Q.stage("E1b")

                    def tr(src, b0, b1):
                        def f():
                            ins = None
                            for kc in range(8):
                                tp = (PS[b0] if kc < 4 else PS[b1])[:].bitcast(BF16)
                                ins = nc.tensor.transpose(out=tp[:, (kc % 4) * 128:(kc % 4 + 1) * 128], in_=src[:, kc * 128:(kc + 1) * 128], identity=identb[:])
                            return ins
                        return f
                    Q.op("pe", tr(YB, 0, 1), r=["ymb%d" % sl, "identb"], w=["ps0", "ps1"])
                    Q.op("act", lambda YT=YT: A_.copy(out=YT[:, 0:4, :].rearrange("p k t -> p (k t)"), in_=PS[0][:].bitcast(BF16)[:, 0:512]), r=["ps0"], w=["ymT%da" % sl])
                    Q.op("dve", lambda YT=YT: V.tensor_copy(out=YT[:, 4:8, :].rearrange("p k t -> p (k t)"), in_=PS[1][:].bitcast(BF16)[:, 0:512]), r=["ps1"], w=["ymT%db" % sl])
                    for hf in range(2):
                        def mm(YT=YT, hf=hf):
                            ins = None
                            for kc in range(8):
                                ins = nc.tensor.matmul(PS[2 + hf][:], lhsT=YT[:, kc, :], rhs=wob[:, kc, hf * 512:(hf + 1) * 512], start=(kc == 0), stop=(kc == 7))
                            return ins
                        Q.op("pe", mm, r=["ymT%da" % sl, "ymT%db" % sl, "wob"], w=["ps%d" % (2 + hf)])
                        Q.op("dve", lambda Y=Y, hf=hf: V.tensor_tensor(out=Y[:, hf * 512:(hf + 1) * 512], in0=PS[2 + hf][:], in1=gbc[:, hf * 512:(hf + 1) * 512], op=ALU.mult),
                             r=["ps%d" % (2 + hf), "gbc", "ymb%d" % sl], w=["ym%d" % sl])
                    Q.op("pool", lambda Y=Y, XA=XA: G.tensor_tensor(out=XA[:], in0=XA[:], in1=Y[:], op=ALU.add), r=["ym%d" % sl, "xa%d" % sl], w=["xa%d" % sl])
                    Q.dma("pool", X1[b, t0:t0 + 128, :], XA[:], r=["xa%d" % sl], w=["X1"], key="xs%d" % sl)
                    Q.op("act", lambda XA=XA, sl=sl: A_.activation(out=junk[:], in_=XA[:], func=AF.Square, accum_out=ss[:, sl:sl + 1]), r=["xa%d" % sl], w=["mjunk", "mss%d" % sl])
                    Q.op("act", lambda sl=sl: A_.activation(out=ss[:, 2 + sl:3 + sl], in_=ss[:, sl:sl + 1], func=AF.Sqrt, scale=1.0 / D, bias=epsc[:, 0:1]), r=["mss%d" % sl, "epsc"], w=["mrs%d" % sl])
                    Q.op("dve", lambda sl=sl: V.reciprocal(out=ss[:, 2 + sl:3 + sl], in_=ss[:, 2 + sl:3 + sl]), r=["mrs%d" % sl], w=["mrs%d" % sl])
                    Q.op("dve", lambda XA=XA, XN=XN, sl=sl: V.tensor_scalar(out=XN[:], in0=XA[:], scalar1=ss[:, 2 + sl:3 + sl], scalar2=None, op0=ALU.mult), r=["xa%d" % sl, "mrs%d" % sl], w=["xnb%d" % sl])
                    Q.stage("E2")
                    Q.op("pe", tr(XN, 4, 5), r=["xnb%d" % sl, "identb"], w=["ps4", "ps5"])
                    for kc in range(8):
                        bk = 4 if kc < 4 else 5
                        src = PS[bk][:].bitcast(BF16)[:, (kc % 4) * 128:(kc % 4 + 1) * 128]
                        if kc < 4:
                            Q.op("act", lambda kc=kc, src=src, t0=t0: A_.activation(out=h2T[:, kc, t0:t0 + 128], in_=src, func=AF.Identity,
                                                                              scale=a2[:, l, kc, b:b + 1], bias=modT[:, l, 24 + kc, b:b + 1]), r=["ps4", "a2", "modT"], w=["h2T"])
                        else:
                            Q.op("dve", lambda kc=kc, src=src, t0=t0: V.tensor_scalar(out=h2T[:, kc, t0:t0 + 128], in0=src, scalar1=a2[:, l, kc, b:b + 1],
                                                                                 scalar2=modT[:, l, 24 + kc, b:b + 1], op0=ALU.mult, op1=ALU.add), r=["ps5", "a2", "modT"], w=["h2T"])
                    def mmr(t0=t0):
                        ins = None
                        for kc in range(8):
                            nc.tensor.matmul(PS[6][:, 0:NEXP], lhsT=h2T[:, kc, t0:t0 + 128], rhs=rwb[:, kc, :], start=(kc == 0), stop=False)
                            ins = nc.tensor.matmul(PS[6][:, 0:NEXP], lhsT=h2T[:, kc, t0:t0 + 128], rhs=rwl[:, kc, :], start=False, stop=(kc == 7))
                        return ins
                    Q.op("pe", mmr, r=["h2T", "rwb", "rwl"], w=["ps6"])
                    Q.op("act", lambda tt=tt: A_.activation(out=RSC[:, tt, :], in_=PS[6][:, 0:NEXP], func=AF.Sigmoid), r=["ps6"], w=["r_sc"])
                    erecs.append(Q.stages)
                emit_ops(P, erecs[0]["E1"])
                if NT > 1:
                    emit_ops(P, erecs[1]["E1"])
                emit_ops(P, erecs[0]["E1b"])
                for tt in range(NT):
                    if tt + 2 < NT:
                        emit_ops(P, erecs[tt + 2]["E1"])
                    emit_ops(P, interleave_ops(erecs[tt]["E2"], erecs[tt + 1]["E1b"] if tt + 1 < NT else []))
                G4 = NT * 4
                TTm = lambda o, a_, b_, op: V.tensor_tensor(out=o, in0=a_, in1=b_, op=op)
                sc2 = RSC[:].rearrange("p t e -> p (t e)")
                bi2 = RBI[:].rearrange("p t e -> p (t e)")
                P.op("dve", lambda: TTm(RBI[:], RSC[:], bcast_mid(rbias[:], NT), ALU.add), r=["r_sc", "rbias"], w=["r_bi"])
                v4 = RBI[:].rearrange("p t (g e) -> p (t g) e", e=4)
                rt = lambda i: RTM[:, i, :]
                P.op("dve", lambda: TTm(rt(0), v4[:, :, 0], v4[:, :, 1], ALU.max), r=["r_bi"], w=["r1"])
                P.op("dve", lambda: TTm(rt(1), v4[:, :, 0], v4[:, :, 1], ALU.min), r=["r_bi"], w=["r2"])
                P.op("dve", lambda: TTm(rt(2), v4[:, :, 2], v4[:, :, 3], ALU.max), r=["r_bi"], w=["r3"])
                P.op("dve", lambda: TTm(rt(3), v4[:, :, 2], v4[:, :, 3], ALU.min), r=["r_bi"], w=["r4"])
                P.op("dve", lambda: TTm(rt(4), rt(0), rt(2), ALU.max), r=["r1", "r3"], w=["r5"])
                P.op("dve", lambda: TTm(rt(5), rt(0), rt(2), ALU.min), r=["r1", "r3"], w=["r6"])
                P.op("dve", lambda: TTm(rt(6), rt(1), rt(3), ALU.max), r=["r2", "r4"], w=["r7"])
                P.op("dve", lambda: TTm(rt(7), rt(5), rt(6), ALU.max), r=["r6", "r7"], w=["r8"])
                P.op("dve", lambda: TTm(rt(8), rt(4), rt(7), ALU.add), r=["r5", "r8"], w=["r9"])
                P.op("dve", lambda: V.tensor_reduce(out=RT1[:, 0, :], in_=rt(8).rearrange("p (t g) -> p t g", g=4), axis=AX.X, op=ALU.max), r=["r9"], w=["r10"])
                P.op("dve", lambda: TTm(rt(9).rearrange("p (t g) -> p t g", g=4), rt(8).rearrange("p (t g) -> p t g", g=4), bcast_last(RT1[:, 0, :], 4), ALU.is_ge),
                     r=["r9", "r10"], w=["r11"])
                P.op("dve", lambda: TTm(rt(10), rt(9), rt(7), ALU.mult), r=["r11", "r8"], w=["r12"])
                P.op("dve", lambda: V.tensor_reduce(out=RT1[:, 1, :], in_=rt(10).rearrange("p (t g) -> p t g", g=4), axis=AX.X, op=ALU.add), r=["r12"], w=["r13"])
                P.op("dve", lambda: TTm(RSEL[:], RBI[:], bcast_last(RT1[:, 1, :], NEXP), ALU.is_ge), r=["r_bi", "r13"], w=["r14"])
                P.op("dve", lambda: TTm(RSEL[:].rearrange("p t (g e) -> p (t g) e", e=4), RSEL[:].rearrange("p t (g e) -> p (t g) e", e=4), bcast_last(rt(9), 4), ALU.mult),
                     r=["r14", "r11"], w=["r14"])
                P.op("dve", lambda: TTm(RSEL[:], RSEL[:], RSC[:], ALU.mult), r=["r14", "r_sc"], w=["r14"])
                P.op("dve", lambda: V.tensor_reduce(out=RT1[:, 2, :], in_=RSEL[:], axis=AX.X, op=ALU.add), r=["r14"], w=["r15"])
                P.op("dve", lambda: V.reciprocal(out=RT1[:, 2, :], in_=RT1[:, 2, :]), r=["r15"], w=["r15"])
                P.op("dve", lambda: TTm(GATE[:], RSEL[:], bcast_last(RT1[:, 2, :], NEXP), ALU.mult), r=["r14", "r15"], w=["GATE"])
                P.flush()
            with ExitStack() as es2:
                wg = [T(es2, "wg%d" % i, [128, 8, FF], BF16) for i in range(2)]
                wu = [T(es2, "wu%d" % i, [128, 8, FF], BF16) for i in range(2)]
                wd = [T(es2, "wd%d" % i, [128, 4, D], BF16) for i in range(2)]
                stg = [T(es2, "stg%d" % i, [128, 4096], F32) for i in range(2)]
                actT = [T(es2, "actT%d" % i, [128, 4, 512], BF16) for i in range(2)]
                sil = [T(es2, "sil%d" % i, [128, 512], F32) for i in range(2)]
                P.op("dve", lambda: V.memset(yacc[:], 0.0), w=["yacc"])
                P.dma("sp", gbc[:], MODS[l, b:b + 1, 5120:6144].partition_broadcast(128), r=["MODS"], w=["gbc"], key="gbc")
                si = 0
                ci = 0
                for e in range(NEL):
                    ws = e % 2
                    for (dst, src, nm) in ((wg[ws], I["exp_w_gate"][l, e].rearrange("(kc p) f -> p kc f", p=128), "wg%d" % ws),
                                           (wu[ws], I["exp_w_up"][l, e].rearrange("(kc p) f -> p kc f", p=128), "wu%d" % ws),
                                           (wd[ws], I["exp_w_down"][l, e].rearrange("(fc p) d -> p fc d", p=128), "wd%d" % ws)):
                        k_ = si % 2
                        si += 1
                        st_ = stg[k_]
                        sv = st_[:].rearrange("p (a f) -> p a f", a=dst.shape[1])
                        P.dma("sp", sv, src, w=["stg%d" % k_], key="stg%d" % k_)
                        P.op("pool", lambda dst=dst, sv=sv: G.tensor_copy(out=dst[:], in_=sv), r=["stg%d" % k_], w=[nm])
                    for tq in range(4):
                        tq0 = tq * 512
                        at = actT[ci % 2]
                        atn = "actT%d" % (ci % 2)
                        ci += 1
                        for fc in range(4):
                            def mmgu(fc=fc, ws=ws, tq0=tq0):
                                ins = None
                                for kc in range(8):
                                    ins = nc.tensor.matmul(PS[fc % 2][:], lhsT=wg[ws][:, kc, fc * 128:(fc + 1) * 128], rhs=h2T[:, kc, tq0:tq0 + 512], start=(kc == 0), stop=(kc == 7))
                                for kc in range(8):
                                    ins = nc.tensor.matmul(PS[2 + fc % 2][:], lhsT=wu[ws][:, kc, fc * 128:(fc + 1) * 128], rhs=h2T[:, kc, tq0:tq0 + 512], start=(kc == 0), stop=(kc == 7))
                                return ins
                            P.op("pe", mmgu, r=["wg%d" % ws, "wu%d" % ws, "h2T"], w=["ps%d" % (fc % 2), "ps%d" % (2 + fc % 2)])
                            sl_ = sil[fc % 2]
                            P.op("act", lambda fc=fc, sl_=sl_: A_.activation(out=sl_[:], in_=PS[fc % 2][:], func=AF.Silu), r=["ps%d" % (fc % 2)], w=["sil%d" % (fc % 2)])
                            P.op("dve", lambda fc=fc, sl_=sl_, at=at: V.tensor_tensor(out=at[:, fc, :], in0=sl_[:], in1=PS[2 + fc % 2][:], op=ALU.mult),
                                 r=["sil%d" % (fc % 2), "ps%d" % (2 + fc % 2)], w=[atn + "_%d" % fc])
                        for ts in range(4):
                            tt = tq * 4 + ts
                            for hf in range(2):
                                pb = 4 + (ts * 2 + hf) % 4

                                def mmd(at=at, ts=ts, hf=hf, pb=pb, ws=ws):
                                    ins = None
                                    for fc in range(4):
                                        ins = nc.tensor.matmul(PS[pb][:], lhsT=at[:, fc, ts * 128:(ts + 1) * 128], rhs=wd[ws][:, fc, hf * 512:(hf + 1) * 512], start=(fc == 0), stop=(fc == 3))
                                    return ins
                                P.op("pe", mmd, r=[atn + "_%d" % fc for fc in range(4)] + ["wd%d" % ws], w=["ps%d" % pb])
                                P.op("dve", lambda tt=tt, hf=hf, pb=pb, e=e: V.scalar_tensor_tensor(
                                    out=yacc[:, tt, hf * 512:(hf + 1) * 512], in0=PS[pb][:], scalar=GATE[:, tt, e:e + 1], in1=yacc[:, tt, hf * 512:(hf + 1) * 512],
                                    op0=ALU.mult, op1=ALU.add), r=["ps%d" % pb, "GATE", "yacc"], w=["yacc"])
                for tt in range(NT):
                    sl = tt % 2
                    t0 = tt * 128
                    XA = xa[sl]
                    P.dma("sp", XA[:], X1[b, t0:t0 + 128, :], r=["X1"], w=["xa%d" % sl], key="xa%d" % sl)
                    P.op("dve", lambda tt=tt: V.tensor_tensor(out=yacc[:, tt, :], in0=yacc[:, tt, :], in1=gbc[:], op=ALU.mult), r=["yacc", "gbc"], w=["yacc"])
                    P.op("pool", lambda tt=tt, XA=XA: G.tensor_tensor(out=XA[:], in0=XA[:], in1=yacc[:, tt, :], op=ALU.add), r=["yacc", "xa%d" % sl], w=["xa%d" % sl])
                    P.dma("pool", xdst[b, t0:t0 + 128, :], XA[:], r=["xa%d" % sl], w=["X2"], key="xs%d" % sl)
                P.flush()


def build(debug=None):
    nc = bass.Bass("TRN2", target_bir_lowering=False)
    dt_in = lambda name, shape: nc.dram_tensor(name, list(shape), F32, kind="ExternalInput").ap()
    I = {}
    I["x"] = dt_in("x", [NB, S, D])
    I["cT"] = dt_in("cT", [128, 8, NB])
    I["ada_w"] = dt_in("ada_w", [DEPTH, D, 6 * D])
    I["ada_bT"] = dt_in("ada_bT", [DEPTH, 128, 48])
    I["ada_b"] = dt_in("ada_b", [DEPTH, 1, 6 * D])
    I["norm1_gT"] = dt_in("norm1_gT", [DEPTH, 128, 8])
    I["norm2_gT"] = dt_in("norm2_gT", [DEPTH, 128, 8])
    I["w_in"] = dt_in("w_in", [DEPTH, D, INC])
    I["gdn_conv_w"] = dt_in("gdn_conv_w", [DEPTH, 1, 4 * 1536])
    I["gdn_a_log"] = dt_in("gdn_a_log", [DEPTH, 1, 8])
    I["gdn_dt_bias"] = dt_in("gdn_dt_bias", [DEPTH, 1, 8])
    I["gdn_norm_g"] = dt_in("gdn_norm_g", [DEPTH, 1, 64])
    I["nsa_q_norm_g"] = dt_in("nsa_q_norm_g", [DEPTH, 1, 64])
    I["nsa_k_norm_g"] = dt_in("nsa_k_norm_g", [DEPTH, 1, 192])
    I["cmp_peT"] = dt_in("cmp_peT", [DEPTH, 2, 64, 32])
    I["cmp_w1"] = dt_in("cmp_w1", [DEPTH, 2, 2048, 256])
    I["cmp_b1T"] = dt_in("cmp_b1T", [DEPTH, 2, 128, 2])
    I["cmp_w2"] = dt_in("cmp_w2", [DEPTH, 2, 256, 64])
    I["cmp_b2"] = dt_in("cmp_b2", [DEPTH, 2, 1, 64])
    I["w_out"] = dt_in("w_out", [DEPTH, D, D])
    I["router_w"] = dt_in("router_w", [D, NEXP])
    I["router_bias"] = dt_in("router_bias", [1, NEXP])
    I["exp_w_gate"] = dt_in("exp_w_gate", [DEPTH, NEXP, D, FF])
    I["exp_w_up"] = dt_in("exp_w_up", [DEPTH, NEXP, D, FF])
    I["exp_w_down"] = dt_in("exp_w_down", [DEPTH, NEXP, FF, D])
    for k, (shp, npdt) in CONST_SHAPES.items():
        if npdt == np.float32:
            I[k] = dt_in(k, shp)
        else:
            I[k] = nc.dram_tensor(k, list(shp), BF16, kind="ExternalInput").ap()
    out = nc.dram_tensor("out", [NB, S, D], F32, kind="ExternalOutput").ap()
    dbg = {}
    if debug:
        for name, shp in debug.items():
            if name.startswith("_"):
                continue
            dbg[name] = nc.dram_tensor("dbg_" + name, list(shp), F32, kind="ExternalOutput").ap()
    PROJ = nc.dram_tensor("PROJ", [NB, PADR + S, INC], F32, kind="Internal").ap()
    MODS = nc.dram_tensor("MODS", [DEPTH, NB, 6 * D], F32, kind="Internal").ap()
    YMIX = nc.dram_tensor("YMIX", [NB, S, D], F32, kind="Internal").ap()
    X1 = nc.dram_tensor("X1", [NB, S, D], F32, kind="Internal").ap()
    X2 = nc.dram_tensor("X2", [NB, S, D], F32, kind="Internal").ap()

    with ExitStack() as ges:
        P = Prog(nc, ges)
        _cnt = [0]

        def T(es, name, shape, dt):
            _cnt[0] += 1
            return es.enter_context(nc.sbuf_tensor("s%d_%s" % (_cnt[0], name), list(shape), dt))
        PS = [ges.enter_context(nc.psum_tensor("psb%d" % i, [128, 512], F32)) for i in range(8)]
        ident = T(ges, "ident", [128, 128], F32)
        identb = T(ges, "identb", [128, 128], BF16)
        condT = T(ges, "condT", [128, 8, NB], F32)
        modT = T(ges, "modT", [128, DEPTH, 48, NB], F32)
        a1 = T(ges, "a1", [128, DEPTH, 8, NB], F32)
        a2 = T(ges, "a2", [128, DEPTH, 8, NB], F32)
        epsc = T(ges, "epsc", [128, 4], F32)
        P.op("dve", lambda: nc.vector.memset(epsc[:, 0:1], EPS), w=["epsc"])
        P.op("dve", lambda: nc.vector.memset(epsc[:, 1:2], 1.0), w=["epsc"])
        P.op("dve", lambda: nc.vector.memset(epsc[:, 2:3], 0.0), w=["epsc"])
        P.op("dve", lambda: nc.vector.memset(epsc[:, 3:4], 1e-30), w=["epsc"])
        cU = T(ges, "cU", [128, 128], F32)
        cOnes = T(ges, "cOnes", [128, 128], F32)
        cBm = T(ges, "cBm", [128, 128], F32)
        cM2 = T(ges, "cM2", [128, 2, 128], F32)
        for nm, t in (("cU", cU), ("cOnes", cOnes), ("cBm", cBm), ("cM2", cM2)):
            P.dma("sp", t[:], I[nm], w=[nm], key="c0")
        P.dma("sp", ident[:], I["ident"], w=["ident"], key="c0")
        P.dma("sp", condT[:], I["cT"], w=["condT"], key="c0")
        P.op("dve", lambda: nc.vector.tensor_copy(out=identb[:], in_=ident[:]), r=["ident"], w=["identb"])
        P.op("act", lambda: nc.scalar.activation(out=condT[:], in_=condT[:], func=AF.Silu), r=["condT"], w=["condT"])
        P.flush()

        with ExitStack() as es:
            wst = [T(es, "adaw%d" % i, [128, 8, 512], F32) for i in range(4)]
            mrow = T(es, "mrow", [NB, 6 * D], F32)
            brow = T(es, "brow", [NB, 6 * D], F32)
            bT = T(es, "bT", [128, DEPTH, 48], F32)
            g1T = T(es, "g1T", [128, DEPTH, 8], F32)
            g2T = T(es, "g2T", [128, DEPTH, 8], F32)
            P.dma("sp", bT[:], I["ada_bT"].rearrange("l p c -> p l c"), w=["bT"], key="c1")
            P.dma("sp", g1T[:], I["norm1_gT"].rearrange("l p c -> p l c"), w=["g1T"], key="c1")
            P.dma("sp", g2T[:], I["norm2_gT"].rearrange("l p c -> p l c"), w=["g2T"], key="c1")
            gi = 0
            for l in range(DEPTH):
                P.dma("sp", brow[:], I["ada_b"][l].partition_broadcast(NB), w=["brow"], key="brow")
                for fg in range(12):
                    sl = gi % 4
                    gi += 1
                    wt = wst[sl]
                    src = I["ada_w"][l].rearrange("(kc p) f -> p kc f", p=128)[:, :, fg * 512:(fg + 1) * 512]
                    P.dma("sp" if sl % 2 == 0 else "pool", wt[:], src, w=["adaw%d" % sl], key="adaw%d" % sl)
                    psr = PS[sl * 2]
                    psc = PS[sl * 2 + 1]

                    def mm(wt=wt, psr=psr):
                        ins = None
                        for kc in range(8):
                            ins = nc.tensor.matmul(psr[0:NB, :], lhsT=condT[:, kc, :], rhs=wt[:, kc, :],
                                                   start=(kc == 0), stop=(kc == 7))
                        return ins
                    P.op("pe", mm, r=["adaw%d" % sl, "condT"], w=["ps%d" % (sl * 2)])
                    P.op("dve", lambda psr=psr, fg=fg: nc.vector.tensor_tensor(
                        out=mrow[:, fg * 512:(fg + 1) * 512], in0=psr[0:NB, :], in1=brow[:, fg * 512:(fg + 1) * 512], op=ALU.add),
                        r=["ps%d" % (sl * 2), "brow"], w=["mrow_%d" % fg])
                mnames = ["mrow_%d" % fg for fg in range(12)]

                def trm():
                    ins = None
                    for c_ in range(48):
                        ins = nc.tensor.transpose(out=PS[1][:, c_ * NB:(c_ + 1) * NB], in_=mrow[:, c_ * 128:(c_ + 1) * 128], identity=ident[0:NB, 0:NB])
                    return ins
                P.op("pe", trm, r=mnames + ["ident"], w=["ps1"])
                P.op("dve", lambda l=l: nc.vector.tensor_copy(out=modT[:, l].rearrange("p c b -> p (c b)"), in_=PS[1][:, 0:48 * NB]), r=["ps1"], w=["modT"])
                P.dma("sp", MODS[l], mrow[:], r=mnames, w=["MODS"], key="mods")
                P.op("dve", lambda l=l: nc.vector.scalar_tensor_tensor(
                    out=a1[:, l], in0=modT[:, l, 8:16, :], scalar=1.0, in1=bcast_last(g1T[:, l, :], NB),
                    op0=ALU.add, op1=ALU.mult), r=["modT", "g1T"], w=["a1"])
                P.op("dve", lambda l=l: nc.vector.scalar_tensor_tensor(
                    out=a2[:, l], in0=modT[:, l, 32:40, :], scalar=1.0, in1=bcast_last(g2T[:, l, :], NB),
                    op0=ALU.add, op1=ALU.mult), r=["modT", "g2T"], w=["a2"])
            P.flush()

        if debug and "mods" in debug:
            with ExitStack() as es:
                t = T(es, "dbgm", [NB, 6 * D], F32)
                P.dma("sp", t[:], MODS[0], r=["MODS"], w=["dbgm"], key="dbgm")
                P.dma("sp", dbg["mods"], t[:], r=["dbgm"], w=["dbgmo"], key="dbgmo")
                t2 = T(es, "dbgm2", [128, DEPTH * 48 * NB], F32)
                P.op("dve", lambda: nc.vector.tensor_copy(out=t2[:], in_=modT[:].rearrange("p l c b -> p (l c b)")), r=["modT"], w=["dbgm2"])
                P.dma("sp", dbg["modT"], t2[:], r=["dbgm2"], w=["dbgmo2"], key="dbgmo2")
                P.flush()
        for l in range(DEPTH):
            if debug and debug.get("_stop") == "A":
                break
            xsrc = I["x"] if l == 0 else X2
            xdst = X2 if l == 0 else out
            with ExitStack() as es:
                wbf = T(es, "winbf", [128, 8, INC], BF16)
                wstg = [T(es, "winst%d" % i, [128, 8, 512], F32) for i in range(2)]
                xt = [T(es, "xt%d" % i, [128, D], F32) for i in range(2)]
                junk = T(es, "junk", [128, D], F32)
                xn = [T(es, "xn%d" % i, [128, D], BF16) for i in range(2)]
                hT = [T(es, "hT%d" % i, [128, 8, 128], BF16) for i in range(2)]
                stage = [T(es, "stage%d" % i, [128, INC], F32) for i in range(2)]
                ss = T(es, "ss", [128, 4], F32)
                zpad = T(es, "zpad", [PADR, INC], F32)
                P.op("dve", lambda: nc.vector.memset(zpad[:], 0.0), w=["zpad"])
                for b in range(NB):
                    P.dma("sp", PROJ[b, 0:PADR, :], zpad[:], r=["zpad"], w=["PROJ"], key="zpad")
                for cg in range(7):
                    c0 = cg * 512
                    cw = min(512, INC - c0)
                    sl = cg % 2
                    P.dma("sp" if sl == 0 else "pool", wstg[sl][:, :, 0:cw], I["w_in"][l].rearrange("(kc p) f -> p kc f", p=128)[:, :, c0:c0 + cw],
                          w=["winst%d" % sl], key="winst%d" % sl)
                    if sl == 0:
                        P.op("pool", lambda sl=sl, c0=c0, cw=cw: nc.gpsimd.tensor_copy(out=wbf[:, :, c0:c0 + cw], in_=wstg[sl][:, :, 0:cw]),
                             r=["winst%d" % sl], w=["winbf%d" % cg])
                    else:
                        P.op("act", lambda sl=sl, c0=c0, cw=cw: nc.scalar.copy(out=wbf[:, :, c0:c0 + cw], in_=wstg[sl][:, :, 0:cw]),
                             r=["winst%d" % sl], w=["winbf%d" % cg])
                it = 0
                BM = (debug or {}).get("_bm", 9)
                brecs = []
                for b in range(NB):
                    for tt in range(NT):
                        sl = it % 2
                        it += 1
                        Q = RecQ()
                        brecs.append(Q.stages)
                        Q.stage("B1")
                        X, XN, HT, ST = xt[sl], xn[sl], hT[sl], stage[sl]
                        Q.dma("sp", X[:], xsrc[b, tt * 128:(tt + 1) * 128, :], w=["xt%d" % sl], key="xt%d" % sl)
                        Q.op("act", lambda X=X, sl=sl: nc.scalar.activation(out=junk[:], in_=X[:], func=AF.Square, accum_out=ss[:, sl:sl + 1]),
                             r=["xt%d" % sl], w=["junk", "ss%d" % sl])
                        Q.op("act", lambda sl=sl: nc.scalar.activation(out=ss[:, 2 + sl:3 + sl], in_=ss[:, sl:sl + 1], func=AF.Sqrt, scale=1.0 / D, bias=epsc[:, 0:1]),
                             r=["ss%d" % sl], w=["rs%d" % sl])
                        Q.op("dve", lambda sl=sl: nc.vector.reciprocal(out=ss[:, 2 + sl:3 + sl], in_=ss[:, 2 + sl:3 + sl]), r=["rs%d" % sl], w=["rs%d" % sl])
                        Q.op("dve", lambda X=X, XN=XN, sl=sl: nc.vector.tensor_scalar(out=XN[:], in0=X[:], scalar1=ss[:, 2 + sl:3 + sl], scalar2=None, op0=ALU.mult),
                             r=["xt%d" % sl, "rs%d" % sl], w=["xn%d" % sl])
                        tpa, tpb_ = PS[sl * 2], PS[sl * 2 + 1]

                        def tr(XN=XN, tpa=tpa, tpb_=tpb_):
                            ins = None
                            for kc in range(8):
                                tp = (tpa if kc < 4 else tpb_)[:].bitcast(BF16)
                                ins = nc.tensor.transpose(out=tp[:, (kc % 4) * 128:(kc % 4 + 1) * 128], in_=XN[:, kc * 128:(kc + 1) * 128], identity=identb[:])
                            return ins
                        Q.op("pe", tr, r=["xn%d" % sl, "identb"], w=["ps%d" % (sl * 2), "ps%d" % (sl * 2 + 1)])
                        for kc in range(8):
                            e = "act" if kc < 4 else "dve"
                            tp = tpa if kc < 4 else tpb_
                            if e == "act":
                                f = lambda HT=HT, tp=tp, kc=kc, b=b: nc.scalar.activation(
                                    out=HT[:, kc, :], in_=tp[:].bitcast(BF16)[:, (kc % 4) * 128:(kc % 4 + 1) * 128], func=AF.Identity,
                                    scale=a1[:, l, kc, b:b + 1], bias=modT[:, l, kc, b:b + 1])
                            else:
                                f = lambda HT=HT, tp=tp, kc=kc, b=b: nc.vector.tensor_scalar(
                                    out=HT[:, kc, :], in0=tp[:].bitcast(BF16)[:, (kc % 4) * 128:(kc % 4 + 1) * 128],
                                    scalar1=a1[:, l, kc, b:b + 1], scalar2=modT[:, l, kc, b:b + 1], op0=ALU.mult, op1=ALU.add)
                            Q.op(e, f, r=["ps%d" % (sl * 2 + (0 if kc < 4 else 1)), "a1", "modT"], w=["hT%d_%d" % (sl, kc)])
                        Q.stage("B2")
                        for cg in range(7):
                            c0 = cg * 512
                            cw = min(512, INC - c0)
                            pb = 4 + (cg % 4)
                            pt = PS[pb]

                            def mm(HT=HT, pt=pt, c0=c0, cw=cw):
                                ins = None
                                for kc in range(8):
                                    ins = nc.tensor.matmul(pt[:, 0:cw], lhsT=HT[:, kc, :], rhs=wbf[:, kc, c0:c0 + cw], start=(kc == 0), stop=(kc == 7))
                                return ins
                            Q.op("pe", mm, r=["hT%d_%d" % (sl, kc) for kc in range(8)] + ["winbf%d" % cg], w=["ps%d" % pb])
                            if cg % 2 == 0:
                                Q.op("act", lambda ST=ST, pt=pt, c0=c0, cw=cw: nc.scalar.copy(out=ST[:, c0:c0 + cw], in_=pt[:, 0:cw]),
                                     r=["ps%d" % pb], w=["stage%d" % sl])
                            else:
                                Q.op("dve", lambda ST=ST, pt=pt, c0=c0, cw=cw: nc.vector.tensor_copy(out=ST[:, c0:c0 + cw], in_=pt[:, 0:cw]),
                                     r=["ps%d" % pb], w=["stage%d" % sl])
                        Q.dma("pool", PROJ[b, PADR + tt * 128:PADR + (tt + 1) * 128, :], ST[:], r=["stage%d" % sl], w=["PROJ"], key="stage%d" % sl)
                emit_ops(P, brecs[0]["B1"])
                for i_ in range(len(brecs)):
                    emit_ops(P, interleave_ops(brecs[i_]["B2"], brecs[i_ + 1]["B1"] if i_ + 1 < len(brecs) else []))
                P.flush()
            if debug and "proj" in debug and l == debug.get("_layer", 0):
                with ExitStack() as es:
                    t = T(es, "dbgt", [128, INC], F32)
                    for tt in range(NT):
                        P.dma("sp", t[:], PROJ[0, PADR + tt * 128:PADR + (tt + 1) * 128, :], r=["PROJ"], w=["dbgt"], key="dbgt")
                        P.dma("sp", dbg["proj"][tt * 128:(tt + 1) * 128, :], t[:], r=["dbgt"], w=["dbgo"], key="dbgo")
                    P.flush()
            if debug and debug.get("_stop") == "B":
                break
            CC = dict(ident=ident, identb=identb, cU=cU, cOnes=cOnes, cBm=cBm, cM2=cM2, epsc=epsc)
            if not (debug and debug.get("_skip_gdn")):
                phase_gdn(nc, P, T, PS, I, l, PROJ, YMIX, CC, debug, dbg)
            if debug and debug.get("_stop") == "GDN":
                break
            if not (debug and debug.get("_skip_nsa")):
                phase_nsa(nc, P, T, PS, I, l, PROJ, YMIX, CC, debug, dbg)
            if debug and debug.get("_stop") == "NSA":
                break
            CC.update(modT=modT, a2=a2)
            phase_out_moe(nc, P, T, PS, I, l, YMIX, X1, xsrc, xdst, MODS, CC, debug, dbg)
            if debug and "x2" in debug:
                with ExitStack() as es:
                    t = T(es, "dbgx2", [128, D], F32)
                    for tt in range(NT):
                        P.dma("sp", t[:], X2[0, tt * 128:(tt + 1) * 128, :], r=["X2"], w=["dbgx2"], key="dbgx2")
                        P.dma("sp", dbg["x2"][tt * 128:(tt + 1) * 128, :], t[:], r=["dbgx2"], w=["dbgx2o"], key="dbgx2o")
                    P.flush()
            if debug and debug.get("_stop") == "L0":
                break
        P.flush()
        print("instructions emitted:", P.nins)
        if os.environ.get("SEMDBG"):
            print({v.name if hasattr(v, "name") else str(v): k for k, v in P.dsem.items()})
            print(list(P.dsem.keys()))
    return nc


def prep_inputs(inputs, core):
    f = lambda a: np.ascontiguousarray(np.asarray(a, dtype=np.float32))
    b0 = core * NB
    m = {}
    m["x"] = f(inputs["x"][b0:b0 + NB])
    m["cT"] = f(np.asarray(inputs["c"])[b0:b0 + NB].reshape(NB, 8, 128).transpose(2, 1, 0))
    m["ada_w"] = f(inputs["ada_w"])
    m["ada_bT"] = f(np.asarray(inputs["ada_b"]).reshape(DEPTH, 48, 128).transpose(0, 2, 1))
    m["ada_b"] = f(np.asarray(inputs["ada_b"]).reshape(DEPTH, 1, 6 * D))
    m["norm1_gT"] = f(np.asarray(inputs["norm1_g"]).reshape(DEPTH, 8, 128).transpose(0, 2, 1))
    m["norm2_gT"] = f(np.asarray(inputs["norm2_g"]).reshape(DEPTH, 8, 128).transpose(0, 2, 1))
    m["w_in"] = f(inputs["w_in"])
    m["gdn_conv_w"] = f(np.asarray(inputs["gdn_conv_w"]).reshape(DEPTH, 1, 4 * 1536))
    m["gdn_a_log"] = f(np.asarray(inputs["gdn_a_log"]).reshape(DEPTH, 1, 8))
    m["gdn_dt_bias"] = f(np.asarray(inputs["gdn_dt_bias"]).reshape(DEPTH, 1, 8))
    m["gdn_norm_g"] = f(np.asarray(inputs["gdn_norm_g"]).reshape(DEPTH, 1, 64))
    m["nsa_q_norm_g"] = f(np.asarray(inputs["nsa_q_norm_g"]).reshape(DEPTH, 1, 64))
    m["nsa_k_norm_g"] = f(np.asarray(inputs["nsa_k_norm_g"]).reshape(DEPTH, 1, 192))
    m["cmp_peT"] = f(np.asarray(inputs["cmp_pe"]).transpose(0, 1, 3, 2))
    m["cmp_w1"] = f(inputs["cmp_w1"])
    m["cmp_b1T"] = f(np.asarray(inputs["cmp_b1"]).reshape(DEPTH, 2, 2, 128).transpose(0, 1, 3, 2))
    m["cmp_w2"] = f(inputs["cmp_w2"])
    m["cmp_b2"] = f(np.asarray(inputs["cmp_b2"]).reshape(DEPTH, 2, 1, 64))
    m["w_out"] = f(inputs["w_out"])
    m["router_w"] = f(inputs["router_w"])
    m["router_bias"] = f(np.asarray(inputs["router_bias"]).reshape(1, NEXP))
    m["exp_w_gate"] = f(inputs["exp_w_gate"])
    m["exp_w_up"] = f(inputs["exp_w_up"])
    m["exp_w_down"] = f(inputs["exp_w_down"])
    m.update(make_consts())
    return m


_NC_CACHE = {}


def kernel(**inputs):
    if "nc" not in _NC_CACHE:
        _NC_CACHE["nc"] = build()
    nc = _NC_CACHE["nc"]
    in_maps = [prep_inputs(inputs, c) for c in range(8)]
    res = run_bass_kernel_spmd(nc, in_maps, core_ids=list(range(8)))
    return np.concatenate([np.asarray(r["out"], dtype=np.float32) for r in res.results], axis=0)
```
